# Optimizing a Trainium2 kernel written in Bass

```python
import jax, jax.numpy as jnp
from jax import lax
import numpy as np

D_MODEL = 1024
BATCH = 2
SEQ = 16384
DEPTH = 2

GRID_W = 64
CTX_LEN = 256
D_MIX = D_MODEL
RWKV_HEAD = 64
RWKV_WIDTH = D_MIX // 2
RWKV_HEADS = RWKV_WIDTH // RWKV_HEAD
DECAY_LORA = 64
ICLR_LORA = 64
GATE_LORA = 128
RWKV_COLS = 3 * RWKV_WIDTH + DECAY_LORA + ICLR_LORA + GATE_LORA
RWKV_SPLITS = (RWKV_WIDTH, 2 * RWKV_WIDTH, 3 * RWKV_WIDTH, 3 * RWKV_WIDTH + DECAY_LORA, 3 * RWKV_WIDTH + DECAY_LORA + ICLR_LORA)
SHIFT_TAPS = 3
POOL_WIDTH = D_MIX // 4
POOL_WINDOWS = (2, 4, 8, 16)
POOL_GROUPS = len(POOL_WINDOWS)
POOL_GC = POOL_WIDTH // POOL_GROUPS
FOURIER_WIDTH = D_MIX - RWKV_WIDTH - POOL_WIDTH
FOURIER_HEADS = 4
FOURIER_HC = FOURIER_WIDTH // FOURIER_HEADS
IN_COLS = RWKV_COLS + POOL_WIDTH + FOURIER_WIDTH
MIX_SPLITS = (RWKV_COLS, RWKV_COLS + POOL_WIDTH)
N_EXPERTS = 16
EXPERT_FF = D_MODEL
CAPACITY_FACTOR = 2
N_MOD = 6
RMS_EPS = 1e-6
LNX_EPS = 64e-5
F32 = jnp.float32

kernel_name = "hybrid_rwkv7_pool_fourier_ec_moe_dit"


def heads(t):
    return t.reshape(t.shape[0], t.shape[1], RWKV_HEADS, RWKV_HEAD)


def rmsnorm(x, g):
    xf = x.astype(F32)
    y = xf * lax.rsqrt(jnp.mean(xf * xf, axis=-1, keepdims=True) + RMS_EPS)
    return (y * g.astype(F32)).astype(x.dtype)


def adaln(cond, ada_w, ada_b):
    return jnp.split(jax.nn.silu(cond) @ ada_w + ada_b, N_MOD, axis=-1)


def modulate(x, shift, scale):
    return x * (1 + scale) + shift


def centred_shift(u, shift_w):
    up = jnp.pad(u, ((0, 0), (1, 1), (0, 0)))
    return up[:, :-2] * shift_w[0] + up[:, 1:-1] * shift_w[1] + up[:, 2:] * shift_w[2]


def rwkv_features(u, shift_w, k_k):
    u = centred_shift(u, shift_w)
    r, k, v, xw, xa, xg = jnp.split(u, RWKV_SPLITS, axis=-1)
    kk = heads(k * k_k).astype(F32)
    kk = kk * lax.rsqrt(jnp.sum(kk * kk, axis=-1, keepdims=True) + 1e-12)
    return r, k, v, xw, xa, xg, kk


def rwkv_direction(k, xw, xa, kk, w0, w_up, a0, a_up, k_a):
    w_log = -jax.nn.softplus(-(w0 + jnp.tanh(xw) @ w_up).astype(F32)) - 0.5
    decay = jnp.exp(-jnp.exp(w_log))
    a_lr = jax.nn.sigmoid(a0 + xa @ a_up)
    k_dir = k * (1 + (a_lr - 1) * k_a)
    return heads(decay), heads(k_dir), -kk, kk * heads(a_lr).astype(F32)


def delta_scan(r, w, k, v, a, b, s0, reverse, with_y):
    seqs = (w, k, v, a, b) + ((r,) if with_y else ())
    xs = tuple(jnp.moveaxis(t.astype(F32), 1, 0) for t in seqs)

    def step(S, inp):
        w_t, k_t, v_t, a_t, b_t = inp[:5]
        sa = jnp.einsum("bhvk,bhk->bhv", S, a_t)
        S = S * w_t[:, :, None, :] + sa[..., None] * b_t[:, :, None, :] + v_t[..., None] * k_t[:, :, None, :]
        y = jnp.einsum("bhvk,bhk->bhv", S, inp[5]) if with_y else None
        return S, y

    S, ys = lax.scan(step, s0, xs, reverse=reverse)
    return (jnp.moveaxis(ys, 0, 1) if with_y else None), S


def rwkv_readout(y, bonus, v, xg, gate_up, lnx_g, lnx_b):
    B, T = y.shape[0], y.shape[1]
    mu = jnp.mean(y, axis=-1, keepdims=True)
    var = jnp.mean(jnp.square(y - mu), axis=-1, keepdims=True)
    yn = ((y - mu) * lax.rsqrt(var + LNX_EPS)).reshape(B, T, RWKV_WIDTH) * lnx_g + lnx_b
    yn = yn + (bonus[..., None] * heads(v).astype(F32)).reshape(B, T, RWKV_WIDTH)
    return (yn * (jax.nn.sigmoid(xg) @ gate_up)).astype(v.dtype)


def rwkv_mixer(u_lat, u_ctx, ctx_out, shift_w, decay_w0, decay_up, iclr_a0, iclr_up, gate_up, k_k, k_a, r_k, lnx_g, lnx_b):
    r_l, k_l, v_l, xw_l, xa_l, xg_l, kk_l = rwkv_features(u_lat, shift_w, k_k)
    r_c, k_c, v_c, xw_c, xa_c, xg_c, kk_c = rwkv_features(u_ctx, shift_w, k_k)
    s_zero = jnp.zeros((u_ctx.shape[0], RWKV_HEADS, RWKV_HEAD, RWKV_HEAD), F32)
    y_lat, bonus_lat, y_ctx, bonus_ctx = [], [], [], []
    for d in range(2):
        rev = d == 1
        w_c, kd_c, a_c, b_c = rwkv_direction(k_c, xw_c, xa_c, kk_c, decay_w0[d], decay_up[d], iclr_a0[d], iclr_up[d], k_a)
        yc, s_ctx = delta_scan(heads(r_c), w_c, kd_c, heads(v_c), a_c, b_c, s_zero, rev, ctx_out)
        w_l, kd_l, a_l, b_l = rwkv_direction(k_l, xw_l, xa_l, kk_l, decay_w0[d], decay_up[d], iclr_a0[d], iclr_up[d], k_a)
        yl, _ = delta_scan(heads(r_l), w_l, kd_l, heads(v_l), a_l, b_l, s_ctx, rev, True)
        y_lat.append(yl)
        bonus_lat.append(jnp.sum(heads(r_l) * kd_l * r_k, axis=-1))
        if ctx_out:
            y_ctx.append(yc)
            bonus_ctx.append(jnp.sum(heads(r_c) * kd_c * r_k, axis=-1))
    out_lat = rwkv_readout(y_lat[0] + y_lat[1], bonus_lat[0] + bonus_lat[1], v_l, xg_l, gate_up, lnx_g, lnx_b)
    out_ctx = rwkv_readout(y_ctx[0] + y_ctx[1], bonus_ctx[0] + bonus_ctx[1], v_c, xg_c, gate_up, lnx_g, lnx_b) if ctx_out else None
    return out_lat, out_ctx


def window_bounds(n, win):
    pos = jnp.arange(n)
    return jnp.maximum(pos - win // 2, 0), jnp.minimum(pos + win // 2, n)


def pool2d_minus_self(u, rows):
    B, T, C = u.shape
    grid = u.reshape(B, rows, GRID_W, C).astype(F32)
    sat = jnp.pad(jnp.cumsum(jnp.cumsum(grid, axis=1), axis=2), ((0, 0), (1, 0), (1, 0), (0, 0)))
    means = []
    for gi, win in enumerate(POOL_WINDOWS):
        s = sat[..., gi * POOL_GC:(gi + 1) * POOL_GC]
        r_lo, r_hi = window_bounds(rows, win)
        c_lo, c_hi = window_bounds(GRID_W, win)
        s_hi, s_lo = s[:, r_hi], s[:, r_lo]
        box = s_hi[:, :, c_hi] - s_lo[:, :, c_hi] - s_hi[:, :, c_lo] + s_lo[:, :, c_lo]
        count = ((r_hi - r_lo)[:, None] * (c_hi - c_lo)[None, :]).astype(F32)
        means.append(box / count[None, :, :, None])
    pooled = jnp.concatenate(means, axis=-1).reshape(B, T, C)
    return (pooled - u.astype(F32)).astype(u.dtype)


def pool1d_minus_self(u):
    B, T, C = u.shape
    cs = jnp.pad(jnp.cumsum(u.astype(F32), axis=1), ((0, 0), (1, 0), (0, 0)))
    means = []
    for gi, win in enumerate(POOL_WINDOWS):
        s = cs[..., gi * POOL_GC:(gi + 1) * POOL_GC]
        lo, hi = window_bounds(T, win)
        means.append((s[:, hi] - s[:, lo]) / (hi - lo).astype(F32)[None, :, None])
    return (jnp.concatenate(means, axis=-1) - u.astype(F32)).astype(u.dtype)


def pool_readout(p, pool_w, pool_scale):
    B, T = p.shape[0], p.shape[1]
    ph = p.reshape(B, T, POOL_GROUPS, POOL_GC)
    return jnp.einsum("btgc,gcd->btgd", ph, pool_w).reshape(B, T, POOL_WIDTH) * pool_scale


def fourier_mixer(u, fourier_w):
    B, T = u.shape[0], u.shape[1]
    uh = u.reshape(B, T, FOURIER_HEADS, FOURIER_HC).astype(F32)
    f = jnp.real(jnp.fft.fft2(uh, axes=(1, 3), norm="ortho")).astype(u.dtype)
    return jnp.einsum("bthc,hcd->bthd", f, fourier_w).reshape(B, T, FOURIER_WIDTH)


def token_mixing(xn_lat, xn_ctx, rows, ctx_out, w_in, shift_w, decay_w0, decay_up, iclr_a0, iclr_up, gate_up,
                 k_k, k_a, r_k, lnx_g, lnx_b, pool_w, pool_scale, fourier_w, w_out):
    p_lat = xn_lat @ w_in
    p_ctx = xn_ctx @ (w_in if ctx_out else w_in[:, :RWKV_COLS])
    u_rw_l, u_pool_l, u_four_l = jnp.split(p_lat, MIX_SPLITS, axis=-1)
    rw_l, rw_c = rwkv_mixer(u_rw_l, p_ctx[..., :RWKV_COLS], ctx_out, shift_w, decay_w0, decay_up, iclr_a0, iclr_up,
                            gate_up, k_k, k_a, r_k, lnx_g, lnx_b)
    out_lat = jnp.concatenate([rw_l, pool_readout(pool2d_minus_self(u_pool_l, rows), pool_w, pool_scale),
                               fourier_mixer(u_four_l, fourier_w)], axis=-1) @ w_out
    if not ctx_out:
        return out_lat, None
    _, u_pool_c, u_four_c = jnp.split(p_ctx, MIX_SPLITS, axis=-1)
    out_ctx = jnp.concatenate([rw_c, pool_readout(pool1d_minus_self(u_pool_c), pool_w, pool_scale),
                               fourier_mixer(u_four_c, fourier_w)], axis=-1) @ w_out
    return out_lat, out_ctx


def expert_choice_ffn(h, router_w, w_gate, w_up, w_down):
    B, T, D = h.shape
    cap = CAPACITY_FACTOR * T // N_EXPERTS
    affinity = jax.nn.softmax((h @ router_w).astype(F32), axis=-1)
    gates, idx = lax.top_k(jnp.swapaxes(affinity, 1, 2), cap)
    xe = jax.vmap(lambda hb, ib: hb[ib])(h, idx)
    hid = jax.nn.silu(jnp.einsum("becd,edf->becf", xe, w_gate)) * jnp.einsum("becd,edf->becf", xe, w_up)
    ye = jnp.einsum("becf,efd->becd", hid, w_down) * gates[..., None].astype(h.dtype)
    return jax.vmap(lambda yb, ib: jnp.zeros((T, D), yb.dtype).at[ib.reshape(-1)].add(yb.reshape(-1, D)))(ye, idx)


def setup_inputs(seed: int = 0) -> dict:
    key = jax.random.key(seed)
    ks = iter(jax.random.split(key, 32))
    L, D, W = DEPTH, D_MODEL, RWKV_WIDTH

    def nrm(shape, scale):
        return jax.random.normal(next(ks), shape, F32) * scale

    shift_base = jnp.array([0.25, 0.5, 0.25], F32)[None, :, None]
    return {
        "x": nrm((BATCH, SEQ, D), 1.0),
        "c": nrm((BATCH, D), 1.0),
        "ctx": nrm((BATCH, CTX_LEN, D), 1.0),
        "c_ctx": nrm((D,), 1.0),
        "ada_w": nrm((L, D, N_MOD * D), 0.5 * D ** -0.5),
        "ada_b": nrm((L, N_MOD * D), 0.01),
        "norm_mix_g": 1.0 + nrm((L, D), 0.02),
        "norm_ffn_g": 1.0 + nrm((L, D), 0.02),
        "w_in": nrm((L, D, IN_COLS), D ** -0.5),
        "shift_w": shift_base + nrm((L, SHIFT_TAPS, RWKV_COLS), 0.05),
        "decay_w0": jax.random.uniform(next(ks), (L, 2, W), F32, -6.0, 0.5),
        "decay_up": nrm((L, 2, DECAY_LORA, W), 0.1),
        "iclr_a0": nrm((L, 2, W), 0.1),
        "iclr_up": nrm((L, 2, ICLR_LORA, W), 0.1),
        "gate_up": nrm((L, GATE_LORA, W), GATE_LORA ** -0.5),
        "k_k": 0.85 + nrm((L, W), 0.05),
        "k_a": 1.0 + nrm((L, W), 0.05),
        "r_k": nrm((L, RWKV_HEADS, RWKV_HEAD), 0.1),
        "lnx_g": 1.0 + nrm((L, W), 0.02),
        "lnx_b": nrm((L, W), 0.01),
        "pool_w": nrm((L, POOL_GROUPS, POOL_GC, POOL_GC), POOL_GC ** -0.5),
        "pool_scale": 1.0 + nrm((L, POOL_WIDTH), 0.1),
        "fourier_w": nrm((L, FOURIER_HEADS, FOURIER_HC, FOURIER_HC), FOURIER_HC ** -0.5),
        "w_out": nrm((L, D_MIX, D), D_MIX ** -0.5),
        "router_w": nrm((L, D, N_EXPERTS), D ** -0.5),
        "exp_w_gate": nrm((L, N_EXPERTS, D, EXPERT_FF), D ** -0.5),
        "exp_w_up": nrm((L, N_EXPERTS, D, EXPERT_FF), D ** -0.5),
        "exp_w_down": nrm((L, N_EXPERTS, EXPERT_FF, D), EXPERT_FF ** -0.5),
        "final_norm_g": 1.0 + nrm((D,), 0.02),
    }


def reference(x, c, ctx, c_ctx, ada_w, ada_b, norm_mix_g, norm_ffn_g, w_in, shift_w, decay_w0, decay_up, iclr_a0,
              iclr_up, gate_up, k_k, k_a, r_k, lnx_g, lnx_b, pool_w, pool_scale, fourier_w, w_out, router_w,
              exp_w_gate, exp_w_up, exp_w_down, final_norm_g):
    rows = x.shape[1] // GRID_W
    h_lat, h_ctx = x, ctx
    for l in range(DEPTH):
        ctx_out = l < DEPTH - 1
        sh1_l, sc1_l, g1_l, sh2_l, sc2_l, g2_l = [m[:, None, :] for m in adaln(c, ada_w[l], ada_b[l])]
        sh1_c, sc1_c, g1_c, sh2_c, sc2_c, g2_c = adaln(c_ctx, ada_w[l], ada_b[l])
        mix_lat, mix_ctx = token_mixing(
            modulate(rmsnorm(h_lat, norm_mix_g[l]), sh1_l, sc1_l),
            modulate(rmsnorm(h_ctx, norm_mix_g[l]), sh1_c, sc1_c),
            rows, ctx_out, w_in[l], shift_w[l], decay_w0[l], decay_up[l], iclr_a0[l], iclr_up[l], gate_up[l],
            k_k[l], k_a[l], r_k[l], lnx_g[l], lnx_b[l], pool_w[l], pool_scale[l], fourier_w[l], w_out[l])
        h_lat = h_lat + g1_l * mix_lat
        h_lat = h_lat + g2_l * expert_choice_ffn(modulate(rmsnorm(h_lat, norm_ffn_g[l]), sh2_l, sc2_l),
                                                 router_w[l], exp_w_gate[l], exp_w_up[l], exp_w_down[l])
        if ctx_out:
            h_ctx = h_ctx + g1_c * mix_ctx
            h_ctx = h_ctx + g2_c * expert_choice_ffn(modulate(rmsnorm(h_ctx, norm_ffn_g[l]), sh2_c, sc2_c),
                                                     router_w[l], exp_w_gate[l], exp_w_up[l], exp_w_down[l])
    return rmsnorm(h_lat, final_norm_g)
```

```python
import numpy as np
from contextlib import ExitStack
import concourse.bass as bass
import concourse.mybir as mybir
from concourse.bass_utils import run_bass_kernel_spmd

F32 = mybir.dt.float32
BF16 = mybir.dt.bfloat16
I32 = mybir.dt.int32
AF = mybir.ActivationFunctionType
ALU = mybir.AluOpType
AX = mybir.AxisListType
ENGS = ("pe", "act", "dve", "pool", "sp")

D = 1024
T_LAT = 16384
T_CTX = 256
TT = T_LAT + T_CTX
NU = TT // 128
DEPTH = 2
CDEC = float(np.exp(-0.5))


class Buf:
    __slots__ = ("w", "r")

    def __init__(self):
        self.w = {}
        self.r = {}


class MBuf:
    def __init__(self):
        self.d = {}

    def __getitem__(self, k):
        b = self.d.get(k)
        if b is None:
            b = self.d[k] = Buf()
        return b


class RegRef:
    def __init__(self, val):
        self.val = val


class _Rec:
    def __getattr__(self, name):
        def f(*a, **k):
            self.call = (name, a, k)
            return self
        return f


class Sched:
    def __init__(self, nc, stack, n_dma_sems=(14, 4, 14)):
        self.nc = nc
        self.stack = stack
        self.ops = {e: [] for e in ENGS}
        self.sems = []
        self.cnt = []
        self.known = {e: {} for e in ENGS}
        self.esem = {}
        for e in ENGS:
            self.esem[e] = self._newsem("prog_" + e)
        self.dpool = {}
        self.dnext = {}
        for q, n in zip(("sp", "act", "pool"), n_dma_sems):
            self.dpool[q] = [self._newsem(f"dma_{q}_{i}") for i in range(n)]
            self.dnext[q] = 0
        self.n_instr = 0

    def _newsem(self, name):
        s = self.stack.enter_context(self.nc.semaphore(name))
        self.sems.append(s)
        self.cnt.append(0)
        return len(self.sems) - 1

    def barrier(self):
        for E in ENGS:
            kn = self.known[E]
            for k, c in enumerate(self.cnt):
                if E == "pe" and k == self.esem["pe"]:
                    continue
                if c > 0 and kn.get(k, 0) < c:
                    self.ops[E].append((1, self.sems[k], c))
                    kn[k] = c

    def scope(self):
        S = self

        class _Sc:
            def __enter__(s2):
                s2.old = S.stack
                s2.st = ExitStack()
                s2.st.__enter__()
                S.stack = s2.st
                return s2

            def __exit__(s2, *a):
                S.barrier()
                S.stack = s2.old
                return s2.st.__exit__(*a)
        return _Sc()

    def sb(self, name, shape, dtype):
        self.uid = getattr(self, "uid", 0) + 1
        return self.stack.enter_context(self.nc.sbuf_tensor(f"s{self.uid}_" + name, list(shape), dtype))

    def ps(self, name, shape, dtype):
        return self.stack.enter_context(self.nc.psum_tensor("p_" + name, list(shape), dtype))

    def _emit(self, E, fn, reads, writes, dma):
        deps = {}
        for b in reads:
            for k, v in b.w.items():
                if deps.get(k, 0) < v:
                    deps[k] = v
        for b in writes:
            for k, v in b.w.items():
                if deps.get(k, 0) < v:
                    deps[k] = v
            for k, v in b.r.items():
                if deps.get(k, 0) < v:
                    deps[k] = v
        if E == "pe":
            deps.pop(self.esem["pe"], None)
        if dma:
            pool = self.dpool[E]
            dk = pool[self.dnext[E] % len(pool)]
            self.dnext[E] += 1
            if self.cnt[dk] > 0 and deps.get(dk, 0) < self.cnt[dk]:
                deps[dk] = self.cnt[dk]
        rec = _Rec()
        fn(rec)
        fn = rec.call
        kn = self.known[E]
        ops = self.ops[E]
        for k, v in deps.items():
            if kn.get(k, 0) < v:
                kn[k] = v
                ops.append((1, self.sems[k], v))
                self.n_instr += 1
        if dma:
            self.cnt[dk] += 16
            ev = (dk, self.cnt[dk])
            ops.append((0, fn, self.sems[dk], 16))
        else:
            k = self.esem[E]
            self.cnt[k] += 1
            ev = (k, self.cnt[k])
            ops.append((0, fn, self.sems[k], 1))
        self.n_instr += 1
        for b in writes:
            b.w = {ev[0]: ev[1]}
            b.r = {}
        for b in reads:
            if b.r.get(ev[0], 0) < ev[1]:
                b.r[ev[0]] = ev[1]
        return ev

    def op(self, E, fn, reads=(), writes=()):
        return self._emit(E, fn, reads, writes, False)

    def dma(self, E, fn, reads=(), writes=()):
        return self._emit(E, fn, reads, writes, True)

    def finish(self):
        ops = self.ops["sp"]
        kn = self.known["sp"]
        for k, c in enumerate(self.cnt):
            if c > 0 and kn.get(k, 0) < c:
                ops.append((1, self.sems[k], c))
                kn[k] = c

    def run_block(self):
        with self.nc.Block() as block:
            def mk(E):
                def body(eng):
                    regs = {}
                    for o in self.ops[E]:
                        if o[0] == 1:
                            eng.wait_ge(o[1], o[2])
                        else:
                            nm, a, k = o[1]
                            if "bounds_check" in k and isinstance(k["bounds_check"], RegRef):
                                v_ = k["bounds_check"].val
                                if v_ not in regs:
                                    regs[v_] = eng.to_reg(v_)
                                k = dict(k)
                                k["bounds_check"] = regs[v_]
                            try:
                                ins = getattr(eng, nm)(*a, **k)
                            except Exception:
                                print("FAILED OP", E, nm, [str(x)[:200] for x in a], {kk: str(vv)[:300] for kk, vv in k.items()})
                                raise
                            ins.then_inc(o[2], o[3])
                return body

            block.tensor(mk("pe"))
            block.scalar(mk("act"))
            block.vector(mk("dve"))
            block.gpsimd(mk("pool"))
            block.sync(mk("sp"))


class Ring:
    def __init__(self, S, name, shape, dtype, n):
        self.t = [S.sb(f"{name}{i}", shape, dtype) for i in range(n)]
        self.b = [Buf() for _ in range(n)]
        self.i = 0

    def next(self):
        i = self.i % len(self.t)
        self.i += 1
        return self.t[i], self.b[i]


def host_consts():
    c = {}
    i = np.arange(128)
    c["ident"] = np.eye(128, dtype=np.float32)
    mu = (i[:, None] < i[None, :]).astype(np.float32)
    c["masks"] = np.stack([mu, mu.T, mu + np.eye(128, dtype=np.float32), mu.T + np.eye(128, dtype=np.float32)], 1).astype(np.float32)
    c["tri"] = np.stack([mu + np.eye(128), mu, mu.T + np.eye(128), mu.T], 1).astype(np.float32)
    def bd(n):
        g = i // n
        return (g[:, None] == g[None, :]).astype(np.float32)
    ml = mu.T
    hm = [ml * bd(16), ml * (bd(32) - bd(16)), ml * (bd(64) - bd(32)), ml * (1 - bd(64))]
    c["hmask"] = np.stack(hm + [m_.T for m_ in hm], 1).astype(np.float32)
    bo = np.zeros((128, 128), np.float32)
    bo[:64, :64] = 1
    bo[64:, 64:] = 1
    c["blockones"] = bo
    ind = np.zeros((128, 4, 8), np.float32)
    for cc in range(4):
        ind[:64, cc, 2 * cc] = 1
        ind[64:, cc, 2 * cc + 1] = 1
    c["ind8"] = ind
    wins = (2, 4, 8, 16)
    def invc(n, w):
        pos = np.arange(n)
        return 1.0 / (np.minimum(pos + w // 2, n) - np.maximum(pos - w // 2, 0)).astype(np.float64)
    c["ic_lat"] = np.stack([np.outer(invc(256, w), invc(64, w)) for w in wins], 0).astype(np.float32)
    c["ic_ctx"] = np.stack([invc(256, w)[None, :] for w in wins], 0).astype(np.float32)
    t = np.arange(128)
    ang = 2 * np.pi * np.outer(t, t) / 128.0
    c["dft128"] = np.stack([np.cos(ang), np.sin(ang), -np.sin(ang)], 1).astype(np.float32)
    angt = 2 * np.pi * np.outer(t, t) / 16384.0
    c["twid"] = np.stack([np.cos(angt), np.sin(angt)], 1).astype(np.float32)
    t2 = np.arange(256)
    a256 = 2 * np.pi * np.outer(t2, t2) / 256.0
    c["dft256"] = np.stack([np.cos(a256).reshape(2, 128, 256), np.sin(a256).reshape(2, 128, 256)], 2).transpose(1, 0, 2, 3).astype(np.float32).copy()
    q = np.arange(64)
    a64 = 2 * np.pi * np.outer(q, q) / 64.0
    pad = np.zeros((64, 2, 2, 128), np.float64)
    for ci, m_ in enumerate((np.cos(a64), np.sin(a64))):
        pad[:, ci, 0, 0:64] = m_ / 1024.0
        pad[:, ci, 1, 64:128] = m_ / 1024.0
    c["dft64pad"] = pad.astype(np.float32)
    return c


def build(debug=()):
    nc = bass.Bass("TRN2", target_bir_lowering=False)
    dbg = set(debug)

    def dram_in(name, shape, dt=F32):
        return nc.dram_tensor(name, list(shape), dt, kind="ExternalInput").ap()

    def dram(name, shape, dt=F32):
        kind = "ExternalOutput" if name in dbg else ("ExternalInput" if ("in:" + name) in dbg else "Internal")
        return nc.dram_tensor(name, list(shape), dt, kind=kind).ap()

    x = dram_in("x", [T_LAT, D])
    ctx = dram_in("ctx", [T_CTX, D])
    ccond = dram_in("ccond", [128, 16])
    ada_w = dram_in("ada_w", [DEPTH, D, 6 * D])
    ada_b = dram_in("ada_b", [DEPTH, 6 * D])
    norm_mix_g = dram_in("norm_mix_g", [DEPTH, D])
    norm_ffn_g = dram_in("norm_ffn_g", [DEPTH, D])
    w_in = dram_in("w_in", [DEPTH, D, 2304])
    pp = dram_in("pp", [DEPTH, 128, 64])
    decay_w0 = dram_in("decay_w0", [DEPTH, 2, 512])
    decay_up = dram_in("decay_up", [DEPTH, 2, 64, 512])
    iclr_up = dram_in("iclr_up", [DEPTH, 2, 64, 512])
    gate_up = dram_in("gate_up", [DEPTH, 128, 512])
    lnx_g = dram_in("lnx_g", [DEPTH, 512])
    lnx_b = dram_in("lnx_b", [DEPTH, 512])
    c_ident = dram_in("ident", [128, 128])
    c_masks = dram_in("masks", [128, 4, 128])
    c_tri = dram_in("tri", [128, 4, 128])
    c_bo = dram_in("blockones", [128, 128])
    c_hmask = dram_in("hmask", [128, 8, 128])
    c_ind8 = dram_in("ind8", [128, 4, 8])

    pool_w = dram_in("pool_w", [DEPTH, 4, 64, 64])
    psc = dram_in("psc", [DEPTH, 64, 4])
    fourier_w = dram_in("fourier_w", [DEPTH, 4, 64, 64])
    w_out = dram_in("w_out", [DEPTH, D, D])
    router_w = dram_in("router_w", [DEPTH, D, 16])
    exp_w_gate = dram_in("exp_w_gate", [DEPTH, 16, D, D])
    exp_w_up = dram_in("exp_w_up", [DEPTH, 16, D, D])
    exp_w_down = dram_in("exp_w_down", [DEPTH, 16, D, D])
    final_norm_g = dram_in("final_norm_g", [1, D])
    c_ic_lat = dram_in("ic_lat", [4, 256, 64])
    c_ic_ctx = dram_in("ic_ctx", [4, 1, 256])
    c_dft128 = dram_in("dft128", [128, 3, 128])
    c_twid = dram_in("twid", [128, 2, 128])
    c_dft256 = dram_in("dft256", [128, 2, 2, 256])
    c_dft64pad = dram_in("dft64pad", [64, 2, 2, 128])

    out = nc.dram_tensor("out", [T_LAT, D], F32, kind="ExternalOutput").ap()
    HW_ = 1048
    H2d = dram("H2d", [TT, HW_])
    XEd = [dram(f"XE{e_}", [2080, HW_]) for e_ in range(16)]
    MOEd = dram("MOEd", [TT, D])
    BIGSLOT = 1.0e6
    FXd = dram("FXd", [2, 256, T_LAT])

    HT = dram("HT", [TT, D])
    MODd = dram("MODd", [2, 6 * D])
    PTd = dram("PTd", [2048, TT])
    PFd = dram("PFd", [TT, 256])
    Yd = [dram(f"Y{d}", [TT, 512]) for d in range(2)]
    VTd = dram("VTd", [TT, 512], BF16)
    GTd = dram("GTd", [TT, 512])
    BONd = [dram(f"BON{d}", [TT, 8]) for d in range(2)]
    CATT = dram("CATT", [1024, TT], BF16)

    with ExitStack() as st:
        S = Sched(nc, st)
        banks = [S.ps(f"bank{i}", [128, 512], F32) for i in range(8)]
        bankb = [Buf() for _ in range(8)]
        pctr = [0]

        def psum():
            i = pctr[0] % 7
            pctr[0] += 1
            return banks[i], bankb[i]

        tog = [0]

        def evE():
            tog[0] ^= 1
            return "dve" if tog[0] else "act"

        def copy_ev(E, out_ap, in_ap, reads, writes):
            if E == "act":
                S.op("act", lambda e: e.copy(out=out_ap, in_=in_ap), reads, writes)
            else:
                S.op(E, lambda e: e.tensor_copy(out=out_ap, in_=in_ap), reads, writes)

        ident = S.sb("ident", [128, 128], F32)
        identb = S.sb("identb", [128, 128], BF16)
        masks = S.sb("masks", [128, 4, 128], F32)
        tri = S.sb("tri", [128, 4, 128], F32)
        blockones = S.sb("blockones", [128, 128], F32)
        ind8 = S.sb("ind8", [128, 4, 8], F32)
        ones_row = S.sb("ones_row", [1, 128], F32)
        hmask = S.sb("hmask", [128, 8, 128], F32)
        CB = Buf()
        for t, src in ((ident, c_ident), (masks, c_masks), (tri, c_tri), (blockones, c_bo), (ind8, c_ind8), (hmask, c_hmask)):
            S.dma("sp", lambda e, t=t, src=src: e.dma_start(out=t[:], in_=src), [], [CB])
        S.op("dve", lambda e: e.tensor_copy(out=identb[:], in_=ident[:]), [CB], [CB])
        S.op("dve", lambda e: e.memset(ones_row[:], 1.0), [], [CB])

        HTb = MBuf()
        S.dma("sp", lambda e: e.dma_start(out=HT[0:T_CTX, :], in_=ctx), [], [HTb[0], HTb[1]])
        for i in range(16):
            S.dma("sp", lambda e, i=i: e.dma_start(out=HT[T_CTX + i * 1024:T_CTX + (i + 1) * 1024, :],
                                                   in_=x[i * 1024:(i + 1) * 1024, :]),
                  [], [HTb[2 + i * 8 + j] for j in range(8)])

        cc_sb = S.sb("cc_sb", [128, 16], F32)
        silu_c = S.sb("silu_c", [128, 16], F32)
        SCB = Buf()
        S.dma("sp", lambda e: e.dma_start(out=cc_sb[:], in_=ccond), [], [SCB])
        S.op("act", lambda e: e.activation(out=silu_c[:], in_=cc_sb[:], func=AF.Silu), [SCB], [SCB])

        MODb = Buf()

        def stage_adaln(l):
            wr = Ring(S, f"adaw{l}_", [128, 8, 512], F32, 2)
            row = S.sb(f"modrow{l}", [2, 6 * D], F32)
            brow = S.sb(f"adab{l}", [2, 6 * D], F32)
            RB = Buf()
            for j in range(2):
                S.dma("sp", lambda e, j=j: e.dma_start(out=brow[j:j + 1, :], in_=ada_b[l:l + 1, :]), [], [RB])
            for n in range(12):
                wt, wb = wr.next()
                S.dma("sp" if n % 2 else "act",
                      lambda e, wt=wt, n=n: e.dma_start(
                          out=wt[:], in_=ada_w[l, :, n * 512:(n + 1) * 512].rearrange("(kc p) n -> p kc n", p=128)),
                      [], [wb])
                pt, pb = psum()
                for kc in range(8):
                    S.op("pe", lambda e, pt=pt, wt=wt, kc=kc: e.matmul(
                        pt[0:2, :], lhsT=silu_c[:, 2 * kc:2 * kc + 2],
                        rhs=wt[:, kc, :], start=(kc == 0), stop=(kc == 7)), [SCB, wb], [pb])
                S.op("dve", lambda e, pt=pt, n=n: e.tensor_tensor(
                    out=row[:, n * 512:(n + 1) * 512], in0=pt[0:2, :], in1=brow[:, n * 512:(n + 1) * 512], op=ALU.add),
                    [pb, RB], [RB])
            S.dma("sp", lambda e: e.dma_start(out=MODd, in_=row[:]), [RB], [MODb])

        def bload(tile_ap, src_row_ap, wbuf, rbufs=()):
            S.dma("sp", lambda e: e.dma_start(out=tile_ap, in_=src_row_ap.to_broadcast([128, src_row_ap.shape[-1]])),
                  list(rbufs), [wbuf])

        PTb = MBuf()
        PFb = MBuf()

        def stage_A(l):
            win = S.sb(f"win", [128, 8, 2304], BF16)
            WB = Buf()
            for kc in range(8):
                S.dma("pool", lambda e, kc=kc: e.dma_start(out=win[:, kc, :], in_=w_in[l, kc * 128:(kc + 1) * 128, :],
                                                            max_dma_last_dim=2048 * 4), [], [WB])
            Gm = [S.sb(f"Gm{j}", [128, D], F32) for j in range(2)]
            SHm = [S.sb(f"SHm{j}", [128, D], F32) for j in range(2)]
            gt = S.sb("gtmp", [128, D], F32)
            GB = Buf()
            bload(gt[:], norm_mix_g[l:l + 1, :], GB)
            for j, rowi in ((0, 1), (1, 0)):
                bload(Gm[j][:], MODd[rowi:rowi + 1, D:2 * D], GB, [MODb])
                bload(SHm[j][:], MODd[rowi:rowi + 1, 0:D], GB, [MODb])
                S.op("dve", lambda e, j=j: e.scalar_tensor_tensor(out=Gm[j][:], in0=Gm[j][:], scalar=1.0, in1=gt[:],
                                                                 op0=ALU.add, op1=ALU.mult), [GB], [GB])
            xr = Ring(S, "xin", [128, D], F32, 2)
            xm = Ring(S, "xmod", [128, D], F32, 2)
            sq = S.sb("sqjunk", [128, D], F32)
            SQB = Buf()
            st_ = Ring(S, "stat", [128, 2], F32, 4)
            xnT = Ring(S, "xnT", [128, 8, 512], BF16, 2)
            stg = Ring(S, "stgA", [128, 512], F32, 4)
            stf = Ring(S, "stgF", [128, 256], F32, 2)
            blocks = [(0, 256)] + [(256 + 512 * i, 512) for i in range(32)]
            for (t0, nt) in blocks:
                j = 0 if t0 < 256 else 1
                xt_, xtb = xnT.next()
                for s in range(nt // 128):
                    u = (t0 // 128) + s
                    xi, xib = xr.next()
                    S.dma("sp", lambda e, xi=xi, u=u: e.dma_start(out=xi[:], in_=HT[u * 128:(u + 1) * 128, :]), [HTb[u]], [xib])
                    sc, scb = st_.next()
                    S.op("act", lambda e, xi=xi, sc=sc: e.activation(out=sq[:], in_=xi[:], func=AF.Square, accum_out=sc[:, 0:1]),
                         [xib], [SQB, scb])
                    S.op("act", lambda e, sc=sc: e.activation(out=sc[:, 1:2], in_=sc[:, 0:1], func=AF.Sqrt, scale=1.0 / D, bias=1e-6),
                         [scb], [scb])
                    S.op("dve", lambda e, sc=sc: e.reciprocal(out=sc[:, 1:2], in_=sc[:, 1:2]), [scb], [scb])
                    xo, xob = xm.next()
                    S.op("dve", lambda e, xo=xo, xi=xi, sc=sc, j=j: e.scalar_tensor_tensor(
                        out=xo[:], in0=xi[:], scalar=sc[:, 1:2], in1=Gm[j][:], op0=ALU.mult, op1=ALU.mult), [xib, scb, GB], [xob])
                    S.op("pool", lambda e, xo=xo, j=j: e.tensor_tensor(out=xo[:], in0=xo[:], in1=SHm[j][:], op=ALU.add), [xob, GB], [xob])
                    for half in range(2):
                        pt, pb = psum()
                        for q in range(4):
                            kc = half * 4 + q
                            S.op("pe", lambda e, pt=pt, xo=xo, kc=kc, q=q: e.transpose(
                                pt[:, q * 128:(q + 1) * 128], xo[:, kc * 128:(kc + 1) * 128], ident[:]), [xob, CB], [pb])
                        copy_ev(evE(), xt_[:, half * 4:(half + 1) * 4, s * 128:(s + 1) * 128],
                                pt[:, :].rearrange("p (q t) -> p q t", q=4), [pb], [xtb])
                for cchunk in range(16):
                    pt, pb = psum()
                    for kc in range(8):
                        S.op("pe", lambda e, pt=pt, kc=kc, cchunk=cchunk, xt_=xt_, nt=nt: e.matmul(
                            pt[:, 0:nt], lhsT=win[:, kc, cchunk * 128:(cchunk + 1) * 128], rhs=xt_[:, kc, 0:nt],
                            start=(kc == 0), stop=(kc == 7)), [WB, xtb], [pb])
                    sg, sgb = stg.next()
                    copy_ev(evE(), sg[:, 0:nt], pt[:, 0:nt], [pb], [sgb])
                    S.dma("sp", lambda e, sg=sg, cchunk=cchunk, t0=t0, nt=nt: e.dma_start(
                        out=PTd[cchunk * 128:(cchunk + 1) * 128, t0:t0 + nt], in_=sg[:, 0:nt]),
                        [sgb], [PTb[(cchunk, t0 // 512 if t0 >= 256 else -1)]])
                for s in range(nt // 128):
                    u = (t0 // 128) + s
                    pt, pb = psum()
                    for kc in range(8):
                        S.op("pe", lambda e, pt=pt, kc=kc, xt_=xt_, s=s: e.matmul(
                            pt[:, 0:256], lhsT=xt_[:, kc, s * 128:(s + 1) * 128], rhs=win[:, kc, 2048:2304],
                            start=(kc == 0), stop=(kc == 7)), [WB, xtb], [pb])
                    sf, sfb = stf.next()
                    copy_ev(evE(), sf[:], pt[:, 0:256], [pb], [sfb])
                    S.dma("sp", lambda e, sf=sf, u=u: e.dma_start(out=PFd[u * 128:(u + 1) * 128, :], in_=sf[:]), [sfb], [PFb[u]])


        Yb = [MBuf(), MBuf()]
        VTb = MBuf()
        GTb = MBuf()
        BONb = [MBuf(), MBuf()]
        CATb = MBuf()

        def seq_of(t0):
            return (0, 256) if t0 < 256 else (256, TT)

        def stage_rwkv(l, need_y_ctx=True):
            ppt = S.sb("ppt", [128, 64], F32)
            omka = S.sb("omka", [128, 4], F32)
            wup = [S.sb(f"wup{d}", [128, 512], F32) for d in range(2)]
            w0row = [S.sb(f"w0row{d}", [1, 512], F32) for d in range(2)]
            gateup = S.sb("gateup", [128, 512], F32)
            PB = Buf()
            S.dma("sp", lambda e: e.dma_start(out=ppt[:], in_=pp[l]), [], [PB])
            for d in range(2):
                S.dma("sp", lambda e, d=d: e.dma_start(out=wup[d][0:64, :], in_=decay_up[l, d]), [], [PB])
                S.dma("sp", lambda e, d=d: e.dma_start(out=wup[d][64:128, :], in_=iclr_up[l, d]), [], [PB])
                S.dma("sp", lambda e, d=d: e.dma_start(out=w0row[d][:], in_=decay_w0[l, d:d + 1, :]), [], [PB])
            S.dma("sp", lambda e: e.dma_start(out=gateup[:], in_=gate_up[l]), [], [PB])
            S.op("dve", lambda e: e.tensor_scalar(out=omka[:], in0=ppt[:, 54:58], scalar1=-1.0, scalar2=1.0,
                                                  op0=ALU.mult, op1=ALU.add), [PB], [PB])
            pin = Ring(S, "pin", [128, 514], F32, 2)
            Ut = [S.sb(f"U{c}", [128, 512], F32) for c in range(14)]
            Ub = [Buf() for _ in range(14)]
            th = S.sb("th", [64, 512], F32)
            THB = Buf()
            sigx = S.sb("sigx", [128, 512], F32)
            SXB = Buf()
            sigt = Ring(S, "sigt", [128, 512], F32, 4)
            tmp = Ring(S, "tmpf", [128, 512], F32, 6)
            Gr = Ring(S, "G", [128, 4, 512], F32, 2)
            GLr = Ring(S, "GL", [128, 4, 4], F32, 2)
            negb = Ring(S, "negb", [128, 4], F32, 2)
            Atr = Ring(S, "At", [128, 4, 512], BF16, 1)
            Btr = Ring(S, "Bt", [128, 4, 512], BF16, 1)
            Ktr = Ring(S, "Kt", [128, 4, 512], BF16, 1)
            Rtr = Ring(S, "Rt", [128, 4, 512], BF16, 1)
            BhTr = Ring(S, "BhT", [128, 4, 512], BF16, 1)
            KhTr = Ring(S, "KhT", [128, 4, 512], BF16, 1)
            Vtr = Ring(S, "Vtok", [128, 4, 512], BF16, 1)
            AtM = [S.sb(f"AtM{i}", [128, 4, 512], BF16) for i in range(2)]
            BtM = [S.sb(f"BtM{i}", [128, 4, 512], BF16) for i in range(2)]
            RtM = [S.sb(f"RtM{i}", [128, 4, 512], BF16) for i in range(2)]
            MKB_ = Buf()
            hb = Ring(S, "hb", [128, 512], BF16, 3)
            bonst = Ring(S, "bonst", [128, 32], F32, 2)
            gst = Ring(S, "gst", [128, 512], F32, 1)
            PTr = Ring(S, "PTs", [128, 4, 128], BF16, 3)
            Pr = Ring(S, "Ps", [128, 4, 128], BF16, 3)
            TTr = Ring(S, "TTs", [128, 4, 128], BF16, 3)
            TTall = Ring(S, "TTall", [128, 8, 128], BF16, 2)
            TNr = Ring(S, "TNs", [128, 4, 128], BF16, 3)
            Cr = Ring(S, "Cs", [128, 4, 128], BF16, 6)
            Zr = Ring(S, "Zs", [128, 4, 128], BF16, 3)
            Makr = Ring(S, "Mak", [128, 8, 128], BF16, 2)
            Mrbr = Ring(S, "Mrb", [128, 8, 128], BF16, 1)
            Mrkr = Ring(S, "Mrk", [128, 8, 128], BF16, 1)
            Xsr = Ring(S, "Xs", [128, 512], BF16, 2)
            Usr = Ring(S, "Us", [128, 512], BF16, 2)
            Ysr = Ring(S, "Ys", [128, 512], F32, 1)
            Sf = S.sb("Sf", [128, 4, 128], F32)
            Sbf = S.sb("Sbf", [128, 4, 128], BF16)
            Stmp = S.sb("Stmp", [128, 4, 128], F32)
            STB = Buf()
            SFB = Buf()
            SBB = Buf()

            blocks = [(0, 256)] + [(256 + 512 * i, 512) for i in range(32)]
            if "short" in dbg:
                blocks = blocks[:3]
            if "blocks1" in dbg:
                blocks = blocks[:1]
            for d in range(1 if "d0" in dbg else 2):
                order = blocks if d == 0 else [blocks[0]] + blocks[:0:-1]
                mS, mSt, mI = (0, 1, 2) if d == 0 else (1, 0, 3)
                S.op("dve", lambda e: e.memset(Sf[:], 0.0), [], [SFB])
                S.op("dve", lambda e: e.memset(Sbf[:], 0.0), [], [SBB])
                for (t0, nt) in order:
                    nun = nt // 128
                    s_lo, s_hi = seq_of(t0)
                    chunks = list(range(0, 8)) + [12] + ([8, 9, 10, 11, 13] if d == 0 else [])
                    for c in chunks:
                        pi, pib = pin.next()
                        lo = t0 - 1
                        hi = t0 + nt + 1
                        a = max(lo, s_lo)
                        bnd = min(hi, s_hi)
                        if a > lo:
                            S.op("pool", lambda e, pi=pi: e.memset(pi[:, 0:1], 0.0), [], [pib])
                        if bnd < hi:
                            S.op("pool", lambda e, pi=pi, nt=nt: e.memset(pi[:, nt + 1:nt + 2], 0.0), [], [pib])
                        S.dma("sp", lambda e, pi=pi, c=c, a=a, bnd=bnd, lo=lo: e.dma_start(
                            out=pi[:, a - lo:bnd - lo], in_=PTd[c * 128:(c + 1) * 128, a:bnd]),
                            [PTb[(c, k)] for k in range(-1, 32)], [pib])
                        eng = "dve"
                        S.op(eng, lambda e, pi=pi, c=c, nt=nt: e.tensor_scalar(
                            out=Ut[c][:, 0:nt], in0=pi[:, 1:nt + 1], scalar1=ppt[:, 3 * c + 1:3 * c + 2], scalar2=None, op0=ALU.mult),
                            [pib, PB], [Ub[c]])
                        S.op(eng, lambda e, pi=pi, c=c, nt=nt: e.scalar_tensor_tensor(
                            out=Ut[c][:, 0:nt], in0=pi[:, 0:nt], scalar=ppt[:, 3 * c:3 * c + 1], in1=Ut[c][:, 0:nt],
                            op0=ALU.mult, op1=ALU.add), [pib, PB, Ub[c]], [Ub[c]])
                        S.op(eng, lambda e, pi=pi, c=c, nt=nt: e.scalar_tensor_tensor(
                            out=Ut[c][:, 0:nt], in0=pi[:, 2:nt + 2], scalar=ppt[:, 3 * c + 2:3 * c + 3], in1=Ut[c][:, 0:nt],
                            op0=ALU.mult, op1=ALU.add), [pib, PB, Ub[c]], [Ub[c]])
                    S.op("act", lambda e, nt=nt: e.activation(out=th[:, 0:nt], in_=Ut[12][0:64, 0:nt], func=AF.Tanh), [Ub[12]], [THB])
                    sgs = []
                    for q in range(nun):
                        pt, pb = psum()
                        S.op("pe", lambda e, pt=pt, q=q: e.matmul(pt[:, :], lhsT=th[:, q * 128:(q + 1) * 128], rhs=wup[d][0:64, :],
                                                                   start=True, stop=False), [THB, PB], [pb])
                        S.op("pe", lambda e, pt=pt: e.matmul(pt[:, :], lhsT=ones_row[:, :], rhs=w0row[d][:, :], start=False, stop=True),
                             [CB, PB], [pb])
                        sg, sgb = sigt.next()
                        S.op("act", lambda e, sg=sg, pt=pt: e.activation(out=sg[:], in_=pt[:, :], func=AF.Sigmoid), [pb], [sgb])
                        sgs.append((sg, sgb))
                    G, Gb = Gr.next()
                    GL, GLb = GLr.next()
                    At, Atb = Atr.next()
                    Bt, Btb = Btr.next()
                    Kt, Ktb = Ktr.next()
                    Rt, Rtb = Rtr.next()
                    BhT, BhTb = BhTr.next()
                    KhT, KhTb = KhTr.next()
                    Vt, Vtb = Vtr.next()
                    if d == 0:
                        S.op("act", lambda e, nt=nt: e.activation(out=sigx[:, 0:nt], in_=Ut[13][:, 0:nt], func=AF.Sigmoid), [Ub[13]], [SXB])
                        for q in range(nun):
                            u = t0 // 128 + q
                            pt, pb = psum()
                            S.op("pe", lambda e, pt=pt, q=q: e.matmul(pt[:, :], lhsT=sigx[:, q * 128:(q + 1) * 128], rhs=gateup[:, :],
                                                                       start=True, stop=True), [SXB, PB], [pb])
                            g_, g_b = gst.next()
                            copy_ev(evE(), g_[:], pt[:, :], [pb], [g_b])
                            S.dma("sp", lambda e, g_=g_, u=u: e.dma_start(out=GTd[u * 128:(u + 1) * 128, :], in_=g_[:]), [g_b], [GTb[u]])
                    else:
                        for q in range(nun):
                            u = t0 // 128 + q
                            S.dma("sp", lambda e, q=q, u=u, Vt=Vt: e.dma_start(out=Vt[:, q, :], in_=VTd[u * 128:(u + 1) * 128, :]),
                                  [VTb[u]], [Vtb])
                    bon_p, bon_pb = banks[7], bankb[7]
                    for cc in range(4):
                        ci, cib = psum()
                        ce, ceb = psum()
                        for q in range(nun):
                            sg, sgb = sgs[q]
                            S.op("pe", lambda e, ci=ci, sg=sg, q=q, cc=cc: e.matmul(
                                ci[:, q * 128:(q + 1) * 128], lhsT=sg[:, cc * 128:(cc + 1) * 128], rhs=tri[:, 2 * d, :],
                                start=True, stop=True), [sgb, CB], [cib])
                            S.op("pe", lambda e, ce=ce, sg=sg, q=q, cc=cc: e.matmul(
                                ce[:, q * 128:(q + 1) * 128], lhsT=sg[:, cc * 128:(cc + 1) * 128], rhs=tri[:, 2 * d + 1, :],
                                start=True, stop=True), [sgb, CB], [ceb])
                        nb_, nbb = negb.next()
                        ecol = 127 if d == 0 else 0
                        S.op("dve", lambda e, nb_=nb_, ci=ci, nun=nun, ecol=ecol: e.tensor_scalar(
                            out=nb_[:, 0:nun], in0=ci[:, ecol:ecol + 128 * (nun - 1) + 1:128], scalar1=-CDEC, scalar2=None, op0=ALU.mult),
                            [cib], [nbb])
                        Ginv, Ginvb = tmp.next()
                        Gx, Gxb = tmp.next()
                        Gh, Ghb = tmp.next()
                        S.op("act", lambda e, ci=ci, cc=cc, nt=nt, G=G: e.activation(out=G[:, cc, 0:nt], in_=ci[:, 0:nt], func=AF.Exp, scale=-CDEC), [cib], [Gb])
                        S.op("act", lambda e, ci=ci, nt=nt, Ginv=Ginv: e.activation(out=Ginv[:, 0:nt], in_=ci[:, 0:nt], func=AF.Exp, scale=CDEC), [cib], [Ginvb])
                        S.op("act", lambda e, ce=ce, nt=nt, Gx=Gx: e.activation(out=Gx[:, 0:nt], in_=ce[:, 0:nt], func=AF.Exp, scale=-CDEC), [ceb], [Gxb])
                        for q in range(nun):
                            S.op("act", lambda e, ci=ci, q=q, Gh=Gh, nb_=nb_: e.activation(
                                out=Gh[:, q * 128:(q + 1) * 128], in_=ci[:, q * 128:(q + 1) * 128], func=AF.Exp, scale=CDEC,
                                bias=nb_[:, q:q + 1]), [cib, nbb], [Ghb])
                        S.op("pool", lambda e, GL=GL, G=G, cc=cc, nun=nun, ecol=ecol: e.tensor_copy(
                            out=GL[:, cc, 0:nun], in_=G[:, cc, ecol:ecol + 128 * (nun - 1) + 1:128]), [Gb], [GLb])
                        pa, pab = psum()
                        S.op("pe", lambda e, pa=pa, cc=cc, nt=nt: e.matmul(pa[:, 0:nt], lhsT=wup[d][64:128, cc * 128:(cc + 1) * 128],
                                                                          rhs=Ut[12][64:128, 0:nt], start=True, stop=True), [PB, Ub[12]], [pab])
                        alr, alrb = tmp.next()
                        S.op("act", lambda e, alr=alr, pa=pa, nt=nt, cc=cc: e.activation(
                            out=alr[:, 0:nt], in_=pa[:, 0:nt], func=AF.Sigmoid, bias=ppt[:, 42 + 4 * d + cc:43 + 4 * d + cc]), [pab, PB], [alrb])
                        kx, kxb = tmp.next()
                        sq_, sqb = tmp.next()
                        S.op("dve", lambda e, kx=kx, cc=cc, nt=nt: e.tensor_scalar(
                            out=kx[:, 0:nt], in0=Ut[4 + cc][:, 0:nt], scalar1=ppt[:, 50 + cc:51 + cc], scalar2=None, op0=ALU.mult), [Ub[4 + cc], PB], [kxb])
                        S.op("pool", lambda e, kx=kx, sq_=sq_, nt=nt: e.tensor_tensor(out=sq_[:, 0:nt], in0=kx[:, 0:nt], in1=kx[:, 0:nt], op=ALU.mult), [kxb], [sqb])
                        pss, pssb = psum()
                        S.op("pe", lambda e, pss=pss, sq_=sq_, nt=nt: e.matmul(pss[:, 0:nt], lhsT=blockones[:, :], rhs=sq_[:, 0:nt], start=True, stop=True),
                             [CB, sqb], [pssb])
                        S.op("act", lambda e, sq_=sq_, pss=pss, nt=nt: e.activation(out=sq_[:, 0:nt], in_=pss[:, 0:nt], func=AF.Sqrt, bias=1e-12), [pssb], [sqb])
                        S.op("dve", lambda e, sq_=sq_, nt=nt: e.reciprocal(out=sq_[:, 0:nt], in_=sq_[:, 0:nt]), [sqb], [sqb])
                        S.op("dve", lambda e, kx=kx, sq_=sq_, nt=nt: e.tensor_tensor(out=kx[:, 0:nt], in0=kx[:, 0:nt], in1=sq_[:, 0:nt], op=ALU.mult), [kxb, sqb], [kxb])
                        S.op("dve", lambda e, At=At, kx=kx, Gx=Gx, cc=cc, nt=nt: e.scalar_tensor_tensor(
                            out=At[:, cc, 0:nt], in0=kx[:, 0:nt], scalar=-1.0, in1=Gx[:, 0:nt], op0=ALU.mult, op1=ALU.mult), [kxb, Gxb], [Atb])
                        S.op("pool", lambda e, sq_=sq_, kx=kx, alr=alr, nt=nt: e.tensor_tensor(out=sq_[:, 0:nt], in0=kx[:, 0:nt], in1=alr[:, 0:nt], op=ALU.mult),
                             [kxb, alrb], [sqb])
                        S.op("dve", lambda e, Bt=Bt, sq_=sq_, Ginv=Ginv, cc=cc, nt=nt: e.tensor_tensor(
                            out=Bt[:, cc, 0:nt], in0=sq_[:, 0:nt], in1=Ginv[:, 0:nt], op=ALU.mult), [sqb, Ginvb], [Btb])
                        bh, bhb = hb.next()
                        S.op("pool", lambda e, bh=bh, sq_=sq_, Gh=Gh, nt=nt: e.tensor_tensor(out=bh[:, 0:nt], in0=sq_[:, 0:nt], in1=Gh[:, 0:nt], op=ALU.mult),
                             [sqb, Ghb], [bhb])
                        S.op("dve", lambda e, alr=alr, cc=cc, nt=nt: e.tensor_scalar(
                            out=alr[:, 0:nt], in0=alr[:, 0:nt], scalar1=ppt[:, 54 + cc:55 + cc], scalar2=omka[:, cc:cc + 1], op0=ALU.mult, op1=ALU.add),
                            [alrb, PB], [alrb])
                        S.op("dve", lambda e, alr=alr, cc=cc, nt=nt: e.tensor_tensor(out=alr[:, 0:nt], in0=alr[:, 0:nt], in1=Ut[4 + cc][:, 0:nt], op=ALU.mult),
                             [alrb, Ub[4 + cc]], [alrb])
                        S.op("dve", lambda e, Kt=Kt, alr=alr, Ginv=Ginv, cc=cc, nt=nt: e.tensor_tensor(
                            out=Kt[:, cc, 0:nt], in0=alr[:, 0:nt], in1=Ginv[:, 0:nt], op=ALU.mult), [alrb, Ginvb], [Ktb])
                        kh, khb = hb.next()
                        S.op("pool", lambda e, kh=kh, alr=alr, Gh=Gh, nt=nt: e.tensor_tensor(out=kh[:, 0:nt], in0=alr[:, 0:nt], in1=Gh[:, 0:nt], op=ALU.mult),
                             [alrb, Ghb], [khb])
                        S.op("pool", lambda e, Rt=Rt, G=G, cc=cc, nt=nt: e.tensor_tensor(out=Rt[:, cc, 0:nt], in0=Ut[cc][:, 0:nt], in1=G[:, cc, 0:nt], op=ALU.mult),
                             [Ub[cc], Gb], [Rtb])
                        for hp_ in range(2):
                            pmc = blockones[:, 64 * hp_:64 * hp_ + 1]
                            for (src_, srcb_, dst_) in ((At, Atb, AtM), (Bt, Btb, BtM), (Rt, Rtb, RtM)):
                                S.op("dve", lambda e: e.tensor_scalar(out=dst_[hp_][:, cc, 0:nt], in0=src_[:, cc, 0:nt], scalar1=pmc, scalar2=None, op0=ALU.mult),
                                     [srcb_, CB], [MKB_])
                        S.op("dve", lambda e, kx=kx, alr=alr, cc=cc, nt=nt: e.scalar_tensor_tensor(
                            out=kx[:, 0:nt], in0=Ut[cc][:, 0:nt], scalar=ppt[:, 58 + cc:59 + cc], in1=alr[:, 0:nt], op0=ALU.mult, op1=ALU.mult),
                            [Ub[cc], alrb, PB], [kxb])
                        for q in range(nun):
                            S.op("pe", lambda e, bon_p=bon_p, kx=kx, q=q, cc=cc: e.matmul(
                                bon_p[:, q * 8 + 2 * cc:q * 8 + 2 * cc + 2], lhsT=kx[:, q * 128:(q + 1) * 128], rhs=ind8[:, 0, 0:2], start=True, stop=True),
                                [kxb, CB], [bon_pb])
                        srcs = [(bh, bhb, BhT, BhTb), (kh, khb, KhT, KhTb)]
                        if d == 0:
                            vh, vhb = hb.next()
                            S.op("act", lambda e, vh=vh, cc=cc, nt=nt: e.copy(out=vh[:, 0:nt], in_=Ut[8 + cc][:, 0:nt]), [Ub[8 + cc]], [vhb])
                            srcs.append((vh, vhb, Vt, Vtb))
                        for (src, srcb, dst, dstb) in srcs:
                            ptb_, ptbb = psum()
                            ptv = ptb_[:, :].bitcast(BF16)
                            for q in range(nun):
                                S.op("pe", lambda e, ptv=ptv, src=src, q=q: e.transpose(
                                    ptv[:, q * 128:(q + 1) * 128], src[:, q * 128:(q + 1) * 128], identb[:]), [srcb, CB], [ptbb])
                            copy_ev(evE(), dst[:, 0:nun, cc * 128:(cc + 1) * 128], ptv[:, 0:nun * 128].rearrange("p (q t) -> p q t", q=nun), [ptbb], [dstb])
                    bs, bsb = bonst.next()
                    copy_ev("dve", bs[:, 0:8 * nun], bon_p[:, 0:8 * nun], [bon_pb], [bsb])
                    for q in range(nun):
                        u = t0 // 128 + q
                        S.dma("sp", lambda e, bs=bs, q=q, u=u: e.dma_start(out=BONd[d][u * 128:(u + 1) * 128, :], in_=bs[:, q * 8:(q + 1) * 8]),
                              [bsb], [BONb[d][u]])
                        if d == 0:
                            S.dma("sp", lambda e, q=q, u=u, Vt=Vt: e.dma_start(out=VTd[u * 128:(u + 1) * 128, :], in_=Vt[:, q, :]), [Vtb], [VTb[u]])
                    qs = range(nun) if d == 0 else range(nun - 1, -1, -1)
                    if "prep_only" in dbg:
                        qs = []
                    for q in qs:
                        u = t0 // 128 + q
                        tk = slice(q * 128, (q + 1) * 128)
                        TTa, TTab = TTall.next()
                        Mak, Makb = Makr.next()
                        Mrb, Mrbb = Mrbr.next()
                        Mrk, Mrkb = Mrkr.next()
                        for grp in range(2):
                            hl = [(2 * grp + j // 2, j % 2) for j in range(4)]
                            pA, pAb = psum()
                            pB, pBb = psum()
                            for j, (cc, hp) in enumerate(hl):
                                ps_ = slice(hp * 64, hp * 64 + 64)
                                S.op("pe", lambda e: e.matmul(
                                    pA[:, j * 128:(j + 1) * 128], lhsT=Bt[:, cc, tk], rhs=AtM[hp][:, cc, tk], start=True, stop=True), [Btb, MKB_], [pAb])
                                S.op("pe", lambda e: e.matmul(
                                    pB[:, j * 128:(j + 1) * 128], lhsT=At[:, cc, tk], rhs=BtM[hp][:, cc, tk], start=True, stop=True), [Atb, MKB_], [pBb])
                            v4 = lambda ap: ap.rearrange("p (j t) -> p j t", j=4)
                            oN, oT = (0, 4) if d == 0 else (4, 0)
                            mk4 = lambda i_: hmask[:, i_:i_ + 1, :].to_broadcast([128, 4, 128])
                            P_, Pb_ = Pr.next()
                            PT_, PTb_ = PTr.next()
                            Cs = []
                            S.op("dve", lambda e: e.tensor_tensor(out=P_[:], in0=v4(pB[:, :]), in1=mk4(oN), op=ALU.mult), [pBb, CB], [Pb_])
                            S.op("dve", lambda e: e.tensor_tensor(out=PT_[:], in0=v4(pA[:, :]), in1=mk4(oT), op=ALU.mult), [pAb, CB], [PTb_])
                            for lvl in range(1, 4):
                                C_, Cb_ = Cr.next()
                                S.op("dve", lambda e: e.tensor_tensor(out=C_[:], in0=v4(pB[:, :]), in1=mk4(oN + lvl), op=ALU.mult), [pBb, CB], [Cb_])
                                if lvl < 3:
                                    CT_, CTb_ = Cr.next()
                                    S.op("dve", lambda e: e.tensor_tensor(out=CT_[:], in0=v4(pA[:, :]), in1=mk4(oT + lvl), op=ALU.mult), [pAb, CB], [CTb_])
                                else:
                                    CT_, CTb_ = None, None
                                Cs.append((C_, Cb_, CT_, CTb_))
                            Tn, Tnb = TNr.next()
                            T_, Tb_ = TTr.next()
                            idb = identb[:, None, :].to_broadcast([128, 4, 128])
                            S.op("dve", lambda e: e.tensor_tensor(out=Tn[:], in0=P_[:], in1=idb, op=ALU.add), [Pb_, CB], [Tnb])
                            S.op("dve", lambda e: e.tensor_tensor(out=T_[:], in0=PT_[:], in1=idb, op=ALU.add), [PTb_, CB], [Tb_])
                            for (Mt, Mtb, lh, lhb, rh, rhb, mk_) in ((Mak, Makb, Kt, Ktb, AtM, MKB_, mS), (Mrb, Mrbb, Bt, Btb, RtM, MKB_, mI), (Mrk, Mrkb, Kt, Ktb, RtM, MKB_, mI)):
                                pM, pMb = psum()
                                for j, (cc, hp) in enumerate(hl):
                                    S.op("pe", lambda e: e.matmul(
                                        pM[:, j * 128:(j + 1) * 128], lhsT=lh[:, cc, tk], rhs=rh[hp][:, cc, tk], start=True, stop=True), [lhb, rhb], [pMb])
                                S.op("dve", lambda e, Mt=Mt, pM=pM, mk_=mk_, grp=grp: e.tensor_tensor(
                                    out=Mt[:, 4 * grp:4 * grp + 4, :], in0=v4(pM[:, :]), in1=masks[:, mk_:mk_ + 1, :].to_broadcast([128, 4, 128]), op=ALU.mult),
                                    [pMb, CB], [Mtb])
                            def mm4(lhs, lhsb, rhs, rhsb):
                                pt_, ptb_ = psum()
                                for j in range(4):
                                    S.op("pe", lambda e: e.matmul(pt_[:, j * 128:(j + 1) * 128], lhsT=lhs[:, j, :], rhs=rhs[:, j, :], start=True, stop=True),
                                         [lhsb, rhsb], [ptb_])
                                return pt_, ptb_
                            for i in range(3):
                                pn, pnb = mm4(PT_, PTb_, P_, Pb_)
                                pnt, pntb = mm4(P_, Pb_, PT_, PTb_)
                                P2, P2b = Pr.next()
                                PT2, PT2b = PTr.next()
                                copy_ev("act", P2[:], v4(pn[:, :]), [pnb], [P2b])
                                copy_ev("act", PT2[:], v4(pnt[:, :]), [pntb], [PT2b])
                                P_, Pb_, PT_, PTb_ = P2, P2b, PT2, PT2b
                                pd, pdb = mm4(PT_, PTb_, Tn, Tnb)
                                pdt, pdtb = mm4(P_, Pb_, T_, Tb_)
                                Tn2, Tn2b = TNr.next()
                                T2, T2b = TTr.next()
                                S.op("dve", lambda e: e.tensor_tensor(out=Tn2[:], in0=v4(pd[:, :]), in1=Tn[:], op=ALU.add), [pdb, Tnb], [Tn2b])
                                S.op("dve", lambda e: e.tensor_tensor(out=T2[:], in0=v4(pdt[:, :]), in1=T_[:], op=ALU.add), [pdtb, Tb_], [T2b])
                                Tn, Tnb, T_, Tb_ = Tn2, Tn2b, T2, T2b
                            for lvl in range(3):
                                C_, Cb_, CT_, CTb_ = Cs[lvl]
                                pzt, pztb = mm4(C_, Cb_, T_, Tb_)
                                Zt, Ztb = Zr.next()
                                copy_ev("act", Zt[:], v4(pzt[:, :]), [pztb], [Ztb])
                                if lvl < 2:
                                    pz, pzb = mm4(CT_, CTb_, Tn, Tnb)
                                    Zn, Znb = Zr.next()
                                    copy_ev("act", Zn[:], v4(pz[:, :]), [pzb], [Znb])
                                pwt, pwtb = mm4(Tn, Tnb, Zt, Ztb)
                                if lvl < 2:
                                    pw, pwb = mm4(T_, Tb_, Zn, Znb)
                                    Tn2, Tn2b = TNr.next()
                                    S.op("dve", lambda e: e.tensor_tensor(out=Tn2[:], in0=v4(pw[:, :]), in1=Tn[:], op=ALU.add), [pwb, Tnb], [Tn2b])
                                    T2, T2b = TTr.next()
                                    S.op("dve", lambda e: e.tensor_tensor(out=T2[:], in0=v4(pwt[:, :]), in1=T_[:], op=ALU.add), [pwtb, Tb_], [T2b])
                                    Tn, Tnb, T_, Tb_ = Tn2, Tn2b, T2, T2b
                                else:
                                    S.op("dve", lambda e: e.tensor_tensor(out=TTa[:, 4 * grp:4 * grp + 4, :], in0=v4(pwt[:, :]), in1=T_[:], op=ALU.add),
                                         [pwtb, Tb_], [TTab])
                        if "inv_only" in dbg:
                            continue
                        pX, pXb = psum()
                        for h in range(8):
                            cc, hp = h // 2, h % 2
                            ps_ = slice(hp * 64, hp * 64 + 64)
                            S.op("pe", lambda e, pX=pX, h=h, Mak=Mak, Vt=Vt, q=q: e.matmul(
                                pX[:, h * 64:(h + 1) * 64], lhsT=Mak[:, h, :], rhs=Vt[:, q, h * 64:(h + 1) * 64], start=True, stop=False), [Makb, Vtb], [pXb])
                            S.op("pe", lambda e, pX=pX, h=h, cc=cc, ps_=ps_: e.matmul(
                                pX[:, h * 64:(h + 1) * 64], lhsT=At[:, cc, tk], rhs=Sbf[:, cc, hp * 64:(hp + 1) * 64], start=False, stop=True), [Atb, SBB], [pXb])
                        Xs, Xsb = Xsr.next()
                        copy_ev("act", Xs[:], pX[:, :], [pXb], [Xsb])
                        if "seq1" in dbg:
                            continue
                        pU, pUb = psum()
                        for h in range(8):
                            S.op("pe", lambda e, pU=pU, h=h, TTa=TTa, Xs=Xs: e.matmul(
                                pU[:, h * 64:(h + 1) * 64], lhsT=TTa[:, h, :], rhs=Xs[:, h * 64:(h + 1) * 64], start=True, stop=True), [TTab, Xsb], [pUb])
                        Us, Usb = Usr.next()
                        copy_ev("dve", Us[:], pU[:, :], [pUb], [Usb])
                        if "seq2" in dbg:
                            continue
                        if need_y_ctx or u >= 2:
                            pY, pYb = psum()
                            for h in range(8):
                                cc, hp = h // 2, h % 2
                                ps_ = slice(hp * 64, hp * 64 + 64)
                                S.op("pe", lambda e, pY=pY, h=h, cc=cc, ps_=ps_: e.matmul(
                                    pY[:, h * 64:(h + 1) * 64], lhsT=Rt[:, cc, tk], rhs=Sbf[:, cc, hp * 64:(hp + 1) * 64], start=True, stop=False), [Rtb, SBB], [pYb])
                                S.op("pe", lambda e, pY=pY, h=h, Mrb=Mrb, Us=Us: e.matmul(
                                    pY[:, h * 64:(h + 1) * 64], lhsT=Mrb[:, h, :], rhs=Us[:, h * 64:(h + 1) * 64], start=False, stop=False), [Mrbb, Usb], [pYb])
                                S.op("pe", lambda e, pY=pY, h=h, Mrk=Mrk, Vt=Vt, q=q: e.matmul(
                                    pY[:, h * 64:(h + 1) * 64], lhsT=Mrk[:, h, :], rhs=Vt[:, q, h * 64:(h + 1) * 64], start=False, stop=True), [Mrkb, Vtb], [pYb])
                            Ys, Ysb = Ysr.next()
                            copy_ev("act", Ys[:], pY[:, :], [pYb], [Ysb])
                            S.dma("sp", lambda e, Ys=Ys, u=u: e.dma_start(out=Yd[d][u * 128:(u + 1) * 128, :], in_=Ys[:]), [Ysb], [Yb[d][u]])
                        if "seq3" in dbg:
                            continue
                        pS, pSb = psum()
                        for cc in range(4):
                            S.op("pe", lambda e, pS=pS, cc=cc, BhT=BhT, Us=Us, q=q: e.matmul(
                                pS[:, cc * 128:(cc + 1) * 128], lhsT=BhT[:, q, cc * 128:(cc + 1) * 128], rhs=Us[:, cc * 128:(cc + 1) * 128], start=True, stop=False),
                                [BhTb, Usb], [pSb])
                            S.op("pe", lambda e, pS=pS, cc=cc, KhT=KhT, Vt=Vt, q=q: e.matmul(
                                pS[:, cc * 128:(cc + 1) * 128], lhsT=KhT[:, q, cc * 128:(cc + 1) * 128], rhs=Vt[:, q, cc * 128:(cc + 1) * 128], start=False, stop=True),
                                [KhTb, Vtb], [pSb])
                        pS3 = pS[:, :].rearrange("p (c t) -> p c t", c=4)
                        S.op("dve", lambda e: e.tensor_tensor(out=Stmp[:], in0=pS3, in1=blockones[:, None, :].to_broadcast([128, 4, 128]), op=ALU.mult), [pSb, CB], [STB])
                        for cc in range(4):
                            S.op("dve", lambda e: e.scalar_tensor_tensor(out=Sf[:, cc, :], in0=Sf[:, cc, :], scalar=GL[:, cc, q:q + 1], in1=Stmp[:, cc, :],
                                                                         op0=ALU.mult, op1=ALU.add), [SFB, GLb, STB], [SFB])
                        S.op("act", lambda e: e.copy(out=Sbf[:], in_=Sf[:]), [SFB], [SBB])


        def stage_readout(l, units):
            lg = S.sb("lnxg", [128, 512], F32)
            lb = S.sb("lnxb", [128, 512], F32)
            LB = Buf()
            bload(lg[:], lnx_g[l:l + 1, :], LB)
            bload(lb[:], lnx_b[l:l + 1, :], LB)
            yr = Ring(S, "ry", [128, 512], F32, 4)
            tr = Ring(S, "rt", [128, 512], F32, 4)
            vr = Ring(S, "rv", [128, 512], BF16, 2)
            sr = Ring(S, "rs", [128, 32], F32, 4)
            ob = Ring(S, "rob", [128, 4, 128], BF16, 2)
            h8 = lambda ap: ap.rearrange("p (h c) -> p h c", h=8)
            ro_cut = 99
            for d_ in dbg:
                if d_.startswith("rocut"):
                    ro_cut = int(d_[5:])
            for u in units:
                rows = slice(u * 128, (u + 1) * 128)
                y0, y0b = yr.next()
                y1, y1b = yr.next()
                g_, gb_ = tr.next()
                v_, vb_ = vr.next()
                st, stb = sr.next()
                S.dma("sp", lambda e: e.dma_start(out=y0[:], in_=Yd[0][rows, :]), [Yb[0][u]], [y0b])
                S.dma("sp", lambda e: e.dma_start(out=y1[:], in_=Yd[1][rows, :]), [Yb[1][u]], [y1b])
                S.dma("sp", lambda e: e.dma_start(out=g_[:], in_=GTd[rows, :]), [GTb[u]], [gb_])
                S.dma("sp", lambda e: e.dma_start(out=v_[:], in_=VTd[rows, :]), [VTb[u]], [vb_])
                S.dma("sp", lambda e: e.dma_start(out=st[:, 0:8], in_=BONd[0][rows, :]), [BONb[0][u]], [stb])
                S.dma("sp", lambda e: e.dma_start(out=st[:, 8:16], in_=BONd[1][rows, :]), [BONb[1][u]], [stb])
                S.op("dve", lambda e: e.tensor_tensor(out=y0[:], in0=y0[:], in1=y1[:], op=ALU.add), [y0b, y1b], [y0b])
                S.op("dve", lambda e: e.tensor_tensor(out=st[:, 0:8], in0=st[:, 0:8], in1=st[:, 8:16], op=ALU.add), [stb], [stb])
                if ro_cut <= 1:
                    continue
                S.op("dve", lambda e: e.tensor_reduce(out=st[:, 16:24], in_=h8(y0[:]), axis=AX.X, op=ALU.add), [y0b], [stb])
                S.op("dve", lambda e: e.tensor_scalar(out=st[:, 16:24], in0=st[:, 16:24], scalar1=1.0 / 64, scalar2=None, op0=ALU.mult), [stb], [stb])
                for hh in range(8):
                    S.op("dve", lambda e: e.tensor_scalar(out=y0[:, hh * 64:(hh + 1) * 64], in0=y0[:, hh * 64:(hh + 1) * 64], scalar1=st[:, 16 + hh:17 + hh], scalar2=None, op0=ALU.subtract), [y0b, stb], [y0b])
                if ro_cut <= 2:
                    continue
                S.op("pool", lambda e: e.tensor_tensor(out=y1[:], in0=y0[:], in1=y0[:], op=ALU.mult), [y0b], [y1b])
                S.op("dve", lambda e: e.tensor_reduce(out=st[:, 24:32], in_=h8(y1[:]), axis=AX.X, op=ALU.add), [y1b], [stb])
                S.op("act", lambda e: e.activation(out=st[:, 24:32], in_=st[:, 24:32], func=AF.Sqrt, scale=1.0 / 64, bias=64e-5), [stb], [stb])
                S.op("dve", lambda e: e.reciprocal(out=st[:, 24:32], in_=st[:, 24:32]), [stb], [stb])
                for hh in range(8):
                    S.op("dve", lambda e: e.tensor_scalar(out=y0[:, hh * 64:(hh + 1) * 64], in0=y0[:, hh * 64:(hh + 1) * 64], scalar1=st[:, 24 + hh:25 + hh], scalar2=None, op0=ALU.mult), [y0b, stb], [y0b])
                if ro_cut <= 3:
                    continue
                S.op("pool", lambda e: e.tensor_tensor(out=y0[:], in0=y0[:], in1=lg[:], op=ALU.mult), [y0b, LB], [y0b])
                S.op("pool", lambda e: e.tensor_tensor(out=y0[:], in0=y0[:], in1=lb[:], op=ALU.add), [y0b, LB], [y0b])
                for hh in range(8):
                    S.op("dve", lambda e: e.tensor_scalar(out=y1[:, hh * 64:(hh + 1) * 64], in0=v_[:, hh * 64:(hh + 1) * 64], scalar1=st[:, hh:hh + 1], scalar2=None, op0=ALU.mult), [vb_, stb], [y1b])
                S.op("dve", lambda e: e.tensor_tensor(out=y0[:], in0=y0[:], in1=y1[:], op=ALU.add), [y0b, y1b], [y0b])
                S.op("dve", lambda e: e.tensor_tensor(out=y0[:], in0=y0[:], in1=g_[:], op=ALU.mult), [y0b, gb_], [y0b])
                if ro_cut <= 4:
                    continue
                pt, pb = psum()
                for q in range(4):
                    S.op("pe", lambda e: e.matmul(pt[:, q * 128:(q + 1) * 128], lhsT=y0[:, q * 128:(q + 1) * 128], rhs=ident[:, :], start=True, stop=True), [y0b, CB], [pb])
                o_, ob_ = ob.next()
                copy_ev("dve", o_[:], pt[:, :].rearrange("p (q t) -> p q t", q=4), [pb], [ob_])
                if ro_cut <= 5:
                    continue
                for q in range(4):
                    S.dma("sp", lambda e: e.dma_start(out=CATT[q * 128:(q + 1) * 128, u * 128:(u + 1) * 128], in_=o_[:, q, :]),
                          [ob_], [CATb[(0, u)]])

        def stage_pool(l, do_ctx):
            pw = S.sb("poolw", [64, 4, 64], F32)
            ps_ = S.sb("poolsc", [64, 4], F32)
            PWB = Buf()
            for g in range(4):
                S.dma("sp", lambda e: e.dma_start(out=pw[:, g, :], in_=pool_w[l, g]), [], [PWB])
            S.dma("sp", lambda e: e.dma_start(out=ps_[:], in_=psc[l]), [], [PWB])
            bufA = S.sb("poolA", [64, 80 * 80], F32)
            bufB = S.sb("poolB", [64, 80 * 80], F32)
            AB, BB = Buf(), Buf()
            ict = S.sb("poolic", [64, 64 * 64], F32)
            ICB = Buf()
            pmr = Ring(S, "poolpm", [64, 4096], F32, 2)
            por = Ring(S, "poolo", [64, 512], BF16, 3)
            seqs = ([(0, 1, 256, c_ic_ctx)] if do_ctx else []) + [(256, 256, 64, c_ic_lat)]
            for (tok0, R_, C_, ictab) in seqs:
                RB_ = 64 if R_ > 1 else 1
                Wp = C_ + 16
                for g in range(4):
                    nst = g + 1
                    hr = 8 if R_ > 1 else 0
                    for r0 in range(0, R_, RB_):
                        Hp = RB_ + 2 * hr
                        A3 = bufA[:, 0:Hp * Wp].rearrange("p (r c) -> p r c", c=Wp)
                        B3 = bufB[:, 0:Hp * Wp].rearrange("p (r c) -> p r c", c=Wp)
                        S.op("pool", lambda e: e.memset(bufA[:, 0:Hp * Wp], 0.0), [], [AB])
                        S.op("pool", lambda e: e.memset(bufB[:, 0:Hp * Wp], 0.0), [], [BB])
                        ra, rb = max(r0 - hr, 0), min(r0 + RB_ + hr, R_)
                        for rc in range(ra, rb, 16):
                            rd = min(rc + 16, rb)
                            src = PTd[1792 + 64 * g:1792 + 64 * (g + 1), tok0 + rc * C_:tok0 + rd * C_].rearrange("p (r c) -> p r c", c=C_)
                            S.dma("sp", lambda e: e.dma_start(out=A3[:, rc - (r0 - hr):rd - (r0 - hr), 8:8 + C_], in_=src),
                                  [PTb[(14 + g // 2, k)] for k in range(-1, 32)], [AB])
                        S.dma("sp", lambda e: e.dma_start(
                            out=ict[:, 0:RB_ * C_],
                            in_=ictab[g:g + 1, r0:r0 + RB_, :].rearrange("g r c -> g (r c)").to_broadcast([64, RB_ * C_])), [], [ICB])
                        cur, curb, oth, othb = A3, AB, B3, BB
                        shifts = [(1, 0), (1, 1), (2, 2), (4, 4)][:nst]
                        for (sa, sb_) in shifts:
                            S.op("dve", lambda e: e.tensor_tensor(out=oth[:, :, sa:Wp - sb_], in0=cur[:, :, 0:Wp - sb_ - sa], in1=cur[:, :, sa + sb_:Wp], op=ALU.add),
                                 [curb], [othb])
                            cur, curb, oth, othb = oth, othb, cur, curb
                        if R_ > 1:
                            for (sa, sb_) in shifts:
                                S.op("dve", lambda e: e.tensor_tensor(out=oth[:, sa:Hp - sb_, :], in0=cur[:, 0:Hp - sb_ - sa, :], in1=cur[:, sa + sb_:Hp, :], op=ALU.add),
                                     [curb], [othb])
                                cur, curb, oth, othb = oth, othb, cur, curb
                        pm, pmb = pmr.next()
                        pm3 = pm[:, 0:RB_ * C_].rearrange("p (r c) -> p r c", c=C_)
                        S.op("dve", lambda e: e.tensor_tensor(out=pm3, in0=cur[:, hr:hr + RB_, 8:8 + C_], in1=ict[:, 0:RB_ * C_].rearrange("p (r c) -> p r c", c=C_), op=ALU.mult),
                             [curb, ICB], [pmb])
                        for rc in range(0, RB_, 16):
                            rd = min(rc + 16, RB_)
                            S.dma("sp", lambda e: e.dma_start(out=oth[:, hr + rc:hr + rd, 8:8 + C_],
                                                              in_=PTd[1792 + 64 * g:1792 + 64 * (g + 1), tok0 + (r0 + rc) * C_:tok0 + (r0 + rd) * C_].rearrange("p (r c) -> p r c", c=C_)),
                                  [PTb[(14 + g // 2, k)] for k in range(-1, 32)], [othb])
                        S.op("dve", lambda e: e.tensor_tensor(out=pm3, in0=pm3, in1=oth[:, hr:hr + RB_, 8:8 + C_], op=ALU.subtract), [pmb, othb], [pmb])
                        ntok = RB_ * C_
                        for c0 in range(0, ntok, 512):
                            nn = min(512, ntok - c0)
                            pt, pb = psum()
                            S.op("pe", lambda e: e.matmul(pt[0:64, 0:nn], lhsT=pw[:, g, :], rhs=pm[:, c0:c0 + nn], start=True, stop=True), [PWB, pmb], [pb])
                            o_, ob_ = por.next()
                            S.op("act", lambda e: e.activation(out=o_[:, 0:nn], in_=pt[0:64, 0:nn], func=AF.Copy, scale=ps_[:, g:g + 1]), [pb, PWB], [ob_])
                            t_a = tok0 + r0 * C_ + c0
                            S.dma("sp", lambda e: e.dma_start(out=CATT[512 + 64 * g:512 + 64 * (g + 1), t_a:t_a + nn], in_=o_[:, 0:nn]), [ob_],
                                  [CATb[(1, g, t_a)]])

        def stage_fourier(l, do_ctx):
            d128 = S.sb("d128", [128, 3, 128], F32)
            tw = S.sb("twid", [128, 2, 128], F32)
            d64 = S.sb("d64", [64, 2, 2, 128], F32)
            fw = S.sb("fw", [64, 4, 64], F32)
            Gt = S.sb("Gt", [128, 2, 2, 64], F32)
            FB = Buf()
            S.dma("sp", lambda e: e.dma_start(out=d128[:], in_=c_dft128), [], [FB])
            S.dma("sp", lambda e: e.dma_start(out=tw[:], in_=c_twid), [], [FB])
            S.dma("sp", lambda e: e.dma_start(out=d64[:], in_=c_dft64pad), [], [FB])
            for h in range(4):
                S.dma("sp", lambda e: e.dma_start(out=fw[:, h, :], in_=fourier_w[l, h]), [], [FB])
            for h in range(4):
                for ci in range(2):
                    pt, pb = psum()
                    S.op("pe", lambda e: e.matmul(pt[:, 0:64], lhsT=d64[:, ci, h % 2, :], rhs=fw[:, h, :], start=True, stop=True), [FB], [pb])
                    hp = slice((h % 2) * 64, (h % 2) * 64 + 64)
                    if ci == 0:
                        S.op("dve", lambda e: e.tensor_copy(out=Gt[hp, h // 2, 0, :], in_=pt[hp, 0:64]), [pb], [FB])
                    else:
                        S.op("dve", lambda e: e.tensor_scalar(out=Gt[hp, h // 2, 1, :], in0=pt[hp, 0:64], scalar1=-1.0, scalar2=None, op0=ALU.mult), [pb], [FB])
            outr = Ring(S, "fo", [64, 512], BF16, 3)
            if do_ctx:
                d256 = S.sb("d256", [128, 2, 2, 256], F32)
                xc = S.sb("fxc", [128, 2, 256], F32)
                XB = Buf()
                S.dma("sp", lambda e: e.dma_start(out=d256[:], in_=c_dft256), [], [XB])
                S.dma("sp", lambda e: e.dma_start(out=xc[:], in_=PFd[0:256, :].rearrange("(a p) c -> p a c", p=128)), [PFb[0], PFb[1]], [XB])
                xri = S.sb("fxri", [128, 2, 2, 256], F32)
                XRB = Buf()
                for pair in range(2):
                    for ci in range(2):
                        pt, pb = psum()
                        for a in range(2):
                            S.op("pe", lambda e: e.matmul(pt[:, 0:256], lhsT=xc[:, a, pair * 128:(pair + 1) * 128], rhs=d256[:, a, ci, :], start=(a == 0), stop=(a == 1)),
                                 [XB], [pb])
                        copy_ev(evE(), xri[:, pair, ci, :], pt[:, 0:256], [pb], [XRB])
                for h in range(4):
                    hp = slice((h % 2) * 64, (h % 2) * 64 + 64)
                    pt, pb = psum()
                    S.op("pe", lambda e: e.matmul(pt[0:64, 0:256], lhsT=Gt[hp, h // 2, 0, :], rhs=xri[hp, h // 2, 0, :], start=True, stop=False), [FB, XRB], [pb])
                    S.op("pe", lambda e: e.matmul(pt[0:64, 0:256], lhsT=Gt[hp, h // 2, 1, :], rhs=xri[hp, h // 2, 1, :], start=False, stop=True), [FB, XRB], [pb])
                    o_, ob_ = outr.next()
                    S.op("dve", lambda e: e.tensor_scalar(out=o_[:, 0:256], in0=pt[0:64, 0:256], scalar1=8.0, scalar2=None, op0=ALU.mult), [pb], [ob_])
                    S.dma("sp", lambda e: e.dma_start(out=CATT[768 + 64 * h:768 + 64 * (h + 1), 0:256], in_=o_[:, 0:256]), [ob_], [CATb[(2, h, -1)]])
            x1 = S.sb("fx1", [128, 128 * 64], F32)
            X1B = Buf()
            x1c = S.sb("fx1c", [128, 128 * 64], F32)
            X1CB = Buf()
            zr = Ring(S, "fz", [128, 2, 128], F32, 3)
            t4 = Ring(S, "ft4", [128, 4, 128], F32, 2)
            xo = Ring(S, "fxo", [128, 2, 128], F32, 3)
            FXb = MBuf()
            for h in range(4):
                for part in range(16):
                    S.dma("sp", lambda e: e.dma_start(
                        out=x1[:, part * 512:(part + 1) * 512].rearrange("p (t c) -> p t c", c=64),
                        in_=PFd[256:TT, h * 64:(h + 1) * 64].rearrange("(t1 t2) c -> t1 t2 c", t2=128)[:, part * 8:(part + 1) * 8, :]),
                        [PFb[u] for u in range(2, NU)], [X1B])
                x13 = x1[:, :].rearrange("p (t c) -> p t c", c=64)
                x1c3 = x1c[:, :].rearrange("p (c t) -> p c t", t=128)
                for c8 in range(0, 64, 8):
                    S.op("dve" if (c8 // 8) % 2 else "pool", lambda e: e.tensor_copy(out=x1c3[:, c8:c8 + 8, :], in_=x13[:, :, c8:c8 + 8].rearrange("p t c -> p c t")),
                         [X1B], [X1CB])
                for c in range(64):
                    py, pyb = psum()
                    S.op("pe", lambda e: e.matmul(py[:, 0:256], lhsT=x1c3[:, c, :], rhs=d128[:, 0:2, :].rearrange("p a k -> p (a k)"), start=True, stop=True), [X1CB, FB], [pyb])
                    t_, tb_ = t4.next()
                    y3 = py[:, 0:256].rearrange("p (a k) -> p a k", a=2)
                    S.op("dve", lambda e: e.tensor_tensor(out=t_[:, 0:2, :], in0=y3, in1=tw[:, :, :], op=ALU.mult), [pyb, FB], [tb_])
                    S.op("dve", lambda e: e.tensor_tensor(out=t_[:, 2, :], in0=py[:, 0:128], in1=tw[:, 1, :], op=ALU.mult), [pyb, FB], [tb_])
                    S.op("dve", lambda e: e.tensor_tensor(out=t_[:, 3, :], in0=py[:, 128:256], in1=tw[:, 0, :], op=ALU.mult), [pyb, FB], [tb_])
                    z_, zb_ = zr.next()
                    S.op("pool", lambda e: e.tensor_tensor(out=z_[:, 0, :], in0=t_[:, 0, :], in1=t_[:, 1, :], op=ALU.subtract), [tb_], [zb_])
                    S.op("pool", lambda e: e.tensor_tensor(out=z_[:, 1, :], in0=t_[:, 2, :], in1=t_[:, 3, :], op=ALU.add), [tb_], [zb_])
                    px, pxb = psum()
                    S.op("pe", lambda e: e.matmul(px[:, 0:128], lhsT=d128[:, 0, :], rhs=z_[:, 0, :], start=True, stop=False), [FB, zb_], [pxb])
                    S.op("pe", lambda e: e.matmul(px[:, 0:128], lhsT=d128[:, 2, :], rhs=z_[:, 1, :], start=False, stop=True), [FB, zb_], [pxb])
                    pxi, pxib = psum()
                    S.op("pe", lambda e: e.matmul(pxi[:, 0:128], lhsT=d128[:, 0, :], rhs=z_[:, 1, :], start=True, stop=False), [FB, zb_], [pxib])
                    S.op("pe", lambda e: e.matmul(pxi[:, 0:128], lhsT=d128[:, 1, :], rhs=z_[:, 0, :], start=False, stop=True), [FB, zb_], [pxib])
                    x_, xb_ = xo.next()
                    copy_ev("act", x_[:, 0, :], px[:, 0:128], [pxb], [xb_])
                    copy_ev("act", x_[:, 1, :], pxi[:, 0:128], [pxib], [xb_])
                    ch = h * 64 + c
                    S.dma("sp", lambda e: e.dma_start(out=FXd[:, ch, :].rearrange("a (k2 k1) -> k2 a k1", k1=128), in_=x_[:]), [xb_], [FXb[h]])
                hp = slice((h % 2) * 64, (h % 2) * 64 + 64)
                fin = Ring(S, f"ffin{h}", [128, 2, 512], F32, 2)
                for c0 in range(0, T_LAT, 512):
                    fi, fib = fin.next()
                    S.dma("sp", lambda e: e.dma_start(out=fi[hp, :, :], in_=FXd[:, h * 64:(h + 1) * 64, c0:c0 + 512].rearrange("a c t -> c a t")), [FXb[h]], [fib])
                    pt, pb = psum()
                    S.op("pe", lambda e: e.matmul(pt[0:64, :], lhsT=Gt[hp, h // 2, 0, :], rhs=fi[hp, 0, :], start=True, stop=False), [FB, fib], [pb])
                    S.op("pe", lambda e: e.matmul(pt[0:64, :], lhsT=Gt[hp, h // 2, 1, :], rhs=fi[hp, 1, :], start=False, stop=True), [FB, fib], [pb])
                    o_, ob_ = outr.next()
                    copy_ev(evE(), o_[:, :], pt[0:64, :], [pb], [ob_])
                    S.dma("sp", lambda e: e.dma_start(out=CATT[768 + 64 * h:768 + 64 * (h + 1), 256 + c0:256 + c0 + 512], in_=o_[:, :]), [ob_], [CATb[(2, h, c0)]])


        H2b = MBuf()
        XEb = Buf()
        MOEb = Buf()
        A_all = S.sb("A_all", [128, NU, 16], F32)
        AAB = Buf()
        ones128 = S.sb("ones128", [128, 128], F32)
        S.op("pool", lambda e: e.memset(ones128[:], 1.0), [], [CB])

        def stage_O(l, units):
            wout = S.sb("wout", [128, 8, D], BF16)
            WOB = Buf()
            for kc in range(8):
                S.dma("pool", lambda e: e.dma_start(out=wout[:, kc, :], in_=w_out[l, kc * 128:(kc + 1) * 128, :]), [], [WOB])
            rw = S.sb("rw", [128, 8, 16], F32)
            S.dma("sp", lambda e: e.dma_start(out=rw[:], in_=router_w[l].rearrange("(kc p) n -> p kc n", p=128)), [], [WOB])
            G1 = [S.sb(f"G1_{j}", [128, D], F32) for j in range(2)]
            G2 = [S.sb(f"G2_{j}", [128, D], F32) for j in range(2)]
            SH2 = [S.sb(f"SH2_{j}", [128, D], F32) for j in range(2)]
            gt = S.sb("gtO", [128, D], F32)
            GB = Buf()
            bload(gt[:], norm_ffn_g[l:l + 1, :], GB)
            for j, rowi in ((0, 1), (1, 0)):
                bload(G1[j][:], MODd[rowi:rowi + 1, 2 * D:3 * D], GB, [MODb])
                bload(G2[j][:], MODd[rowi:rowi + 1, 4 * D:5 * D], GB, [MODb])
                bload(SH2[j][:], MODd[rowi:rowi + 1, 3 * D:4 * D], GB, [MODb])
                S.op("dve", lambda e: e.scalar_tensor_tensor(out=G2[j][:], in0=G2[j][:], scalar=1.0, in1=gt[:], op0=ALU.add, op1=ALU.mult), [GB], [GB])
            catr = Ring(S, "catO", [128, 8, 128], BF16, 2)
            hr_ = Ring(S, "hO", [128, D], F32, 2)
            h2r = Ring(S, "h2O", [128, HW_], F32, 2)
            tmpr = Ring(S, "tmpO", [128, 512], F32, 2)
            sq = S.sb("sqO", [128, D], F32)
            SQB = Buf()
            str_ = Ring(S, "stO", [128, 8], F32, 4)
            h2T = Ring(S, "h2T", [128, 8, 128], F32, 2)
            idr = Ring(S, "idO", [128, 1], I32, 2)
            for u in units:
                j = 0 if u < 2 else 1
                rows = slice(u * 128, (u + 1) * 128)
                ct, ctb = catr.next()
                S.dma("sp", lambda e: e.dma_start(out=ct[:], in_=CATT[:, rows].rearrange("(kc p) t -> p kc t", p=128)),
                      [CATb[(0, u)]] + [b for k_, b in CATb.d.items() if k_[0] != 0], [ctb])
                h_, hb_ = hr_.next()
                S.dma("sp", lambda e: e.dma_start(out=h_[:], in_=HT[rows, :]), [HTb[u]], [hb_])
                for half in range(2):
                    pt, pb = psum()
                    for kc in range(8):
                        S.op("pe", lambda e: e.matmul(pt[:, :], lhsT=ct[:, kc, :], rhs=wout[:, kc, half * 512:(half + 1) * 512], start=(kc == 0), stop=(kc == 7)), [ctb, WOB], [pb])
                    t_, tb_ = tmpr.next()
                    S.op("dve", lambda e: e.tensor_tensor(out=t_[:], in0=pt[:, :], in1=G1[j][:, half * 512:(half + 1) * 512], op=ALU.mult), [pb, GB], [tb_])
                    S.op("pool", lambda e: e.tensor_tensor(out=h_[:, half * 512:(half + 1) * 512], in0=h_[:, half * 512:(half + 1) * 512], in1=t_[:], op=ALU.add), [hb_, tb_], [hb_])
                S.dma("sp", lambda e: e.dma_start(out=HT[rows, :], in_=h_[:]), [hb_], [HTb[u]])
                sc, scb = str_.next()
                S.op("act", lambda e: e.activation(out=sq[:], in_=h_[:], func=AF.Square, accum_out=sc[:, 0:1]), [hb_], [SQB, scb])
                S.op("act", lambda e: e.activation(out=sc[:, 1:2], in_=sc[:, 0:1], func=AF.Sqrt, scale=1.0 / D, bias=1e-6), [scb], [scb])
                S.op("dve", lambda e: e.reciprocal(out=sc[:, 1:2], in_=sc[:, 1:2]), [scb], [scb])
                h2, h2b = h2r.next()
                S.op("dve", lambda e: e.scalar_tensor_tensor(out=h2[:, 0:D], in0=h_[:], scalar=sc[:, 1:2], in1=G2[j][:], op0=ALU.mult, op1=ALU.mult), [hb_, scb, GB], [h2b])
                S.op("pool", lambda e: e.tensor_tensor(out=h2[:, 0:D], in0=h2[:, 0:D], in1=SH2[j][:], op=ALU.add), [h2b, GB], [h2b])
                hT, hTb = h2T.next()
                for half in range(2):
                    pt, pb = psum()
                    for q in range(4):
                        kc = half * 4 + q
                        S.op("pe", lambda e: e.transpose(pt[:, q * 128:(q + 1) * 128], h2[:, kc * 128:(kc + 1) * 128], ident[:]), [h2b, CB], [pb])
                    copy_ev(evE(), hT[:, half * 4:(half + 1) * 4, :], pt[:, :].rearrange("p (q t) -> p q t", q=4), [pb], [hTb])
                pl, plb = psum()
                for kc in range(8):
                    S.op("pe", lambda e: e.matmul(pl[:, 0:16], lhsT=hT[:, kc, :], rhs=rw[:, kc, :], start=(kc == 0), stop=(kc == 7)), [hTb, WOB], [plb])
                S.op("dve", lambda e: e.tensor_reduce(out=sc[:, 2:3], in_=pl[:, 0:16], axis=AX.X, op=ALU.max), [plb], [scb])
                S.op("dve", lambda e: e.tensor_scalar(out=sc[:, 2:3], in0=sc[:, 2:3], scalar1=-1.0, scalar2=None, op0=ALU.mult), [scb], [scb])
                S.op("act", lambda e: e.activation(out=h2[:, 1025:1041], in_=pl[:, 0:16], func=AF.Exp, bias=sc[:, 2:3], accum_out=sc[:, 3:4]), [plb, scb], [h2b, scb])
                S.op("dve", lambda e: e.reciprocal(out=sc[:, 3:4], in_=sc[:, 3:4]), [scb], [scb])
                S.op("dve", lambda e: e.tensor_scalar(out=h2[:, 1025:1041], in0=h2[:, 1025:1041], scalar1=sc[:, 3:4], scalar2=None, op0=ALU.mult), [h2b, scb], [h2b])
                S.op("pool", lambda e: e.tensor_copy(out=A_all[:, u, :], in_=h2[:, 1025:1041]), [h2b], [AAB])
                S.op("pool", lambda e: e.memset(h2[:, 1041:HW_], 0.0), [], [h2b])
                id_, idb_ = idr.next()
                S.op("pool", lambda e: e.iota(id_[:], pattern=[[1, 1]], base=u * 128, channel_multiplier=1), [], [idb_])
                S.op("pool", lambda e: e.tensor_copy(out=h2[:, 1024:1025].bitcast(I32), in_=id_[:]), [idb_], [h2b])
                S.dma("sp", lambda e: e.dma_start(out=H2d[rows, :], in_=h2[:]), [h2b], [H2b[u]])

        def stage_route(u0, nun, cap, slot0):
            J = nun
            Av = A_all[:, u0:u0 + nun, :]
            cmpA = S.sb("cmpA", [128, J, 16], F32)
            cmpB = S.sb("cmpB", [128, J, 16], F32)
            CAB, CBB = Buf(), Buf()
            lo = S.sb("lo", [128, 16], F32)
            mid = S.sb("mid", [128, 16], F32)
            cnt = S.sb("cnt", [128, 16], F32)
            ge = S.sb("ge", [128, 16], F32)
            LB_ = Buf()
            S.op("dve", lambda e: e.memset(lo[:], 0.0), [], [LB_])
            bc = lambda t_: t_[:, None, :].to_broadcast([128, J, 16])
            for it in range(26):
                hw = 2.0 ** -(it + 1)
                S.op("dve", lambda e: e.tensor_scalar(out=mid[:], in0=lo[:], scalar1=hw, scalar2=None, op0=ALU.add), [LB_], [LB_])
                S.op("dve", lambda e: e.tensor_tensor(out=cmpA[:], in0=Av, in1=bc(mid), op=ALU.is_ge), [AAB, LB_], [CAB])
                S.op("dve", lambda e: e.tensor_reduce(out=cnt[:], in_=cmpA[:].rearrange("p j e -> p e j"), axis=AX.X, op=ALU.add), [CAB], [LB_])
                pt, pb = psum()
                S.op("pe", lambda e: e.matmul(pt[:, 0:16], lhsT=ones128[:, :], rhs=cnt[:, :], start=True, stop=True), [CB, LB_], [pb])
                S.op("dve", lambda e: e.tensor_scalar(out=ge[:], in0=pt[:, 0:16], scalar1=float(cap), scalar2=None, op0=ALU.is_ge), [pb], [LB_])
                S.op("dve", lambda e: e.scalar_tensor_tensor(out=lo[:], in0=ge[:], scalar=hw, in1=lo[:], op0=ALU.mult, op1=ALU.add), [LB_], [LB_])
            Mk = S.sb("Mk", [128, J, 16], F32)
            MKB = Buf()
            S.op("dve", lambda e: e.tensor_tensor(out=Mk[:], in0=Av, in1=bc(lo), op=ALU.is_ge), [AAB, LB_], [MKB])
            S.op("dve", lambda e: e.tensor_reduce(out=cnt[:], in_=Mk[:].rearrange("p j e -> p e j"), axis=AX.X, op=ALU.add), [MKB], [LB_])
            pt, pb = psum()
            S.op("pe", lambda e: e.matmul(pt[:, 0:16], lhsT=masks[:, 0, :], rhs=cnt[:, :], start=True, stop=True), [CB, LB_], [pb])
            base = S.sb("base", [128, 16], F32)
            S.op("dve", lambda e: e.tensor_scalar(out=base[:], in0=pt[:, 0:16], scalar1=float(slot0), scalar2=None, op0=ALU.add), [pb], [LB_])
            S.op("dve", lambda e: e.tensor_copy(out=cmpA[:], in_=Mk[:]), [MKB], [CAB])
            cur, curb, oth, othb = cmpA, CAB, cmpB, CBB
            sh = 1
            while sh < J:
                S.op("dve", lambda e: e.tensor_tensor(out=oth[:, sh:J, :], in0=cur[:, sh:J, :], in1=cur[:, 0:J - sh, :], op=ALU.add), [curb], [othb])
                S.op("pool", lambda e: e.tensor_copy(out=oth[:, 0:sh, :], in_=cur[:, 0:sh, :]), [curb], [othb])
                cur, curb, oth, othb = oth, othb, cur, curb
                sh *= 2
            S.op("dve", lambda e: e.tensor_tensor(out=cur[:], in0=cur[:], in1=Mk[:], op=ALU.subtract), [curb, MKB], [curb])
            S.op("dve", lambda e: e.tensor_tensor(out=cur[:], in0=cur[:], in1=bc(base), op=ALU.add), [curb, LB_], [curb])
            S.op("dve", lambda e: e.scalar_tensor_tensor(out=cur[:], in0=cur[:], scalar=-BIGSLOT, in1=Mk[:], op0=ALU.add, op1=ALU.mult), [curb, MKB], [curb])
            S.op("dve", lambda e: e.tensor_scalar(out=cur[:], in0=cur[:], scalar1=BIGSLOT, scalar2=None, op0=ALU.add), [curb], [curb])
            SL = S.sb("SL", [128, J, 16], I32)
            SLB = Buf()
            S.op("dve", lambda e: e.tensor_copy(out=SL[:], in_=cur[:]), [curb], [SLB])
            xr = Ring(S, "h2disp", [128, HW_], F32, 3)
            for jj in range(nun):
                u = u0 + jj
                x_, xb_ = xr.next()
                S.dma("sp", lambda e: e.dma_start(out=x_[:], in_=H2d[u * 128:(u + 1) * 128, :]), [H2b[u]], [xb_])
                for ex in range(16):
                    S.dma("pool", lambda e: e.indirect_dma_start(
                        out=XEd[ex], out_offset=bass.IndirectOffsetOnAxis(ap=SL[:, jj, ex:ex + 1], axis=0),
                        in_=x_[:, :], in_offset=None, bounds_check=RegRef(slot0 + cap - 1), oob_is_err=False), [xb_, SLB], [XEb])

        def stage_ffn(l, chunks):
            zero1k = S.sb("zero1k", [128, D], F32)
            S.op("pool", lambda e: e.memset(zero1k[:], 0.0), [], [CB])
            for u in range(NU):
                S.dma("sp", lambda e: e.dma_start(out=MOEd[u * 128:(u + 1) * 128, :], in_=zero1k[:]), [CB], [MOEb])
            wr = [Ring(S, f"ew{k_}", [128, 8, D], BF16, 2) for k_ in range(3)]
            xer = Ring(S, "xe", [128, HW_], F32, 3)
            xeT = S.sb("xeT", [128, 8, 512], BF16)
            XTB = Buf()
            hT = S.sb("hTf", [128, 8, 512], BF16)
            HTB_ = Buf()
            tmpr = Ring(S, "ftmp", [128, 512], F32, 2)
            yer = Ring(S, "ye", [128, D], F32, 2)
            gc = S.sb("gcol", [128, 4], F32)
            ix = [S.sb(f"ixc{i}", [128, 1], I32) for i in range(4)]
            GCB = Buf()
            for ex in range(16):
                W = []
                for k_, src in enumerate((exp_w_gate, exp_w_up, exp_w_down)):
                    wt, wb = wr[k_].next()
                    for kc in range(8):
                        S.dma("pool", lambda e: e.dma_start(out=wt[:, kc, :], in_=src[l, ex, kc * 128:(kc + 1) * 128, :]), [], [wb])
                    W.append((wt, wb))
                (wg, wgb), (wu, wub), (wd, wdb) = W
                for (s0, ns) in chunks:
                    subs = [(i * 128, min(128, ns - i * 128)) for i in range((ns + 127) // 128)]
                    for si, (so, sn) in enumerate(subs):
                        xe, xeb = xer.next()
                        S.dma("sp", lambda e: e.dma_start(out=xe[0:sn, :], in_=XEd[ex][s0 + so:s0 + so + sn, :]), [XEb], [xeb])
                        S.op("pool", lambda e: e.tensor_copy(out=gc[0:sn, si:si + 1], in_=xe[0:sn, 1025 + ex:1026 + ex]), [xeb], [GCB])
                        S.op("pool", lambda e: e.tensor_copy(out=ix[si][0:sn, :], in_=xe[0:sn, 1024:1025].bitcast(I32)), [xeb], [GCB])
                        for half in range(2):
                            pt, pb = psum()
                            for q in range(4):
                                kc = half * 4 + q
                                S.op("pe", lambda e: e.transpose(pt[:, q * 128:q * 128 + sn], xe[0:sn, kc * 128:(kc + 1) * 128], ident[0:sn, 0:sn]), [xeb, CB], [pb])
                            copy_ev(evE(), xeT[:, half * 4:(half + 1) * 4, so:so + sn], pt[:, :].rearrange("p (q t) -> p q t", q=4)[:, :, 0:sn], [pb], [XTB])
                    for fc in range(8):
                        pg, pgb = psum()
                        pu, pub = psum()
                        for kc in range(8):
                            S.op("pe", lambda e: e.matmul(pg[:, 0:ns], lhsT=wg[:, kc, fc * 128:(fc + 1) * 128], rhs=xeT[:, kc, 0:ns], start=(kc == 0), stop=(kc == 7)), [wgb, XTB], [pgb])
                        for kc in range(8):
                            S.op("pe", lambda e: e.matmul(pu[:, 0:ns], lhsT=wu[:, kc, fc * 128:(fc + 1) * 128], rhs=xeT[:, kc, 0:ns], start=(kc == 0), stop=(kc == 7)), [wub, XTB], [pub])
                        t_, tb_ = tmpr.next()
                        S.op("act", lambda e: e.activation(out=t_[:, 0:ns], in_=pg[:, 0:ns], func=AF.Silu), [pgb], [tb_])
                        S.op("dve", lambda e: e.tensor_tensor(out=hT[:, fc, 0:ns], in0=t_[:, 0:ns], in1=pu[:, 0:ns], op=ALU.mult), [tb_, pub], [HTB_])
                    for si, (so, sn) in enumerate(subs):
                        ye, yeb = yer.next()
                        for half in range(2):
                            pt, pb = psum()
                            for fc in range(8):
                                S.op("pe", lambda e: e.matmul(pt[0:sn, :], lhsT=hT[:, fc, so:so + sn], rhs=wd[:, fc, half * 512:(half + 1) * 512], start=(fc == 0), stop=(fc == 7)), [HTB_, wdb], [pb])
                            S.op("dve", lambda e: e.tensor_scalar(out=ye[0:sn, half * 512:(half + 1) * 512], in0=pt[0:sn, :], scalar1=gc[0:sn, si:si + 1], scalar2=None, op0=ALU.mult), [pb, GCB], [yeb])
                        S.dma("pool", lambda e: e.indirect_dma_start(
                            out=MOEd, out_offset=bass.IndirectOffsetOnAxis(ap=ix[si][0:sn, :], axis=0),
                            in_=ye[0:sn, :], in_offset=None, bounds_check=RegRef(TT - 1), oob_is_err=True, compute_op=ALU.add), [yeb, GCB], [MOEb])

        def stage_combine(l, units):
            G2g = [S.sb(f"G2g{j}", [128, D], F32) for j in range(2)]
            GB = Buf()
            for j, rowi in ((0, 1), (1, 0)):
                bload(G2g[j][:], MODd[rowi:rowi + 1, 5 * D:6 * D], GB, [MODb])
            hr_ = Ring(S, "hC", [128, D], F32, 3)
            mr_ = Ring(S, "mC", [128, D], F32, 3)
            for u in units:
                j = 0 if u < 2 else 1
                rows = slice(u * 128, (u + 1) * 128)
                h_, hb_ = hr_.next()
                m_, mb_ = mr_.next()
                S.dma("sp", lambda e: e.dma_start(out=h_[:], in_=HT[rows, :]), [HTb[u]], [hb_])
                S.dma("sp", lambda e: e.dma_start(out=m_[:], in_=MOEd[rows, :]), [MOEb], [mb_])
                S.op("dve", lambda e: e.tensor_tensor(out=m_[:], in0=m_[:], in1=G2g[j][:], op=ALU.mult), [mb_, GB], [mb_])
                S.op("pool", lambda e: e.tensor_tensor(out=h_[:], in0=h_[:], in1=m_[:], op=ALU.add), [hb_, mb_], [hb_])
                S.dma("sp", lambda e: e.dma_start(out=HT[rows, :], in_=h_[:]), [hb_], [HTb[u]])

        def stage_final():
            gt = S.sb("fng", [128, D], F32)
            FGB = Buf()
            bload(gt[:], final_norm_g[0:1, :], FGB)
            xr = Ring(S, "fin_x", [128, D], F32, 3)
            sq = S.sb("fin_sq", [128, D], F32)
            SQB = Buf()
            st_ = Ring(S, "fin_st", [128, 2], F32, 4)
            OB = MBuf()
            for u in range(2, NU):
                xi, xib = xr.next()
                S.dma("sp", lambda e: e.dma_start(out=xi[:], in_=HT[u * 128:(u + 1) * 128, :]), [HTb[u]], [xib])
                sc, scb = st_.next()
                S.op("act", lambda e: e.activation(out=sq[:], in_=xi[:], func=AF.Square, accum_out=sc[:, 0:1]), [xib], [SQB, scb])
                S.op("act", lambda e: e.activation(out=sc[:, 1:2], in_=sc[:, 0:1], func=AF.Sqrt, scale=1.0 / D, bias=1e-6), [scb], [scb])
                S.op("dve", lambda e: e.reciprocal(out=sc[:, 1:2], in_=sc[:, 1:2]), [scb], [scb])
                S.op("dve", lambda e: e.scalar_tensor_tensor(out=xi[:], in0=xi[:], scalar=sc[:, 1:2], in1=gt[:], op0=ALU.mult, op1=ALU.mult), [xib, scb, FGB], [xib])
                S.dma("sp", lambda e: e.dma_start(out=out[(u - 2) * 128:(u - 1) * 128, :], in_=xi[:]), [xib], [OB[u]])

        nlayers = 1 if "l0only" in dbg else DEPTH
        for l in range(nlayers):
            last = (l == DEPTH - 1)
            units = range(2, NU) if last else range(NU)
            if "noadaln" not in dbg:
                with S.scope():
                    stage_adaln(l)
            if "noA" not in dbg:
                with S.scope():
                    stage_A(l)
            if "noscan" not in dbg:
                with S.scope():
                    stage_rwkv(l, need_y_ctx=not last)
                if "noreadout" not in dbg:
                    with S.scope():
                        stage_readout(l, range(2) if "blocks1" in dbg else (units if "short" not in dbg else range(10)))
            if "nopf" not in dbg:
                if "nopool" not in dbg:
                    with S.scope():
                        stage_pool(l, not last)
                if "nofourier" not in dbg:
                    with S.scope():
                        stage_fourier(l, not last)
            if "noO" not in dbg:
                with S.scope():
                    stage_O(l, units)
            if "nomoe" not in dbg:
                if not last:
                    with S.scope():
                        stage_route(0, 2, 32, 2048)
                with S.scope():
                    stage_route(2, 128, 2048, 0)
                with S.scope():
                    stage_ffn(l, [(0, 512), (512, 512), (1024, 512), (1536, 512)] + ([] if last else [(2048, 32)]))
                with S.scope():
                    stage_combine(l, units)
        with S.scope():
            stage_final()
        S.finish()
        S.run_block()
        print("instructions:", S.n_instr)
    return nc


def prep_inputs(inputs, b):
    hc = host_consts()
    m = dict(hc)
    f = lambda a: np.ascontiguousarray(np.asarray(a, dtype=np.float32))
    m["x"] = f(inputs["x"][b])
    m["ctx"] = f(inputs["ctx"][b])
    cc = np.zeros((128, 16), np.float32)
    cc[:, 0:16:2] = np.asarray(inputs["c"][b]).reshape(8, 128).T
    cc[:, 1:16:2] = np.asarray(inputs["c_ctx"]).reshape(8, 128).T
    m["ccond"] = cc
    for k in ("ada_w", "ada_b", "norm_mix_g", "norm_ffn_g", "w_in", "decay_w0", "decay_up", "iclr_up", "gate_up", "lnx_g", "lnx_b"):
        m[k] = f(inputs[k])
    pp = np.zeros((DEPTH, 128, 64), np.float32)
    for l in range(DEPTH):
        sw = np.asarray(inputs["shift_w"][l])
        pp[l, :, 0:42] = sw.reshape(3, 14, 128).transpose(2, 1, 0).reshape(128, 42)
        for d in range(2):
            pp[l, :, 42 + 4 * d:46 + 4 * d] = np.asarray(inputs["iclr_a0"][l, d]).reshape(4, 128).T
        pp[l, :, 50:54] = np.asarray(inputs["k_k"][l]).reshape(4, 128).T
        pp[l, :, 54:58] = np.asarray(inputs["k_a"][l]).reshape(4, 128).T
        pp[l, :, 58:62] = np.asarray(inputs["r_k"][l]).reshape(4, 128).T
        pp[l, :, 62:64] = np.asarray(inputs["pool_scale"][l]).reshape(2, 128).T
    m["pp"] = pp
    for k in ("pool_w", "fourier_w", "w_out", "router_w", "exp_w_gate", "exp_w_up", "exp_w_down"):
        m[k] = f(inputs[k])
    m["final_norm_g"] = f(inputs["final_norm_g"]).reshape(1, D)
    m["psc"] = np.ascontiguousarray(f(inputs["pool_scale"]).reshape(DEPTH, 4, 64).transpose(0, 2, 1))
    return m


_NC_CACHE = {}


def kernel(**inputs):
    if "nc" not in _NC_CACHE:
        _NC_CACHE["nc"] = build()
    nc = _NC_CACHE["nc"]
    in_maps = [prep_inputs(inputs, b) for b in range(2)]
    res = run_bass_kernel_spmd(nc, in_maps, core_ids=[0, 1])
    return np.stack([res.results[b]["out"] for b in range(2)], 0)
```

```python
import numpy as np
from contextlib import ExitStack
import concourse.bass as bass
import concourse.mybir as mybir
from concourse.bass_utils import run_bass_kernel_spmd

F32 = mybir.dt.float32
BF16 = mybir.dt.bfloat16
I32 = mybir.dt.int32
AF = mybir.ActivationFunctionType
ALU = mybir.AluOpType
AX = mybir.AxisListType
ENGS = ("pe", "act", "dve", "pool", "sp")

D = 1024
T_LAT = 16384
T_CTX = 256
TT = T_LAT + T_CTX
NU = TT // 128
DEPTH = 2
CDEC = float(np.exp(-0.5))


class Buf:
    __slots__ = ("w", "r")

    def __init__(self):
        self.w = {}
        self.r = {}


class MBuf:
    def __init__(self):
        self.d = {}

    def __getitem__(self, k):
        b = self.d.get(k)
        if b is None:
            b = self.d[k] = Buf()
        return b


class RegRef:
    def __init__(self, val):
        self.val = val


class _Rec:
    def __getattr__(self, name):
        def f(*a, **k):
            self.call = (name, a, k)
            return self
        return f


class Sched:
    def __init__(self, nc, stack, n_dma_sems=(14, 4, 14)):
        self.nc = nc
        self.stack = stack
        self.ops = {e: [] for e in ENGS}
        self.sems = []
        self.cnt = []
        self.known = {e: {} for e in ENGS}
        self.esem = {}
        for e in ENGS:
            self.esem[e] = self._newsem("prog_" + e)
        self.dpool = {}
        self.dnext = {}
        for q, n in zip(("sp", "act", "pool"), n_dma_sems):
            self.dpool[q] = [self._newsem(f"dma_{q}_{i}") for i in range(n)]
            self.dnext[q] = 0
        self.n_instr = 0

    def _newsem(self, name):
        s = self.stack.enter_context(self.nc.semaphore(name))
        self.sems.append(s)
        self.cnt.append(0)
        return len(self.sems) - 1

    def barrier(self):
        for E in ENGS:
            kn = self.known[E]
            for k, c in enumerate(self.cnt):
                if E == "pe" and k == self.esem["pe"]:
                    continue
                if c > 0 and kn.get(k, 0) < c:
                    self.ops[E].append((1, self.sems[k], c))
                    kn[k] = c

    def scope(self):
        S = self

        class _Sc:
            def __enter__(s2):
                s2.old = S.stack
                s2.st = ExitStack()
                s2.st.__enter__()
                S.stack = s2.st
                return s2

            def __exit__(s2, *a):
                S.barrier()
                S.stack = s2.old
                return s2.st.__exit__(*a)
        return _Sc()

    def sb(self, name, shape, dtype):
        self.uid = getattr(self, "uid", 0) + 1
        return self.stack.enter_context(self.nc.sbuf_tensor(f"s{self.uid}_" + name, list(shape), dtype))

    def ps(self, name, shape, dtype):
        return self.stack.enter_context(self.nc.psum_tensor("p_" + name, list(shape), dtype))

    def _emit(self, E, fn, reads, writes, dma):
        deps = {}
        for b in reads:
            for k, v in b.w.items():
                if deps.get(k, 0) < v:
                    deps[k] = v
        for b in writes:
            for k, v in b.w.items():
                if deps.get(k, 0) < v:
                    deps[k] = v
            for k, v in b.r.items():
                if deps.get(k, 0) < v:
                    deps[k] = v
        if E == "pe":
            deps.pop(self.esem["pe"], None)
        if dma:
            pool = self.dpool[E]
            dk = pool[self.dnext[E] % len(pool)]
            self.dnext[E] += 1
            if self.cnt[dk] > 0 and deps.get(dk, 0) < self.cnt[dk]:
                deps[dk] = self.cnt[dk]
        rec = _Rec()
        fn(rec)
        fn = rec.call
        kn = self.known[E]
        ops = self.ops[E]
        for k, v in deps.items():
            if kn.get(k, 0) < v:
                kn[k] = v
                ops.append((1, self.sems[k], v))
                self.n_instr += 1
        if dma:
            self.cnt[dk] += 16
            ev = (dk, self.cnt[dk])
            ops.append((0, fn, self.sems[dk], 16))
        else:
            k = self.esem[E]
            self.cnt[k] += 1
            ev = (k, self.cnt[k])
            ops.append((0, fn, self.sems[k], 1))
        self.n_instr += 1
        for b in writes:
            b.w = {ev[0]: ev[1]}
            b.r = {}
        for b in reads:
            if b.r.get(ev[0], 0) < ev[1]:
                b.r[ev[0]] = ev[1]
        return ev

    def op(self, E, fn, reads=(), writes=()):
        return self._emit(E, fn, reads, writes, False)

    def dma(self, E, fn, reads=(), writes=()):
        return self._emit(E, fn, reads, writes, True)

    def finish(self):
        ops = self.ops["sp"]
        kn = self.known["sp"]
        for k, c in enumerate(self.cnt):
            if c > 0 and kn.get(k, 0) < c:
                ops.append((1, self.sems[k], c))
                kn[k] = c

    def run_block(self):
        with self.nc.Block() as block:
            def mk(E):
                def body(eng):
                    regs = {}
                    for o in self.ops[E]:
                        if o[0] == 1:
                            eng.wait_ge(o[1], o[2])
                        else:
                            nm, a, k = o[1]
                            if "bounds_check" in k and isinstance(k["bounds_check"], RegRef):
                                v_ = k["bounds_check"].val
                                if v_ not in regs:
                                    regs[v_] = eng.to_reg(v_)
                                k = dict(k)
                                k["bounds_check"] = regs[v_]
                            try:
                                ins = getattr(eng, nm)(*a, **k)
                            except Exception:
                                print("FAILED OP", E, nm, [str(x)[:200] for x in a], {kk: str(vv)[:300] for kk, vv in k.items()})
                                raise
                            ins.then_inc(o[2], o[3])
                return body

            block.tensor(mk("pe"))
            block.scalar(mk("act"))
            block.vector(mk("dve"))
            block.gpsimd(mk("pool"))
            block.sync(mk("sp"))


class Ring:
    def __init__(self, S, name, shape, dtype, n):
        self.t = [S.sb(f"{name}{i}", shape, dtype) for i in range(n)]
        self.b = [Buf() for _ in range(n)]
        self.i = 0

    def next(self):
        i = self.i % len(self.t)
        self.i += 1
        return self.t[i], self.b[i]


def host_consts():
    c = {}
    i = np.arange(128)
    c["ident"] = np.eye(128, dtype=np.float32)
    mu = (i[:, None] < i[None, :]).astype(np.float32)
    c["masks"] = np.stack([mu, mu.T, mu + np.eye(128, dtype=np.float32), mu.T + np.eye(128, dtype=np.float32)], 1).astype(np.float32)
    c["tri"] = np.stack([mu + np.eye(128), mu, mu.T + np.eye(128), mu.T], 1).astype(np.float32)
    def bd(n):
        g = i // n
        return (g[:, None] == g[None, :]).astype(np.float32)
    ml = mu.T
    hm = [ml * bd(16), ml * (bd(32) - bd(16)), ml * (bd(64) - bd(32)), ml * (1 - bd(64))]
    c["hmask"] = np.stack(hm + [m_.T for m_ in hm], 1).astype(np.float32)
    bo = np.zeros((128, 128), np.float32)
    bo[:64, :64] = 1
    bo[64:, 64:] = 1
    c["blockones"] = bo
    ind = np.zeros((128, 4, 8), np.float32)
    for cc in range(4):
        ind[:64, cc, 2 * cc] = 1
        ind[64:, cc, 2 * cc + 1] = 1
    c["ind8"] = ind
    wins = (2, 4, 8, 16)
    def invc(n, w):
        pos = np.arange(n)
        return 1.0 / (np.minimum(pos + w // 2, n) - np.maximum(pos - w // 2, 0)).astype(np.float64)
    c["ic_lat"] = np.stack([np.outer(invc(256, w), invc(64, w)) for w in wins], 0).astype(np.float32)
    c["ic_ctx"] = np.stack([invc(256, w)[None, :] for w in wins], 0).astype(np.float32)
    t = np.arange(128)
    ang = 2 * np.pi * np.outer(t, t) / 128.0
    c["dft128"] = np.stack([np.cos(ang), np.sin(ang), -np.sin(ang)], 1).astype(np.float32)
    angt = 2 * np.pi * np.outer(t, t) / 16384.0
    c["twid"] = np.stack([np.cos(angt), np.sin(angt)], 1).astype(np.float32)
    t2 = np.arange(256)
    a256 = 2 * np.pi * np.outer(t2, t2) / 256.0
    c["dft256"] = np.stack([np.cos(a256).reshape(2, 128, 256), np.sin(a256).reshape(2, 128, 256)], 2).transpose(1, 0, 2, 3).astype(np.float32).copy()
    q = np.arange(64)
    a64 = 2 * np.pi * np.outer(q, q) / 64.0
    pad = np.zeros((64, 2, 2, 128), np.float64)
    for ci, m_ in enumerate((np.cos(a64), np.sin(a64))):
        pad[:, ci, 0, 0:64] = m_ / 1024.0
        pad[:, ci, 1, 64:128] = m_ / 1024.0
    c["dft64pad"] = pad.astype(np.float32)
    return c


def build(debug=()):
    nc = bass.Bass("TRN2", target_bir_lowering=False)
    dbg = set(debug)

    def dram_in(name, shape, dt=F32):
        return nc.dram_tensor(name, list(shape), dt, kind="ExternalInput").ap()

    def dram(name, shape, dt=F32):
        kind = "ExternalOutput" if name in dbg else ("ExternalInput" if ("in:" + name) in dbg else "Internal")
        return nc.dram_tensor(name, list(shape), dt, kind=kind).ap()

    x = dram_in("x", [T_LAT, D])
    ctx = dram_in("ctx", [T_CTX, D])
    ccond = dram_in("ccond", [128, 16])
    ada_w = dram_in("ada_w", [DEPTH, D, 6 * D])
    ada_b = dram_in("ada_b", [DEPTH, 6 * D])
    norm_mix_g = dram_in("norm_mix_g", [DEPTH, D])
    norm_ffn_g = dram_in("norm_ffn_g", [DEPTH, D])
    w_in = dram_in("w_in", [DEPTH, D, 2304])
    pp = dram_in("pp", [DEPTH, 128, 64])
    decay_w0 = dram_in("decay_w0", [DEPTH, 2, 512])
    decay_up = dram_in("decay_up", [DEPTH, 2, 64, 512])
    iclr_up = dram_in("iclr_up", [DEPTH, 2, 64, 512])
    gate_up = dram_in("gate_up", [DEPTH, 128, 512])
    lnx_g = dram_in("lnx_g", [DEPTH, 512])
    lnx_b = dram_in("lnx_b", [DEPTH, 512])
    c_ident = dram_in("ident", [128, 128])
    c_masks = dram_in("masks", [128, 4, 128])
    c_tri = dram_in("tri", [128, 4, 128])
    c_bo = dram_in("blockones", [128, 128])
    c_hmask = dram_in("hmask", [128, 8, 128])
    c_ind8 = dram_in("ind8", [128, 4, 8])

    pool_w = dram_in("pool_w", [DEPTH, 4, 64, 64])
    psc = dram_in("psc", [DEPTH, 64, 4])
    fourier_w = dram_in("fourier_w", [DEPTH, 4, 64, 64])
    w_out = dram_in("w_out", [DEPTH, D, D])
    router_w = dram_in("router_w", [DEPTH, D, 16])
    exp_w_gate = dram_in("exp_w_gate", [DEPTH, 16, D, D])
    exp_w_up = dram_in("exp_w_up", [DEPTH, 16, D, D])
    exp_w_down = dram_in("exp_w_down", [DEPTH, 16, D, D])
    final_norm_g = dram_in("final_norm_g", [1, D])
    c_ic_lat = dram_in("ic_lat", [4, 256, 64])
    c_ic_ctx = dram_in("ic_ctx", [4, 1, 256])
    c_dft128 = dram_in("dft128", [128, 3, 128])
    c_twid = dram_in("twid", [128, 2, 128])
    c_dft256 = dram_in("dft256", [128, 2, 2, 256])
    c_dft64pad = dram_in("dft64pad", [64, 2, 2, 128])

    out = nc.dram_tensor("out", [T_LAT, D], F32, kind="ExternalOutput").ap()
    HW_ = 1048
    H2d = dram("H2d", [TT, HW_])
    XEd = [dram(f"XE{e_}", [2080, HW_]) for e_ in range(16)]
    MOEd = dram("MOEd", [TT, D])
    BIGSLOT = 1.0e6
    FXd = dram("FXd", [2, 256, T_LAT])

    HT = dram("HT", [TT, D])
    MODd = dram("MODd", [2, 6 * D])
    PTd = dram("PTd", [2048, TT])
    PFd = dram("PFd", [TT, 256])
    Yd = [dram(f"Y{d}", [TT, 512]) for d in range(2)]
    VTd = dram("VTd", [TT, 512], BF16)
    GTd = dram("GTd", [TT, 512])
    BONd = [dram(f"BON{d}", [TT, 8]) for d in range(2)]
    CATT = dram("CATT", [1024, TT], BF16)

    with ExitStack() as st:
        S = Sched(nc, st)
        banks = [S.ps(f"bank{i}", [128, 512], F32) for i in range(8)]
        bankb = [Buf() for _ in range(8)]
        pctr = [0]

        def psum():
            i = pctr[0] % 7
            pctr[0] += 1
            return banks[i], bankb[i]

        tog = [0]

        def evE():
            tog[0] ^= 1
            return "dve" if tog[0] else "act"

        def copy_ev(E, out_ap, in_ap, reads, writes):
            if E == "act":
                S.op("act", lambda e: e.copy(out=out_ap, in_=in_ap), reads, writes)
            else:
                S.op(E, lambda e: e.tensor_copy(out=out_ap, in_=in_ap), reads, writes)

        ident = S.sb("ident", [128, 128], F32)
        identb = S.sb("identb", [128, 128], BF16)
        masks = S.sb("masks", [128, 4, 128], F32)
        tri = S.sb("tri", [128, 4, 128], F32)
        blockones = S.sb("blockones", [128, 128], F32)
        ind8 = S.sb("ind8", [128, 4, 8], F32)
        ones_row = S.sb("ones_row", [1, 128], F32)
        hmask = S.sb("hmask", [128, 8, 128], F32)
        CB = Buf()
        for t, src in ((ident, c_ident), (masks, c_masks), (tri, c_tri), (blockones, c_bo), (ind8, c_ind8), (hmask, c_hmask)):
            S.dma("sp", lambda e, t=t, src=src: e.dma_start(out=t[:], in_=src), [], [CB])
        S.op("dve", lambda e: e.tensor_copy(out=identb[:], in_=ident[:]), [CB], [CB])
        S.op("dve", lambda e: e.memset(ones_row[:], 1.0), [], [CB])

        HTb = MBuf()
        S.dma("sp", lambda e: e.dma_start(out=HT[0:T_CTX, :], in_=ctx), [], [HTb[0], HTb[1]])
        for i in range(16):
            S.dma("sp", lambda e, i=i: e.dma_start(out=HT[T_CTX + i * 1024:T_CTX + (i + 1) * 1024, :],
                                                   in_=x[i * 1024:(i + 1) * 1024, :]),
                  [], [HTb[2 + i * 8 + j] for j in range(8)])

        cc_sb = S.sb("cc_sb", [128, 16], F32)
        silu_c = S.sb("silu_c", [128, 16], F32)
        SCB = Buf()
        S.dma("sp", lambda e: e.dma_start(out=cc_sb[:], in_=ccond), [], [SCB])
        S.op("act", lambda e: e.activation(out=silu_c[:], in_=cc_sb[:], func=AF.Silu), [SCB], [SCB])

        MODb = Buf()

        def stage_adaln(l):
            wr = Ring(S, f"adaw{l}_", [128, 8, 512], F32, 2)
            row = S.sb(f"modrow{l}", [2, 6 * D], F32)
            brow = S.sb(f"adab{l}", [2, 6 * D], F32)
            RB = Buf()
            for j in range(2):
                S.dma("sp", lambda e, j=j: e.dma_start(out=brow[j:j + 1, :], in_=ada_b[l:l + 1, :]), [], [RB])
            for n in range(12):
                wt, wb = wr.next()
                S.dma("sp" if n % 2 else "act",
                      lambda e, wt=wt, n=n: e.dma_start(
                          out=wt[:], in_=ada_w[l, :, n * 512:(n + 1) * 512].rearrange("(kc p) n -> p kc n", p=128)),
                      [], [wb])
                pt, pb = psum()
                for kc in range(8):
                    S.op("pe", lambda e, pt=pt, wt=wt, kc=kc: e.matmul(
                        pt[0:2, :], lhsT=silu_c[:, 2 * kc:2 * kc + 2],
                        rhs=wt[:, kc, :], start=(kc == 0), stop=(kc == 7)), [SCB, wb], [pb])
                S.op("dve", lambda e, pt=pt, n=n: e.tensor_tensor(
                    out=row[:, n * 512:(n + 1) * 512], in0=pt[0:2, :], in1=brow[:, n * 512:(n + 1) * 512], op=ALU.add),
                    [pb, RB], [RB])
            S.dma("sp", lambda e: e.dma_start(out=MODd, in_=row[:]), [RB], [MODb])

        def bload(tile_ap, src_row_ap, wbuf, rbufs=()):
            S.dma("sp", lambda e: e.dma_start(out=tile_ap, in_=src_row_ap.to_broadcast([128, src_row_ap.shape[-1]])),
                  list(rbufs), [wbuf])

        PTb = MBuf()
        PFb = MBuf()

        def stage_A(l):
            win = S.sb(f"win", [128, 8, 2304], BF16)
            WB = Buf()
            for kc in range(8):
                S.dma("pool", lambda e, kc=kc: e.dma_start(out=win[:, kc, :], in_=w_in[l, kc * 128:(kc + 1) * 128, :],
                                                            max_dma_last_dim=2048 * 4), [], [WB])
            Gm = [S.sb(f"Gm{j}", [128, D], F32) for j in range(2)]
            SHm = [S.sb(f"SHm{j}", [128, D], F32) for j in range(2)]
            gt = S.sb("gtmp", [128, D], F32)
            GB = Buf()
            bload(gt[:], norm_mix_g[l:l + 1, :], GB)
            for j, rowi in ((0, 1), (1, 0)):
                bload(Gm[j][:], MODd[rowi:rowi + 1, D:2 * D], GB, [MODb])
                bload(SHm[j][:], MODd[rowi:rowi + 1, 0:D], GB, [MODb])
                S.op("dve", lambda e, j=j: e.scalar_tensor_tensor(out=Gm[j][:], in0=Gm[j][:], scalar=1.0, in1=gt[:],
                                                                 op0=ALU.add, op1=ALU.mult), [GB], [GB])
            xr = Ring(S, "xin", [128, D], F32, 2)
            xm = Ring(S, "xmod", [128, D], F32, 2)
            sq = S.sb("sqjunk", [128, D], F32)
            SQB = Buf()
            st_ = Ring(S, "stat", [128, 2], F32, 4)
            xnT = Ring(S, "xnT", [128, 8, 512], BF16, 2)
            stg = Ring(S, "stgA", [128, 512], F32, 4)
            stf = Ring(S, "stgF", [128, 256], F32, 2)
            blocks = [(0, 256)] + [(256 + 512 * i, 512) for i in range(32)]
            for (t0, nt) in blocks:
                j = 0 if t0 < 256 else 1
                xt_, xtb = xnT.next()
                for s in range(nt // 128):
                    u = (t0 // 128) + s
                    xi, xib = xr.next()
                    S.dma("sp", lambda e, xi=xi, u=u: e.dma_start(out=xi[:], in_=HT[u * 128:(u + 1) * 128, :]), [HTb[u]], [xib])
                    sc, scb = st_.next()
                    S.op("act", lambda e, xi=xi, sc=sc: e.activation(out=sq[:], in_=xi[:], func=AF.Square, accum_out=sc[:, 0:1]),
                         [xib], [SQB, scb])
                    S.op("act", lambda e, sc=sc: e.activation(out=sc[:, 1:2], in_=sc[:, 0:1], func=AF.Sqrt, scale=1.0 / D, bias=1e-6),
                         [scb], [scb])
                    S.op("dve", lambda e, sc=sc: e.reciprocal(out=sc[:, 1:2], in_=sc[:, 1:2]), [scb], [scb])
                    xo, xob = xm.next()
                    S.op("dve", lambda e, xo=xo, xi=xi, sc=sc, j=j: e.scalar_tensor_tensor(
                        out=xo[:], in0=xi[:], scalar=sc[:, 1:2], in1=Gm[j][:], op0=ALU.mult, op1=ALU.mult), [xib, scb, GB], [xob])
                    S.op("pool", lambda e, xo=xo, j=j: e.tensor_tensor(out=xo[:], in0=xo[:], in1=SHm[j][:], op=ALU.add), [xob, GB], [xob])
                    for half in range(2):
                        pt, pb = psum()
                        for q in range(4):
                            kc = half * 4 + q
                            S.op("pe", lambda e, pt=pt, xo=xo, kc=kc, q=q: e.transpose(
                                pt[:, q * 128:(q + 1) * 128], xo[:, kc * 128:(kc + 1) * 128], ident[:]), [xob, CB], [pb])
                        copy_ev(evE(), xt_[:, half * 4:(half + 1) * 4, s * 128:(s + 1) * 128],
                                pt[:, :].rearrange("p (q t) -> p q t", q=4), [pb], [xtb])
                for cchunk in range(16):
                    pt, pb = psum()
                    for kc in range(8):
                        S.op("pe", lambda e, pt=pt, kc=kc, cchunk=cchunk, xt_=xt_, nt=nt: e.matmul(
                            pt[:, 0:nt], lhsT=win[:, kc, cchunk * 128:(cchunk + 1) * 128], rhs=xt_[:, kc, 0:nt],
                            start=(kc == 0), stop=(kc == 7)), [WB, xtb], [pb])
                    sg, sgb = stg.next()
                    copy_ev(evE(), sg[:, 0:nt], pt[:, 0:nt], [pb], [sgb])
                    S.dma("sp", lambda e, sg=sg, cchunk=cchunk, t0=t0, nt=nt: e.dma_start(
                        out=PTd[cchunk * 128:(cchunk + 1) * 128, t0:t0 + nt], in_=sg[:, 0:nt]),
                        [sgb], [PTb[(cchunk, t0 // 512 if t0 >= 256 else -1)]])
                for s in range(nt // 128):
                    u = (t0 // 128) + s
                    pt, pb = psum()
                    for kc in range(8):
                        S.op("pe", lambda e, pt=pt, kc=kc, xt_=xt_, s=s: e.matmul(
                            pt[:, 0:256], lhsT=xt_[:, kc, s * 128:(s + 1) * 128], rhs=win[:, kc, 2048:2304],
                            start=(kc == 0), stop=(kc == 7)), [WB, xtb], [pb])
                    sf, sfb = stf.next()
                    copy_ev(evE(), sf[:], pt[:, 0:256], [pb], [sfb])
                    S.dma("sp", lambda e, sf=sf, u=u: e.dma_start(out=PFd[u * 128:(u + 1) * 128, :], in_=sf[:]), [sfb], [PFb[u]])


        Yb = [MBuf(), MBuf()]
        VTb = MBuf()
        GTb = MBuf()
        BONb = [MBuf(), MBuf()]
        CATb = MBuf()

        def seq_of(t0):
            return (0, 256) if t0 < 256 else (256, TT)

        def stage_rwkv(l, need_y_ctx=True):
            ppt = S.sb("ppt", [128, 64], F32)
            omka = S.sb("omka", [128, 4], F32)
            wup = [S.sb(f"wup{d}", [128, 512], F32) for d in range(2)]
            w0row = [S.sb(f"w0row{d}", [1, 512], F32) for d in range(2)]
            gateup = S.sb("gateup", [128, 512], F32)
            PB = Buf()
            S.dma("sp", lambda e: e.dma_start(out=ppt[:], in_=pp[l]), [], [PB])
            for d in range(2):
                S.dma("sp", lambda e, d=d: e.dma_start(out=wup[d][0:64, :], in_=decay_up[l, d]), [], [PB])
                S.dma("sp", lambda e, d=d: e.dma_start(out=wup[d][64:128, :], in_=iclr_up[l, d]), [], [PB])
                S.dma("sp", lambda e, d=d: e.dma_start(out=w0row[d][:], in_=decay_w0[l, d:d + 1, :]), [], [PB])
            S.dma("sp", lambda e: e.dma_start(out=gateup[:], in_=gate_up[l]), [], [PB])
            S.op("dve", lambda e: e.tensor_scalar(out=omka[:], in0=ppt[:, 54:58], scalar1=-1.0, scalar2=1.0,
                                                  op0=ALU.mult, op1=ALU.add), [PB], [PB])
            pin = Ring(S, "pin", [128, 514], F32, 2)
            Ut = [S.sb(f"U{c}", [128, 512], F32) for c in range(14)]
            Ub = [Buf() for _ in range(14)]
            th = S.sb("th", [64, 512], F32)
            THB = Buf()
            sigx = S.sb("sigx", [128, 512], F32)
            SXB = Buf()
            sigt = Ring(S, "sigt", [128, 512], F32, 4)
            tmp = Ring(S, "tmpf", [128, 512], F32, 6)
            Gr = Ring(S, "G", [128, 4, 512], F32, 2)
            GLr = Ring(S, "GL", [128, 4, 4], F32, 2)
            negb = Ring(S, "negb", [128, 4], F32, 2)
            Atr = Ring(S, "At", [128, 4, 512], BF16, 1)
            Btr = Ring(S, "Bt", [128, 4, 512], BF16, 1)
            Ktr = Ring(S, "Kt", [128, 4, 512], BF16, 1)
            Rtr = Ring(S, "Rt", [128, 4, 512], BF16, 1)
            BhTr = Ring(S, "BhT", [128, 4, 512], BF16, 1)
            KhTr = Ring(S, "KhT", [128, 4, 512], BF16, 1)
            Vtr = Ring(S, "Vtok", [128, 4, 512], BF16, 1)
            AtM = [S.sb(f"AtM{i}", [128, 4, 512], BF16) for i in range(2)]
            BtM = [S.sb(f"BtM{i}", [128, 4, 512], BF16) for i in range(2)]
            RtM = [S.sb(f"RtM{i}", [128, 4, 512], BF16) for i in range(2)]
            MKB_ = Buf()
            hb = Ring(S, "hb", [128, 512], BF16, 3)
            bonst = Ring(S, "bonst", [128, 32], F32, 2)
            gst = Ring(S, "gst", [128, 512], F32, 1)
            PTr = Ring(S, "PTs", [128, 4, 128], BF16, 3)
            Pr = Ring(S, "Ps", [128, 4, 128], BF16, 3)
            TTr = Ring(S, "TTs", [128, 4, 128], BF16, 3)
            TTall = Ring(S, "TTall", [128, 8, 128], BF16, 2)
            TNr = Ring(S, "TNs", [128, 4, 128], BF16, 3)
            Cr = Ring(S, "Cs", [128, 4, 128], BF16, 6)
            Zr = Ring(S, "Zs", [128, 4, 128], BF16, 3)
            Makr = Ring(S, "Mak", [128, 8, 128], BF16, 2)
            Mrbr = Ring(S, "Mrb", [128, 8, 128], BF16, 1)
            Mrkr = Ring(S, "Mrk", [128, 8, 128], BF16, 1)
            Xsr = Ring(S, "Xs", [128, 512], BF16, 2)
            Usr = Ring(S, "Us", [128, 512], BF16, 2)
            Ysr = Ring(S, "Ys", [128, 512], F32, 1)
            Sf = S.sb("Sf", [128, 4, 128], F32)
            Sbf = S.sb("Sbf", [128, 4, 128], BF16)
            Stmp = S.sb("Stmp", [128, 4, 128], F32)
            STB = Buf()
            SFB = Buf()
            SBB = Buf()

            blocks = [(0, 256)] + [(256 + 512 * i, 512) for i in range(32)]
            if "short" in dbg:
                blocks = blocks[:3]
            if "blocks1" in dbg:
                blocks = blocks[:1]
            for d in range(1 if "d0" in dbg else 2):
                order = blocks if d == 0 else [blocks[0]] + blocks[:0:-1]
                mS, mSt, mI = (0, 1, 2) if d == 0 else (1, 0, 3)
                S.op("dve", lambda e: e.memset(Sf[:], 0.0), [], [SFB])
                S.op("dve", lambda e: e.memset(Sbf[:], 0.0), [], [SBB])
                for (t0, nt) in order:
                    nun = nt // 128
                    s_lo, s_hi = seq_of(t0)
                    chunks = list(range(0, 8)) + [12] + ([8, 9, 10, 11, 13] if d == 0 else [])
                    for c in chunks:
                        pi, pib = pin.next()
                        lo = t0 - 1
                        hi = t0 + nt + 1
                        a = max(lo, s_lo)
                        bnd = min(hi, s_hi)
                        if a > lo:
                            S.op("pool", lambda e, pi=pi: e.memset(pi[:, 0:1], 0.0), [], [pib])
                        if bnd < hi:
                            S.op("pool", lambda e, pi=pi, nt=nt: e.memset(pi[:, nt + 1:nt + 2], 0.0), [], [pib])
                        S.dma("sp", lambda e, pi=pi, c=c, a=a, bnd=bnd, lo=lo: e.dma_start(
                            out=pi[:, a - lo:bnd - lo], in_=PTd[c * 128:(c + 1) * 128, a:bnd]),
                            [PTb[(c, k)] for k in range(-1, 32)], [pib])
                        eng = "dve"
                        S.op(eng, lambda e, pi=pi, c=c, nt=nt: e.tensor_scalar(
                            out=Ut[c][:, 0:nt], in0=pi[:, 1:nt + 1], scalar1=ppt[:, 3 * c + 1:3 * c + 2], scalar2=None, op0=ALU.mult),
                            [pib, PB], [Ub[c]])
                        S.op(eng, lambda e, pi=pi, c=c, nt=nt: e.scalar_tensor_tensor(
                            out=Ut[c][:, 0:nt], in0=pi[:, 0:nt], scalar=ppt[:, 3 * c:3 * c + 1], in1=Ut[c][:, 0:nt],
                            op0=ALU.mult, op1=ALU.add), [pib, PB, Ub[c]], [Ub[c]])
                        S.op(eng, lambda e, pi=pi, c=c, nt=nt: e.scalar_tensor_tensor(
                            out=Ut[c][:, 0:nt], in0=pi[:, 2:nt + 2], scalar=ppt[:, 3 * c + 2:3 * c + 3], in1=Ut[c][:, 0:nt],
                            op0=ALU.mult, op1=ALU.add), [pib, PB, Ub[c]], [Ub[c]])
                    S.op("act", lambda e, nt=nt: e.activation(out=th[:, 0:nt], in_=Ut[12][0:64, 0:nt], func=AF.Tanh), [Ub[12]], [THB])
                    sgs = []
                    for q in range(nun):
                        pt, pb = psum()
                        S.op("pe", lambda e, pt=pt, q=q: e.matmul(pt[:, :], lhsT=th[:, q * 128:(q + 1) * 128], rhs=wup[d][0:64, :],
                                                                   start=True, stop=False), [THB, PB], [pb])
                        S.op("pe", lambda e, pt=pt: e.matmul(pt[:, :], lhsT=ones_row[:, :], rhs=w0row[d][:, :], start=False, stop=True),
                             [CB, PB], [pb])
                        sg, sgb = sigt.next()
                        S.op("act", lambda e, sg=sg, pt=pt: e.activation(out=sg[:], in_=pt[:, :], func=AF.Sigmoid), [pb], [sgb])
                        sgs.append((sg, sgb))
                    G, Gb = Gr.next()
                    GL, GLb = GLr.next()
                    At, Atb = Atr.next()
                    Bt, Btb = Btr.next()
                    Kt, Ktb = Ktr.next()
                    Rt, Rtb = Rtr.next()
                    BhT, BhTb = BhTr.next()
                    KhT, KhTb = KhTr.next()
                    Vt, Vtb = Vtr.next()
                    if d == 0:
                        S.op("act", lambda e, nt=nt: e.activation(out=sigx[:, 0:nt], in_=Ut[13][:, 0:nt], func=AF.Sigmoid), [Ub[13]], [SXB])
                        for q in range(nun):
                            u = t0 // 128 + q
                            pt, pb = psum()
                            S.op("pe", lambda e, pt=pt, q=q: e.matmul(pt[:, :], lhsT=sigx[:, q * 128:(q + 1) * 128], rhs=gateup[:, :],
                                                                       start=True, stop=True), [SXB, PB], [pb])
                            g_, g_b = gst.next()
                            copy_ev(evE(), g_[:], pt[:, :], [pb], [g_b])
                            S.dma("sp", lambda e, g_=g_, u=u: e.dma_start(out=GTd[u * 128:(u + 1) * 128, :], in_=g_[:]), [g_b], [GTb[u]])
                    else:
                        for q in range(nun):
                            u = t0 // 128 + q
                            S.dma("sp", lambda e, q=q, u=u, Vt=Vt: e.dma_start(out=Vt[:, q, :], in_=VTd[u * 128:(u + 1) * 128, :]),
                                  [VTb[u]], [Vtb])
                    bon_p, bon_pb = banks[7], bankb[7]
                    for cc in range(4):
                        ci, cib = psum()
                        ce, ceb = psum()
                        for q in range(nun):
                            sg, sgb = sgs[q]
                            S.op("pe", lambda e, ci=ci, sg=sg, q=q, cc=cc: e.matmul(
                                ci[:, q * 128:(q + 1) * 128], lhsT=sg[:, cc * 128:(cc + 1) * 128], rhs=tri[:, 2 * d, :],
                                start=True, stop=True), [sgb, CB], [cib])
                            S.op("pe", lambda e, ce=ce, sg=sg, q=q, cc=cc: e.matmul(
                                ce[:, q * 128:(q + 1) * 128], lhsT=sg[:, cc * 128:(cc + 1) * 128], rhs=tri[:, 2 * d + 1, :],
                                start=True, stop=True), [sgb, CB], [ceb])
                        nb_, nbb = negb.next()
                        ecol = 127 if d == 0 else 0
                        S.op("dve", lambda e, nb_=nb_, ci=ci, nun=nun, ecol=ecol: e.tensor_scalar(
                            out=nb_[:, 0:nun], in0=ci[:, ecol:ecol + 128 * (nun - 1) + 1:128], scalar1=-CDEC, scalar2=None, op0=ALU.mult),
                            [cib], [nbb])
                        Ginv, Ginvb = tmp.next()
                        Gx, Gxb = tmp.next()
                        Gh, Ghb = tmp.next()
                        S.op("act", lambda e, ci=ci, cc=cc, nt=nt, G=G: e.activation(out=G[:, cc, 0:nt], in_=ci[:, 0:nt], func=AF.Exp, scale=-CDEC), [cib], [Gb])
                        S.op("act", lambda e, ci=ci, nt=nt, Ginv=Ginv: e.activation(out=Ginv[:, 0:nt], in_=ci[:, 0:nt], func=AF.Exp, scale=CDEC), [cib], [Ginvb])
                        S.op("act", lambda e, ce=ce, nt=nt, Gx=Gx: e.activation(out=Gx[:, 0:nt], in_=ce[:, 0:nt], func=AF.Exp, scale=-CDEC), [ceb], [Gxb])
                        for q in range(nun):
                            S.op("act", lambda e, ci=ci, q=q, Gh=Gh, nb_=nb_: e.activation(
                                out=Gh[:, q * 128:(q + 1) * 128], in_=ci[:, q * 128:(q + 1) * 128], func=AF.Exp, scale=CDEC,
                                bias=nb_[:, q:q + 1]), [cib, nbb], [Ghb])
                        S.op("pool", lambda e, GL=GL, G=G, cc=cc, nun=nun, ecol=ecol: e.tensor_copy(
                            out=GL[:, cc, 0:nun], in_=G[:, cc, ecol:ecol + 128 * (nun - 1) + 1:128]), [Gb], [GLb])
                        pa, pab = psum()
                        S.op("pe", lambda e, pa=pa, cc=cc, nt=nt: e.matmul(pa[:, 0:nt], lhsT=wup[d][64:128, cc * 128:(cc + 1) * 128],
                                                                          rhs=Ut[12][64:128, 0:nt], start=True, stop=True), [PB, Ub[12]], [pab])
                        alr, alrb = tmp.next()
                        S.op("act", lambda e, alr=alr, pa=pa, nt=nt, cc=cc: e.activation(
                            out=alr[:, 0:nt], in_=pa[:, 0:nt], func=AF.Sigmoid, bias=ppt[:, 42 + 4 * d + cc:43 + 4 * d + cc]), [pab, PB], [alrb])
                        kx, kxb = tmp.next()
                        sq_, sqb = tmp.next()
                        S.op("dve", lambda e, kx=kx, cc=cc, nt=nt: e.tensor_scalar(
                            out=kx[:, 0:nt], in0=Ut[4 + cc][:, 0:nt], scalar1=ppt[:, 50 + cc:51 + cc], scalar2=None, op0=ALU.mult), [Ub[4 + cc], PB], [kxb])
                        S.op("pool", lambda e, kx=kx, sq_=sq_, nt=nt: e.tensor_tensor(out=sq_[:, 0:nt], in0=kx[:, 0:nt], in1=kx[:, 0:nt], op=ALU.mult), [kxb], [sqb])
                        pss, pssb = psum()
                        S.op("pe", lambda e, pss=pss, sq_=sq_, nt=nt: e.matmul(pss[:, 0:nt], lhsT=blockones[:, :], rhs=sq_[:, 0:nt], start=True, stop=True),
                             [CB, sqb], [pssb])
                        S.op("act", lambda e, sq_=sq_, pss=pss, nt=nt: e.activation(out=sq_[:, 0:nt], in_=pss[:, 0:nt], func=AF.Sqrt, bias=1e-12), [pssb], [sqb])
                        S.op("dve", lambda e, sq_=sq_, nt=nt: e.reciprocal(out=sq_[:, 0:nt], in_=sq_[:, 0:nt]), [sqb], [sqb])
                        S.op("dve", lambda e, kx=kx, sq_=sq_, nt=nt: e.tensor_tensor(out=kx[:, 0:nt], in0=kx[:, 0:nt], in1=sq_[:, 0:nt], op=ALU.mult), [kxb, sqb], [kxb])
                        S.op("dve", lambda e, At=At, kx=kx, Gx=Gx, cc=cc, nt=nt: e.scalar_tensor_tensor(
                            out=At[:, cc, 0:nt], in0=kx[:, 0:nt], scalar=-1.0, in1=Gx[:, 0:nt], op0=ALU.mult, op1=ALU.mult), [kxb, Gxb], [Atb])
                        S.op("pool", lambda e, sq_=sq_, kx=kx, alr=alr, nt=nt: e.tensor_tensor(out=sq_[:, 0:nt], in0=kx[:, 0:nt], in1=alr[:, 0:nt], op=ALU.mult),
                             [kxb, alrb], [sqb])
                        S.op("dve", lambda e, Bt=Bt, sq_=sq_, Ginv=Ginv, cc=cc, nt=nt: e.tensor_tensor(
                            out=Bt[:, cc, 0:nt], in0=sq_[:, 0:nt], in1=Ginv[:, 0:nt], op=ALU.mult), [sqb, Ginvb], [Btb])
                        bh, bhb = hb.next()
                        S.op("pool", lambda e, bh=bh, sq_=sq_, Gh=Gh, nt=nt: e.tensor_tensor(out=bh[:, 0:nt], in0=sq_[:, 0:nt], in1=Gh[:, 0:nt], op=ALU.mult),
                             [sqb, Ghb], [bhb])
                        S.op("dve", lambda e, alr=alr, cc=cc, nt=nt: e.tensor_scalar(
                            out=alr[:, 0:nt], in0=alr[:, 0:nt], scalar1=ppt[:, 54 + cc:55 + cc], scalar2=omka[:, cc:cc + 1], op0=ALU.mult, op1=ALU.add),
                            [alrb, PB], [alrb])
                        S.op("dve", lambda e, alr=alr, cc=cc, nt=nt: e.tensor_tensor(out=alr[:, 0:nt], in0=alr[:, 0:nt], in1=Ut[4 + cc][:, 0:nt], op=ALU.mult),
                             [alrb, Ub[4 + cc]], [alrb])
                        S.op("dve", lambda e, Kt=Kt, alr=alr, Ginv=Ginv, cc=cc, nt=nt: e.tensor_tensor(
                            out=Kt[:, cc, 0:nt], in0=alr[:, 0:nt], in1=Ginv[:, 0:nt], op=ALU.mult), [alrb, Ginvb], [Ktb])
                        kh, khb = hb.next()
                        S.op("pool", lambda e, kh=kh, alr=alr, Gh=Gh, nt=nt: e.tensor_tensor(out=kh[:, 0:nt], in0=alr[:, 0:nt], in1=Gh[:, 0:nt], op=ALU.mult),
                             [alrb, Ghb], [khb])
                        S.op("pool", lambda e, Rt=Rt, G=G, cc=cc, nt=nt: e.tensor_tensor(out=Rt[:, cc, 0:nt], in0=Ut[cc][:, 0:nt], in1=G[:, cc, 0:nt], op=ALU.mult),
                             [Ub[cc], Gb], [Rtb])
                        for hp_ in range(2):
                            pmc = blockones[:, 64 * hp_:64 * hp_ + 1]
                            for (src_, srcb_, dst_) in ((At, Atb, AtM), (Bt, Btb, BtM), (Rt, Rtb, RtM)):
                                S.op("dve", lambda e: e.tensor_scalar(out=dst_[hp_][:, cc, 0:nt], in0=src_[:, cc, 0:nt], scalar1=pmc, scalar2=None, op0=ALU.mult),
                                     [srcb_, CB], [MKB_])
                        S.op("dve", lambda e, kx=kx, alr=alr, cc=cc, nt=nt: e.scalar_tensor_tensor(
                            out=kx[:, 0:nt], in0=Ut[cc][:, 0:nt], scalar=ppt[:, 58 + cc:59 + cc], in1=alr[:, 0:nt], op0=ALU.mult, op1=ALU.mult),
                            [Ub[cc], alrb, PB], [kxb])
                        for q in range(nun):
                            S.op("pe", lambda e, bon_p=bon_p, kx=kx, q=q, cc=cc: e.matmul(
                                bon_p[:, q * 8 + 2 * cc:q * 8 + 2 * cc + 2], lhsT=kx[:, q * 128:(q + 1) * 128], rhs=ind8[:, 0, 0:2], start=True, stop=True),
                                [kxb, CB], [bon_pb])
                        srcs = [(bh, bhb, BhT, BhTb), (kh, khb, KhT, KhTb)]
                        if d == 0:
                            vh, vhb = hb.next()
                            S.op("act", lambda e, vh=vh, cc=cc, nt=nt: e.copy(out=vh[:, 0:nt], in_=Ut[8 + cc][:, 0:nt]), [Ub[8 + cc]], [vhb])
                            srcs.append((vh, vhb, Vt, Vtb))
                        for (src, srcb, dst, dstb) in srcs:
                            ptb_, ptbb = psum()
                            ptv = ptb_[:, :].bitcast(BF16)
                            for q in range(nun):
                                S.op("pe", lambda e, ptv=ptv, src=src, q=q: e.transpose(
                                    ptv[:, q * 128:(q + 1) * 128], src[:, q * 128:(q + 1) * 128], identb[:]), [srcb, CB], [ptbb])
                            copy_ev(evE(), dst[:, 0:nun, cc * 128:(cc + 1) * 128], ptv[:, 0:nun * 128].rearrange("p (q t) -> p q t", q=nun), [ptbb], [dstb])
                    bs, bsb = bonst.next()
                    copy_ev("dve", bs[:, 0:8 * nun], bon_p[:, 0:8 * nun], [bon_pb], [bsb])
                    for q in range(nun):
                        u = t0 // 128 + q
                        S.dma("sp", lambda e, bs=bs, q=q, u=u: e.dma_start(out=BONd[d][u * 128:(u + 1) * 128, :], in_=bs[:, q * 8:(q + 1) * 8]),
                              [bsb], [BONb[d][u]])
                        if d == 0:
                            S.dma("sp", lambda e, q=q, u=u, Vt=Vt: e.dma_start(out=VTd[u * 128:(u + 1) * 128, :], in_=Vt[:, q, :]), [Vtb], [VTb[u]])
                    qs = range(nun) if d == 0 else range(nun - 1, -1, -1)
                    if "prep_only" in dbg:
                        qs = []
                    for q in qs:
                        u = t0 // 128 + q
                        tk = slice(q * 128, (q + 1) * 128)
                        TTa, TTab = TTall.next()
                        Mak, Makb = Makr.next()
                        Mrb, Mrbb = Mrbr.next()
                        Mrk, Mrkb = Mrkr.next()
                        for grp in range(2):
                            hl = [(2 * grp + j // 2, j % 2) for j in range(4)]
                            pA, pAb = psum()
                            pB, pBb = psum()
                            for j, (cc, hp) in enumerate(hl):
                                ps_ = slice(hp * 64, hp * 64 + 64)
                                S.op("pe", lambda e: e.matmul(
                                    pA[:, j * 128:(j + 1) * 128], lhsT=Bt[:, cc, tk], rhs=AtM[hp][:, cc, tk], start=True, stop=True), [Btb, MKB_], [pAb])
                                S.op("pe", lambda e: e.matmul(
                                    pB[:, j * 128:(j + 1) * 128], lhsT=At[:, cc, tk], rhs=BtM[hp][:, cc, tk], start=True, stop=True), [Atb, MKB_], [pBb])
                            v4 = lambda ap: ap.rearrange("p (j t) -> p j t", j=4)
                            oN, oT = (0, 4) if d == 0 else (4, 0)
                            mk4 = lambda i_: hmask[:, i_:i_ + 1, :].to_broadcast([128, 4, 128])
                            P_, Pb_ = Pr.next()
                            PT_, PTb_ = PTr.next()
                            Cs = []
                            S.op("dve", lambda e: e.tensor_tensor(out=P_[:], in0=v4(pB[:, :]), in1=mk4(oN), op=ALU.mult), [pBb, CB], [Pb_])
                            S.op("dve", lambda e: e.tensor_tensor(out=PT_[:], in0=v4(pA[:, :]), in1=mk4(oT), op=ALU.mult), [pAb, CB], [PTb_])
                            for lvl in range(1, 4):
                                C_, Cb_ = Cr.next()
                                S.op("dve", lambda e: e.tensor_tensor(out=C_[:], in0=v4(pB[:, :]), in1=mk4(oN + lvl), op=ALU.mult), [pBb, CB], [Cb_])
                                if lvl < 3:
                                    CT_, CTb_ = Cr.next()
                                    S.op("dve", lambda e: e.tensor_tensor(out=CT_[:], in0=v4(pA[:, :]), in1=mk4(oT + lvl), op=ALU.mult), [pAb, CB], [CTb_])
                                else:
                                    CT_, CTb_ = None, None
                                Cs.append((C_, Cb_, CT_, CTb_))
                            Tn, Tnb = TNr.next()
                            T_, Tb_ = TTr.next()
                            idb = identb[:, None, :].to_broadcast([128, 4, 128])
                            S.op("dve", lambda e: e.tensor_tensor(out=Tn[:], in0=P_[:], in1=idb, op=ALU.add), [Pb_, CB], [Tnb])
                            S.op("dve", lambda e: e.tensor_tensor(out=T_[:], in0=PT_[:], in1=idb, op=ALU.add), [PTb_, CB], [Tb_])
                            for (Mt, Mtb, lh, lhb, rh, rhb, mk_) in ((Mak, Makb, Kt, Ktb, AtM, MKB_, mS), (Mrb, Mrbb, Bt, Btb, RtM, MKB_, mI), (Mrk, Mrkb, Kt, Ktb, RtM, MKB_, mI)):
                                pM, pMb = psum()
                                for j, (cc, hp) in enumerate(hl):
                                    S.op("pe", lambda e: e.matmul(
                                        pM[:, j * 128:(j + 1) * 128], lhsT=lh[:, cc, tk], rhs=rh[hp][:, cc, tk], start=True, stop=True), [lhb, rhb], [pMb])
                                S.op("dve", lambda e, Mt=Mt, pM=pM, mk_=mk_, grp=grp: e.tensor_tensor(
                                    out=Mt[:, 4 * grp:4 * grp + 4, :], in0=v4(pM[:, :]), in1=masks[:, mk_:mk_ + 1, :].to_broadcast([128, 4, 128]), op=ALU.mult),
                                    [pMb, CB], [Mtb])
                            def mm4(lhs, lhsb, rhs, rhsb):
                                pt_, ptb_ = psum()
                                for j in range(4):
                                    S.op("pe", lambda e: e.matmul(pt_[:, j * 128:(j + 1) * 128], lhsT=lhs[:, j, :], rhs=rhs[:, j, :], start=True, stop=True),
                                         [lhsb, rhsb], [ptb_])
                                return pt_, ptb_
                            for i in range(3):
                                pn, pnb = mm4(PT_, PTb_, P_, Pb_)
                                pnt, pntb = mm4(P_, Pb_, PT_, PTb_)
                                P2, P2b = Pr.next()
                                PT2, PT2b = PTr.next()
                                copy_ev("act", P2[:], v4(pn[:, :]), [pnb], [P2b])
                                copy_ev("act", PT2[:], v4(pnt[:, :]), [pntb], [PT2b])
                                P_, Pb_, PT_, PTb_ = P2, P2b, PT2, PT2b
                                pd, pdb = mm4(PT_, PTb_, Tn, Tnb)
                                pdt, pdtb = mm4(P_, Pb_, T_, Tb_)
                                Tn2, Tn2b = TNr.next()
                                T2, T2b = TTr.next()
                                S.op("dve", lambda e: e.tensor_tensor(out=Tn2[:], in0=v4(pd[:, :]), in1=Tn[:], op=ALU.add), [pdb, Tnb], [Tn2b])
                                S.op("dve", lambda e: e.tensor_tensor(out=T2[:], in0=v4(pdt[:, :]), in1=T_[:], op=ALU.add), [pdtb, Tb_], [T2b])
                                Tn, Tnb, T_, Tb_ = Tn2, Tn2b, T2, T2b
                            for lvl in range(3):
                                C_, Cb_, CT_, CTb_ = Cs[lvl]
                                pzt, pztb = mm4(C_, Cb_, T_, Tb_)
                                Zt, Ztb = Zr.next()
                                copy_ev("act", Zt[:], v4(pzt[:, :]), [pztb], [Ztb])
                                if lvl < 2:
                                    pz, pzb = mm4(CT_, CTb_, Tn, Tnb)
                                    Zn, Znb = Zr.next()
                                    copy_ev("act", Zn[:], v4(pz[:, :]), [pzb], [Znb])
                                pwt, pwtb = mm4(Tn, Tnb, Zt, Ztb)
                                if lvl < 2:
                                    pw, pwb = mm4(T_, Tb_, Zn, Znb)
                                    Tn2, Tn2b = TNr.next()
                                    S.op("dve", lambda e: e.tensor_tensor(out=Tn2[:], in0=v4(pw[:, :]), in1=Tn[:], op=ALU.add), [pwb, Tnb], [Tn2b])
                                    T2, T2b = TTr.next()
                                    S.op("dve", lambda e: e.tensor_tensor(out=T2[:], in0=v4(pwt[:, :]), in1=T_[:], op=ALU.add), [pwtb, Tb_], [T2b])
                                    Tn, Tnb, T_, Tb_ = Tn2, Tn2b, T2, T2b
                                else:
                                    S.op("dve", lambda e: e.tensor_tensor(out=TTa[:, 4 * grp:4 * grp + 4, :], in0=v4(pwt[:, :]), in1=T_[:], op=ALU.add),
                                         [pwtb, Tb_], [TTab])
                        if "inv_only" in dbg:
                            continue
                        pX, pXb = psum()
                        for h in range(8):
                            cc, hp = h // 2, h % 2
                            ps_ = slice(hp * 64, hp * 64 + 64)
                            S.op("pe", lambda e, pX=pX, h=h, Mak=Mak, Vt=Vt, q=q: e.matmul(
                                pX[:, h * 64:(h + 1) * 64], lhsT=Mak[:, h, :], rhs=Vt[:, q, h * 64:(h + 1) * 64], start=True, stop=False), [Makb, Vtb], [pXb])
                            S.op("pe", lambda e, pX=pX, h=h, cc=cc, ps_=ps_: e.matmul(
                                pX[:, h * 64:(h + 1) * 64], lhsT=At[:, cc, tk], rhs=Sbf[:, cc, hp * 64:(hp + 1) * 64], start=False, stop=True), [Atb, SBB], [pXb])
                        Xs, Xsb = Xsr.next()
                        copy_ev("act", Xs[:], pX[:, :], [pXb], [Xsb])
                        if "seq1" in dbg:
                            continue
                        pU, pUb = psum()
                        for h in range(8):
                            S.op("pe", lambda e, pU=pU, h=h, TTa=TTa, Xs=Xs: e.matmul(
                                pU[:, h * 64:(h + 1) * 64], lhsT=TTa[:, h, :], rhs=Xs[:, h * 64:(h + 1) * 64], start=True, stop=True), [TTab, Xsb], [pUb])
                        Us, Usb = Usr.next()
                        copy_ev("dve", Us[:], pU[:, :], [pUb], [Usb])
                        if "seq2" in dbg:
                            continue
                        if need_y_ctx or u >= 2:
                            pY, pYb = psum()
                            for h in range(8):
                                cc, hp = h // 2, h % 2
                                ps_ = slice(hp * 64, hp * 64 + 64)
                                S.op("pe", lambda e, pY=pY, h=h, cc=cc, ps_=ps_: e.matmul(
                                    pY[:, h * 64:(h + 1) * 64], lhsT=Rt[:, cc, tk], rhs=Sbf[:, cc, hp * 64:(hp + 1) * 64], start=True, stop=False), [Rtb, SBB], [pYb])
                                S.op("pe", lambda e, pY=pY, h=h, Mrb=Mrb, Us=Us: e.matmul(
                                    pY[:, h * 64:(h + 1) * 64], lhsT=Mrb[:, h, :], rhs=Us[:, h * 64:(h + 1) * 64], start=False, stop=False), [Mrbb, Usb], [pYb])
                                S.op("pe", lambda e, pY=pY, h=h, Mrk=Mrk, Vt=Vt, q=q: e.matmul(
                                    pY[:, h * 64:(h + 1) * 64], lhsT=Mrk[:, h, :], rhs=Vt[:, q, h * 64:(h + 1) * 64], start=False, stop=True), [Mrkb, Vtb], [pYb])
                            Ys, Ysb = Ysr.next()
                            copy_ev("act", Ys[:], pY[:, :], [pYb], [Ysb])
                            S.dma("sp", lambda e, Ys=Ys, u=u: e.dma_start(out=Yd[d][u * 128:(u + 1) * 128, :], in_=Ys[:]), [Ysb], [Yb[d][u]])
                        if "seq3" in dbg:
                            continue
                        pS, pSb = psum()
                        for cc in range(4):
                            S.op("pe", lambda e, pS=pS, cc=cc, BhT=BhT, Us=Us, q=q: e.matmul(
                                pS[:, cc * 128:(cc + 1) * 128], lhsT=BhT[:, q, cc * 128:(cc + 1) * 128], rhs=Us[:, cc * 128:(cc + 1) * 128], start=True, stop=False),
                                [BhTb, Usb], [pSb])
                            S.op("pe", lambda e, pS=pS, cc=cc, KhT=KhT, Vt=Vt, q=q: e.matmul(
                                pS[:, cc * 128:(cc + 1) * 128], lhsT=KhT[:, q, cc * 128:(cc + 1) * 128], rhs=Vt[:, q, cc * 128:(cc + 1) * 128], start=False, stop=True),
                                [KhTb, Vtb], [pSb])
                        pS3 = pS[:, :].rearrange("p (c t) -> p c t", c=4)
                        S.op("dve", lambda e: e.tensor_tensor(out=Stmp[:], in0=pS3, in1=blockones[:, None, :].to_broadcast([128, 4, 128]), op=ALU.mult), [pSb, CB], [STB])
                        for cc in range(4):
                            S.op("dve", lambda e: e.scalar_tensor_tensor(out=Sf[:, cc, :], in0=Sf[:, cc, :], scalar=GL[:, cc, q:q + 1], in1=Stmp[:, cc, :],
                                                                         op0=ALU.mult, op1=ALU.add), [SFB, GLb, STB], [SFB])
                        S.op("act", lambda e: e.copy(out=Sbf[:], in_=Sf[:]), [SFB], [SBB])


        def stage_readout(l, units):
            lg = S.sb("lnxg", [128, 512], F32)
            lb = S.sb("lnxb", [128, 512], F32)
            LB = Buf()
            bload(lg[:], lnx_g[l:l + 1, :], LB)
            bload(lb[:], lnx_b[l:l + 1, :], LB)
            yr = Ring(S, "ry", [128, 512], F32, 4)
            tr = Ring(S, "rt", [128, 512], F32, 4)
            vr = Ring(S, "rv", [128, 512], BF16, 2)
            sr = Ring(S, "rs", [128, 32], F32, 4)
            ob = Ring(S, "rob", [128, 4, 128], BF16, 2)
            h8 = lambda ap: ap.rearrange("p (h c) -> p h c", h=8)
            ro_cut = 99
            for d_ in dbg:
                if d_.startswith("rocut"):
                    ro_cut = int(d_[5:])
            for u in units:
                rows = slice(u * 128, (u + 1) * 128)
                y0, y0b = yr.next()
                y1, y1b = yr.next()
                g_, gb_ = tr.next()
                v_, vb_ = vr.next()
                st, stb = sr.next()
                S.dma("sp", lambda e: e.dma_start(out=y0[:], in_=Yd[0][rows, :]), [Yb[0][u]], [y0b])
                S.dma("sp", lambda e: e.dma_start(out=y1[:], in_=Yd[1][rows, :]), [Yb[1][u]], [y1b])
                S.dma("sp", lambda e: e.dma_start(out=g_[:], in_=GTd[rows, :]), [GTb[u]], [gb_])
                S.dma("sp", lambda e: e.dma_start(out=v_[:], in_=VTd[rows, :]), [VTb[u]], [vb_])
                S.dma("sp", lambda e: e.dma_start(out=st[:, 0:8], in_=BONd[0][rows, :]), [BONb[0][u]], [stb])
                S.dma("sp", lambda e: e.dma_start(out=st[:, 8:16], in_=BONd[1][rows, :]), [BONb[1][u]], [stb])
                S.op("dve", lambda e: e.tensor_tensor(out=y0[:], in0=y0[:], in1=y1[:], op=ALU.add), [y0b, y1b], [y0b])
                S.op("dve", lambda e: e.tensor_tensor(out=st[:, 0:8], in0=st[:, 0:8], in1=st[:, 8:16], op=ALU.add), [stb], [stb])
                if ro_cut <= 1:
                    continue
                S.op("dve", lambda e: e.tensor_reduce(out=st[:, 16:24], in_=h8(y0[:]), axis=AX.X, op=ALU.add), [y0b], [stb])
                S.op("dve", lambda e: e.tensor_scalar(out=st[:, 16:24], in0=st[:, 16:24], scalar1=1.0 / 64, scalar2=None, op0=ALU.mult), [stb], [stb])
                for hh in range(8):
                    S.op("dve", lambda e: e.tensor_scalar(out=y0[:, hh * 64:(hh + 1) * 64], in0=y0[:, hh * 64:(hh + 1) * 64], scalar1=st[:, 16 + hh:17 + hh], scalar2=None, op0=ALU.subtract), [y0b, stb], [y0b])
                if ro_cut <= 2:
                    continue
                S.op("pool", lambda e: e.tensor_tensor(out=y1[:], in0=y0[:], in1=y0[:], op=ALU.mult), [y0b], [y1b])
                S.op("dve", lambda e: e.tensor_reduce(out=st[:, 24:32], in_=h8(y1[:]), axis=AX.X, op=ALU.add), [y1b], [stb])
                S.op("act", lambda e: e.activation(out=st[:, 24:32], in_=st[:, 24:32], func=AF.Sqrt, scale=1.0 / 64, bias=64e-5), [stb], [stb])
                S.op("dve", lambda e: e.reciprocal(out=st[:, 24:32], in_=st[:, 24:32]), [stb], [stb])
                for hh in range(8):
                    S.op("dve", lambda e: e.tensor_scalar(out=y0[:, hh * 64:(hh + 1) * 64], in0=y0[:, hh * 64:(hh + 1) * 64], scalar1=st[:, 24 + hh:25 + hh], scalar2=None, op0=ALU.mult), [y0b, stb], [y0b])
                if ro_cut <= 3:
                    continue
                S.op("pool", lambda e: e.tensor_tensor(out=y0[:], in0=y0[:], in1=lg[:], op=ALU.mult), [y0b, LB], [y0b])
                S.op("pool", lambda e: e.tensor_tensor(out=y0[:], in0=y0[:], in1=lb[:], op=ALU.add), [y0b, LB], [y0b])
                for hh in range(8):
                    S.op("dve", lambda e: e.tensor_scalar(out=y1[:, hh * 64:(hh + 1) * 64], in0=v_[:, hh * 64:(hh + 1) * 64], scalar1=st[:, hh:hh + 1], scalar2=None, op0=ALU.mult), [vb_, stb], [y1b])
                S.op("dve", lambda e: e.tensor_tensor(out=y0[:], in0=y0[:], in1=y1[:], op=ALU.add), [y0b, y1b], [y0b])
                S.op("dve", lambda e: e.tensor_tensor(out=y0[:], in0=y0[:], in1=g_[:], op=ALU.mult), [y0b, gb_], [y0b])
                if ro_cut <= 4:
                    continue
                pt, pb = psum()
                for q in range(4):
                    S.op("pe", lambda e: e.matmul(pt[:, q * 128:(q + 1) * 128], lhsT=y0[:, q * 128:(q + 1) * 128], rhs=ident[:, :], start=True, stop=True), [y0b, CB], [pb])
                o_, ob_ = ob.next()
                copy_ev("dve", o_[:], pt[:, :].rearrange("p (q t) -> p q t", q=4), [pb], [ob_])
                if ro_cut <= 5:
                    continue
                for q in range(4):
                    S.dma("sp", lambda e: e.dma_start(out=CATT[q * 128:(q + 1) * 128, u * 128:(u + 1) * 128], in_=o_[:, q, :]),
                          [ob_], [CATb[(0, u)]])

        def stage_pool(l, do_ctx):
            pw = S.sb("poolw", [64, 4, 64], F32)
            ps_ = S.sb("poolsc", [64, 4], F32)
            PWB = Buf()
            for g in range(4):
                S.dma("sp", lambda e: e.dma_start(out=pw[:, g, :], in_=pool_w[l, g]), [], [PWB])
            S.dma("sp", lambda e: e.dma_start(out=ps_[:], in_=psc[l]), [], [PWB])
            bufA = S.sb("poolA", [64, 80 * 80], F32)
            bufB = S.sb("poolB", [64, 80 * 80], F32)
            AB, BB = Buf(), Buf()
            ict = S.sb("poolic", [64, 64 * 64], F32)
            ICB = Buf()
            pmr = Ring(S, "poolpm", [64, 4096], F32, 2)
            por = Ring(S, "poolo", [64, 512], BF16, 3)
            seqs = ([(0, 1, 256, c_ic_ctx)] if do_ctx else []) + [(256, 256, 64, c_ic_lat)]
            for (tok0, R_, C_, ictab) in seqs:
                RB_ = 64 if R_ > 1 else 1
                Wp = C_ + 16
                for g in range(4):
                    nst = g + 1
                    hr = 8 if R_ > 1 else 0
                    for r0 in range(0, R_, RB_):
                        Hp = RB_ + 2 * hr
                        A3 = bufA[:, 0:Hp * Wp].rearrange("p (r c) -> p r c", c=Wp)
                        B3 = bufB[:, 0:Hp * Wp].rearrange("p (r c) -> p r c", c=Wp)
                        S.op("pool", lambda e: e.memset(bufA[:, 0:Hp * Wp], 0.0), [], [AB])
                        S.op("pool", lambda e: e.memset(bufB[:, 0:Hp * Wp], 0.0), [], [BB])
                        ra, rb = max(r0 - hr, 0), min(r0 + RB_ + hr, R_)
                        for rc in range(ra, rb, 16):
                            rd = min(rc + 16, rb)
                            src = PTd[1792 + 64 * g:1792 + 64 * (g + 1), tok0 + rc * C_:tok0 + rd * C_].rearrange("p (r c) -> p r c", c=C_)
                            S.dma("sp", lambda e: e.dma_start(out=A3[:, rc - (r0 - hr):rd - (r0 - hr), 8:8 + C_], in_=src),
                                  [PTb[(14 + g // 2, k)] for k in range(-1, 32)], [AB])
                        S.dma("sp", lambda e: e.dma_start(
                            out=ict[:, 0:RB_ * C_],
                            in_=ictab[g:g + 1, r0:r0 + RB_, :].rearrange("g r c -> g (r c)").to_broadcast([64, RB_ * C_])), [], [ICB])
                        cur, curb, oth, othb = A3, AB, B3, BB
                        shifts = [(1, 0), (1, 1), (2, 2), (4, 4)][:nst]
                        for (sa, sb_) in shifts:
                            S.op("dve", lambda e: e.tensor_tensor(out=oth[:, :, sa:Wp - sb_], in0=cur[:, :, 0:Wp - sb_ - sa], in1=cur[:, :, sa + sb_:Wp], op=ALU.add),
                                 [curb], [othb])
                            cur, curb, oth, othb = oth, othb, cur, curb
                        if R_ > 1:
                            for (sa, sb_) in shifts:
                                S.op("dve", lambda e: e.tensor_tensor(out=oth[:, sa:Hp - sb_, :], in0=cur[:, 0:Hp - sb_ - sa, :], in1=cur[:, sa + sb_:Hp, :], op=ALU.add),
                                     [curb], [othb])
                                cur, curb, oth, othb = oth, othb, cur, curb
                        pm, pmb = pmr.next()
                        pm3 = pm[:, 0:RB_ * C_].rearrange("p (r c) -> p r c", c=C_)
                        S.op("dve", lambda e: e.tensor_tensor(out=pm3, in0=cur[:, hr:hr + RB_, 8:8 + C_], in1=ict[:, 0:RB_ * C_].rearrange("p (r c) -> p r c", c=C_), op=ALU.mult),
                             [curb, ICB], [pmb])
                        for rc in range(0, RB_, 16):
                            rd = min(rc + 16, RB_)
                            S.dma("sp", lambda e: e.dma_start(out=oth[:, hr + rc:hr + rd, 8:8 + C_],
                                                              in_=PTd[1792 + 64 * g:1792 + 64 * (g + 1), tok0 + (r0 + rc) * C_:tok0 + (r0 + rd) * C_].rearrange("p (r c) -> p r c", c=C_)),
                                  [PTb[(14 + g // 2, k)] for k in range(-1, 32)], [othb])
                        S.op("dve", lambda e: e.tensor_tensor(out=pm3, in0=pm3, in1=oth[:, hr:hr + RB_, 8:8 + C_], op=ALU.subtract), [pmb, othb], [pmb])
                        ntok = RB_ * C_
                        for c0 in range(0, ntok, 512):
                            nn = min(512, ntok - c0)
                            pt, pb = psum()
                            S.op("pe", lambda e: e.matmul(pt[0:64, 0:nn], lhsT=pw[:, g, :], rhs=pm[:, c0:c0 + nn], start=True, stop=True), [PWB, pmb], [pb])
                            o_, ob_ = por.next()
                            S.op("act", lambda e: e.activation(out=o_[:, 0:nn], in_=pt[0:64, 0:nn], func=AF.Copy, scale=ps_[:, g:g + 1]), [pb, PWB], [ob_])
                            t_a = tok0 + r0 * C_ + c0
                            S.dma("sp", lambda e: e.dma_start(out=CATT[512 + 64 * g:512 + 64 * (g + 1), t_a:t_a + nn], in_=o_[:, 0:nn]), [ob_],
                                  [CATb[(1, g, t_a)]])

        def stage_fourier(l, do_ctx):
            d128 = S.sb("d128", [128, 3, 128], F32)
            tw = S.sb("twid", [128, 2, 128], F32)
            d64 = S.sb("d64", [64, 2, 2, 128], F32)
            fw = S.sb("fw", [64, 4, 64], F32)
            Gt = S.sb("Gt", [128, 2, 2, 64], F32)
            FB = Buf()
            S.dma("sp", lambda e: e.dma_start(out=d128[:], in_=c_dft128), [], [FB])
            S.dma("sp", lambda e: e.dma_start(out=tw[:], in_=c_twid), [], [FB])
            S.dma("sp", lambda e: e.dma_start(out=d64[:], in_=c_dft64pad), [], [FB])
            for h in range(4):
                S.dma("sp", lambda e: e.dma_start(out=fw[:, h, :], in_=fourier_w[l, h]), [], [FB])
            for h in range(4):
                for ci in range(2):
                    pt, pb = psum()
                    S.op("pe", lambda e: e.matmul(pt[:, 0:64], lhsT=d64[:, ci, h % 2, :], rhs=fw[:, h, :], start=True, stop=True), [FB], [pb])
                    hp = slice((h % 2) * 64, (h % 2) * 64 + 64)
                    if ci == 0:
                        S.op("dve", lambda e: e.tensor_copy(out=Gt[hp, h // 2, 0, :], in_=pt[hp, 0:64]), [pb], [FB])
                    else:
                        S.op("dve", lambda e: e.tensor_scalar(out=Gt[hp, h // 2, 1, :], in0=pt[hp, 0:64], scalar1=-1.0, scalar2=None, op0=ALU.mult), [pb], [FB])
            outr = Ring(S, "fo", [64, 512], BF16, 3)
            if do_ctx:
                d256 = S.sb("d256", [128, 2, 2, 256], F32)
                xc = S.sb("fxc", [128, 2, 256], F32)
                XB = Buf()
                S.dma("sp", lambda e: e.dma_start(out=d256[:], in_=c_dft256), [], [XB])
                S.dma("sp", lambda e: e.dma_start(out=xc[:], in_=PFd[0:256, :].rearrange("(a p) c -> p a c", p=128)), [PFb[0], PFb[1]], [XB])
                xri = S.sb("fxri", [128, 2, 2, 256], F32)
                XRB = Buf()
                for pair in range(2):
                    for ci in range(2):
                        pt, pb = psum()
                        for a in range(2):
                            S.op("pe", lambda e: e.matmul(pt[:, 0:256], lhsT=xc[:, a, pair * 128:(pair + 1) * 128], rhs=d256[:, a, ci, :], start=(a == 0), stop=(a == 1)),
                                 [XB], [pb])
                        copy_ev(evE(), xri[:, pair, ci, :], pt[:, 0:256], [pb], [XRB])
                for h in range(4):
                    hp = slice((h % 2) * 64, (h % 2) * 64 + 64)
                    pt, pb = psum()
                    S.op("pe", lambda e: e.matmul(pt[0:64, 0:256], lhsT=Gt[hp, h // 2, 0, :], rhs=xri[hp, h // 2, 0, :], start=True, stop=False), [FB, XRB], [pb])
                    S.op("pe", lambda e: e.matmul(pt[0:64, 0:256], lhsT=Gt[hp, h // 2, 1, :], rhs=xri[hp, h // 2, 1, :], start=False, stop=True), [FB, XRB], [pb])
                    o_, ob_ = outr.next()
                    S.op("dve", lambda e: e.tensor_scalar(out=o_[:, 0:256], in0=pt[0:64, 0:256], scalar1=8.0, scalar2=None, op0=ALU.mult), [pb], [ob_])
                    S.dma("sp", lambda e: e.dma_start(out=CATT[768 + 64 * h:768 + 64 * (h + 1), 0:256], in_=o_[:, 0:256]), [ob_], [CATb[(2, h, -1)]])
            x1 = S.sb("fx1", [128, 128 * 64], F32)
            X1B = Buf()
            x1c = S.sb("fx1c", [128, 128 * 64], F32)
            X1CB = Buf()
            zr = Ring(S, "fz", [128, 2, 128], F32, 3)
            t4 = Ring(S, "ft4", [128, 4, 128], F32, 2)
            xo = Ring(S, "fxo", [128, 2, 128], F32, 3)
            FXb = MBuf()
            for h in range(4):
                for part in range(16):
                    S.dma("sp", lambda e: e.dma_start(
                        out=x1[:, part * 512:(part + 1) * 512].rearrange("p (t c) -> p t c", c=64),
                        in_=PFd[256:TT, h * 64:(h + 1) * 64].rearrange("(t1 t2) c -> t1 t2 c", t2=128)[:, part * 8:(part + 1) * 8, :]),
                        [PFb[u] for u in range(2, NU)], [X1B])
                x13 = x1[:, :].rearrange("p (t c) -> p t c", c=64)
                x1c3 = x1c[:, :].rearrange("p (c t) -> p c t", t=128)
                for c8 in range(0, 64, 8):
                    S.op("dve" if (c8 // 8) % 2 else "pool", lambda e: e.tensor_copy(out=x1c3[:, c8:c8 + 8, :], in_=x13[:, :, c8:c8 + 8].rearrange("p t c -> p c t")),
                         [X1B], [X1CB])
                for c in range(64):
                    py, pyb = psum()
                    S.op("pe", lambda e: e.matmul(py[:, 0:256], lhsT=x1c3[:, c, :], rhs=d128[:, 0:2, :].rearrange("p a k -> p (a k)"), start=True, stop=True), [X1CB, FB], [pyb])
                    t_, tb_ = t4.next()
                    y3 = py[:, 0:256].rearrange("p (a k) -> p a k", a=2)
                    S.op("dve", lambda e: e.tensor_tensor(out=t_[:, 0:2, :], in0=y3, in1=tw[:, :, :], op=ALU.mult), [pyb, FB], [tb_])
                    S.op("dve", lambda e: e.tensor_tensor(out=t_[:, 2, :], in0=py[:, 0:128], in1=tw[:, 1, :], op=ALU.mult), [pyb, FB], [tb_])
                    S.op("dve", lambda e: e.tensor_tensor(out=t_[:, 3, :], in0=py[:, 128:256], in1=tw[:, 0, :], op=ALU.mult), [pyb, FB], [tb_])
                    z_, zb_ = zr.next()
                    S.op("pool", lambda e: e.tensor_tensor(out=z_[:, 0, :], in0=t_[:, 0, :], in1=t_[:, 1, :], op=ALU.subtract), [tb_], [zb_])
                    S.op("pool", lambda e: e.tensor_tensor(out=z_[:, 1, :], in0=t_[:, 2, :], in1=t_[:, 3, :], op=ALU.add), [tb_], [zb_])
                    px, pxb = psum()
                    S.op("pe", lambda e: e.matmul(px[:, 0:128], lhsT=d128[:, 0, :], rhs=z_[:, 0, :], start=True, stop=False), [FB, zb_], [pxb])
                    S.op("pe", lambda e: e.matmul(px[:, 0:128], lhsT=d128[:, 2, :], rhs=z_[:, 1, :], start=False, stop=True), [FB, zb_], [pxb])
                    pxi, pxib = psum()
                    S.op("pe", lambda e: e.matmul(pxi[:, 0:128], lhsT=d128[:, 0, :], rhs=z_[:, 1, :], start=True, stop=False), [FB, zb_], [pxib])
                    S.op("pe", lambda e: e.matmul(pxi[:, 0:128], lhsT=d128[:, 1, :], rhs=z_[:, 0, :], start=False, stop=True), [FB, zb_], [pxib])
                    x_, xb_ = xo.next()
                    copy_ev("act", x_[:, 0, :], px[:, 0:128], [pxb], [xb_])
                    copy_ev("act", x_[:, 1, :], pxi[:, 0:128], [pxib], [xb_])
                    ch = h * 64 + c
                    S.dma("sp", lambda e: e.dma_start(out=FXd[:, ch, :].rearrange("a (k2 k1) -> k2 a k1", k1=128), in_=x_[:]), [xb_], [FXb[h]])
                hp = slice((h % 2) * 64, (h % 2) * 64 + 64)
                fin = Ring(S, f"ffin{h}", [128, 2, 512], F32, 2)
                for c0 in range(0, T_LAT, 512):
                    fi, fib = fin.next()
                    S.dma("sp", lambda e: e.dma_start(out=fi[hp, :, :], in_=FXd[:, h * 64:(h + 1) * 64, c0:c0 + 512].rearrange("a c t -> c a t")), [FXb[h]], [fib])
                    pt, pb = psum()
                    S.op("pe", lambda e: e.matmul(pt[0:64, :], lhsT=Gt[hp, h // 2, 0, :], rhs=fi[hp, 0, :], start=True, stop=False), [FB, fib], [pb])
                    S.op("pe", lambda e: e.matmul(pt[0:64, :], lhsT=Gt[hp, h // 2, 1, :], rhs=fi[hp, 1, :], start=False, stop=True), [FB, fib], [pb])
                    o_, ob_ = outr.next()
                    copy_ev(evE(), o_[:, :], pt[0:64, :], [pb], [ob_])
                    S.dma("sp", lambda e: e.dma_start(out=CATT[768 + 64 * h:768 + 64 * (h + 1), 256 + c0:256 + c0 + 512], in_=o_[:, :]), [ob_], [CATb[(2, h, c0)]])


        H2b = MBuf()
        XEw = MBuf()
        MOEb = Buf()
        A_all = S.sb("A_all", [128, NU, 16], F32)
        AAB = Buf()
        ones128 = S.sb("ones128", [128, 128], F32)
        S.op("pool", lambda e: e.memset(ones128[:], 1.0), [], [CB])

        def stage_O(l, units):
            wout = S.sb("wout", [128, 8, D], BF16)
            WOB = Buf()
            for kc in range(8):
                S.dma("pool", lambda e: e.dma_start(out=wout[:, kc, :], in_=w_out[l, kc * 128:(kc + 1) * 128, :]), [], [WOB])
            rw = S.sb("rw", [128, 8, 16], F32)
            S.dma("sp", lambda e: e.dma_start(out=rw[:], in_=router_w[l].rearrange("(kc p) n -> p kc n", p=128)), [], [WOB])
            G1 = [S.sb(f"G1_{j}", [128, D], F32) for j in range(2)]
            G2 = [S.sb(f"G2_{j}", [128, D], F32) for j in range(2)]
            SH2 = [S.sb(f"SH2_{j}", [128, D], F32) for j in range(2)]
            gt = S.sb("gtO", [128, D], F32)
            GB = Buf()
            bload(gt[:], norm_ffn_g[l:l + 1, :], GB)
            for j, rowi in ((0, 1), (1, 0)):
                bload(G1[j][:], MODd[rowi:rowi + 1, 2 * D:3 * D], GB, [MODb])
                bload(G2[j][:], MODd[rowi:rowi + 1, 4 * D:5 * D], GB, [MODb])
                bload(SH2[j][:], MODd[rowi:rowi + 1, 3 * D:4 * D], GB, [MODb])
                S.op("dve", lambda e: e.scalar_tensor_tensor(out=G2[j][:], in0=G2[j][:], scalar=1.0, in1=gt[:], op0=ALU.add, op1=ALU.mult), [GB], [GB])
            catr = Ring(S, "catO", [128, 8, 128], BF16, 2)
            hr_ = Ring(S, "hO", [128, D], F32, 2)
            h2r = Ring(S, "h2O", [128, HW_], F32, 2)
            tmpr = Ring(S, "tmpO", [128, 512], F32, 2)
            sq = S.sb("sqO", [128, D], F32)
            SQB = Buf()
            str_ = Ring(S, "stO", [128, 8], F32, 4)
            h2T = Ring(S, "h2T", [128, 8, 128], F32, 2)
            idr = Ring(S, "idO", [128, 1], I32, 2)
            for u in units:
                j = 0 if u < 2 else 1
                rows = slice(u * 128, (u + 1) * 128)
                ct, ctb = catr.next()
                S.dma("sp", lambda e: e.dma_start(out=ct[:], in_=CATT[:, rows].rearrange("(kc p) t -> p kc t", p=128)),
                      [CATb[(0, u)]] + [b for k_, b in CATb.d.items() if k_[0] != 0], [ctb])
                h_, hb_ = hr_.next()
                S.dma("sp", lambda e: e.dma_start(out=h_[:], in_=HT[rows, :]), [HTb[u]], [hb_])
                for half in range(2):
                    pt, pb = psum()
                    for kc in range(8):
                        S.op("pe", lambda e: e.matmul(pt[:, :], lhsT=ct[:, kc, :], rhs=wout[:, kc, half * 512:(half + 1) * 512], start=(kc == 0), stop=(kc == 7)), [ctb, WOB], [pb])
                    t_, tb_ = tmpr.next()
                    S.op("dve", lambda e: e.tensor_tensor(out=t_[:], in0=pt[:, :], in1=G1[j][:, half * 512:(half + 1) * 512], op=ALU.mult), [pb, GB], [tb_])
                    S.op("pool", lambda e: e.tensor_tensor(out=h_[:, half * 512:(half + 1) * 512], in0=h_[:, half * 512:(half + 1) * 512], in1=t_[:], op=ALU.add), [hb_, tb_], [hb_])
                S.dma("sp", lambda e: e.dma_start(out=HT[rows, :], in_=h_[:]), [hb_], [HTb[u]])
                sc, scb = str_.next()
                S.op("act", lambda e: e.activation(out=sq[:], in_=h_[:], func=AF.Square, accum_out=sc[:, 0:1]), [hb_], [SQB, scb])
                S.op("act", lambda e: e.activation(out=sc[:, 1:2], in_=sc[:, 0:1], func=AF.Sqrt, scale=1.0 / D, bias=1e-6), [scb], [scb])
                S.op("dve", lambda e: e.reciprocal(out=sc[:, 1:2], in_=sc[:, 1:2]), [scb], [scb])
                h2, h2b = h2r.next()
                S.op("dve", lambda e: e.scalar_tensor_tensor(out=h2[:, 0:D], in0=h_[:], scalar=sc[:, 1:2], in1=G2[j][:], op0=ALU.mult, op1=ALU.mult), [hb_, scb, GB], [h2b])
                S.op("pool", lambda e: e.tensor_tensor(out=h2[:, 0:D], in0=h2[:, 0:D], in1=SH2[j][:], op=ALU.add), [h2b, GB], [h2b])
                hT, hTb = h2T.next()
                for half in range(2):
                    pt, pb = psum()
                    for q in range(4):
                        kc = half * 4 + q
                        S.op("pe", lambda e: e.transpose(pt[:, q * 128:(q + 1) * 128], h2[:, kc * 128:(kc + 1) * 128], ident[:]), [h2b, CB], [pb])
                    copy_ev(evE(), hT[:, half * 4:(half + 1) * 4, :], pt[:, :].rearrange("p (q t) -> p q t", q=4), [pb], [hTb])
                pl, plb = psum()
                for kc in range(8):
                    S.op("pe", lambda e: e.matmul(pl[:, 0:16], lhsT=hT[:, kc, :], rhs=rw[:, kc, :], start=(kc == 0), stop=(kc == 7)), [hTb, WOB], [plb])
                S.op("dve", lambda e: e.tensor_reduce(out=sc[:, 2:3], in_=pl[:, 0:16], axis=AX.X, op=ALU.max), [plb], [scb])
                S.op("dve", lambda e: e.tensor_scalar(out=sc[:, 2:3], in0=sc[:, 2:3], scalar1=-1.0, scalar2=None, op0=ALU.mult), [scb], [scb])
                S.op("act", lambda e: e.activation(out=h2[:, 1025:1041], in_=pl[:, 0:16], func=AF.Exp, bias=sc[:, 2:3], accum_out=sc[:, 3:4]), [plb, scb], [h2b, scb])
                S.op("dve", lambda e: e.reciprocal(out=sc[:, 3:4], in_=sc[:, 3:4]), [scb], [scb])
                S.op("dve", lambda e: e.tensor_scalar(out=h2[:, 1025:1041], in0=h2[:, 1025:1041], scalar1=sc[:, 3:4], scalar2=None, op0=ALU.mult), [h2b, scb], [h2b])
                S.op("pool", lambda e: e.tensor_copy(out=A_all[:, u, :], in_=h2[:, 1025:1041]), [h2b], [AAB])
                S.op("pool", lambda e: e.memset(h2[:, 1041:HW_], 0.0), [], [h2b])
                id_, idb_ = idr.next()
                S.op("pool", lambda e: e.iota(id_[:], pattern=[[1, 1]], base=u * 128, channel_multiplier=1), [], [idb_])
                S.op("pool", lambda e: e.tensor_copy(out=h2[:, 1024:1025].bitcast(I32), in_=id_[:]), [idb_], [h2b])
                S.dma("sp", lambda e: e.dma_start(out=H2d[rows, :], in_=h2[:]), [h2b], [H2b[u]])

        def stage_route(u0, nun, cap, slot0):
            J = nun
            Av = A_all[:, u0:u0 + nun, :]
            cmpA = S.sb("cmpA", [128, J, 16], F32)
            cmpB = S.sb("cmpB", [128, J, 16], F32)
            CAB, CBB = Buf(), Buf()
            lo = S.sb("lo", [128, 16], F32)
            mid = S.sb("mid", [128, 16], F32)
            cnt = S.sb("cnt", [128, 16], F32)
            ge = S.sb("ge", [128, 16], F32)
            LB_ = Buf()
            S.op("dve", lambda e: e.memset(lo[:], 0.0), [], [LB_])
            bc = lambda t_: t_[:, None, :].to_broadcast([128, J, 16])
            for it in range(26):
                hw = 2.0 ** -(it + 1)
                S.op("dve", lambda e: e.tensor_scalar(out=mid[:], in0=lo[:], scalar1=hw, scalar2=None, op0=ALU.add), [LB_], [LB_])
                S.op("dve", lambda e: e.tensor_tensor(out=cmpA[:], in0=Av, in1=bc(mid), op=ALU.is_ge), [AAB, LB_], [CAB])
                S.op("dve", lambda e: e.tensor_reduce(out=cnt[:], in_=cmpA[:].rearrange("p j e -> p e j"), axis=AX.X, op=ALU.add), [CAB], [LB_])
                pt, pb = psum()
                S.op("pe", lambda e: e.matmul(pt[:, 0:16], lhsT=ones128[:, :], rhs=cnt[:, :], start=True, stop=True), [CB, LB_], [pb])
                S.op("dve", lambda e: e.tensor_scalar(out=ge[:], in0=pt[:, 0:16], scalar1=float(cap), scalar2=None, op0=ALU.is_ge), [pb], [LB_])
                S.op("dve", lambda e: e.scalar_tensor_tensor(out=lo[:], in0=ge[:], scalar=hw, in1=lo[:], op0=ALU.mult, op1=ALU.add), [LB_], [LB_])
            Mk = S.sb("Mk", [128, J, 16], F32)
            MKB = Buf()
            S.op("dve", lambda e: e.tensor_tensor(out=Mk[:], in0=Av, in1=bc(lo), op=ALU.is_ge), [AAB, LB_], [MKB])
            S.op("dve", lambda e: e.tensor_reduce(out=cnt[:], in_=Mk[:].rearrange("p j e -> p e j"), axis=AX.X, op=ALU.add), [MKB], [LB_])
            pt, pb = psum()
            S.op("pe", lambda e: e.matmul(pt[:, 0:16], lhsT=masks[:, 0, :], rhs=cnt[:, :], start=True, stop=True), [CB, LB_], [pb])
            base = S.sb("base", [128, 16], F32)
            S.op("dve", lambda e: e.tensor_scalar(out=base[:], in0=pt[:, 0:16], scalar1=float(slot0), scalar2=None, op0=ALU.add), [pb], [LB_])
            S.op("dve", lambda e: e.tensor_copy(out=cmpA[:], in_=Mk[:]), [MKB], [CAB])
            cur, curb, oth, othb = cmpA, CAB, cmpB, CBB
            sh = 1
            while sh < J:
                S.op("dve", lambda e: e.tensor_tensor(out=oth[:, sh:J, :], in0=cur[:, sh:J, :], in1=cur[:, 0:J - sh, :], op=ALU.add), [curb], [othb])
                S.op("pool", lambda e: e.tensor_copy(out=oth[:, 0:sh, :], in_=cur[:, 0:sh, :]), [curb], [othb])
                cur, curb, oth, othb = oth, othb, cur, curb
                sh *= 2
            S.op("dve", lambda e: e.tensor_tensor(out=cur[:], in0=cur[:], in1=Mk[:], op=ALU.subtract), [curb, MKB], [curb])
            S.op("dve", lambda e: e.tensor_tensor(out=cur[:], in0=cur[:], in1=bc(base), op=ALU.add), [curb, LB_], [curb])
            S.op("dve", lambda e: e.scalar_tensor_tensor(out=cur[:], in0=cur[:], scalar=-BIGSLOT, in1=Mk[:], op0=ALU.add, op1=ALU.mult), [curb, MKB], [curb])
            S.op("dve", lambda e: e.tensor_scalar(out=cur[:], in0=cur[:], scalar1=BIGSLOT, scalar2=None, op0=ALU.add), [curb], [curb])
            SL = S.sb("SL", [128, J, 16], I32)
            SLB = Buf()
            S.op("dve", lambda e: e.tensor_copy(out=SL[:], in_=cur[:]), [curb], [SLB])
            xr = Ring(S, "h2disp", [128, HW_], F32, 3)
            for jj in range(nun):
                u = u0 + jj
                x_, xb_ = xr.next()
                S.dma("sp", lambda e: e.dma_start(out=x_[:], in_=H2d[u * 128:(u + 1) * 128, :]), [H2b[u]], [xb_])
                for ex in range(16):
                    S.dma("pool", lambda e: e.indirect_dma_start(
                        out=XEd[ex], out_offset=bass.IndirectOffsetOnAxis(ap=SL[:, jj, ex:ex + 1], axis=0),
                        in_=x_[:, :], in_offset=None, bounds_check=RegRef(slot0 + cap - 1), oob_is_err=False), [xb_, SLB], [XEw[(ex, u)]])

        def stage_ffn(l, chunks):
            zero1k = S.sb("zero1k", [128, D], F32)
            S.op("pool", lambda e: e.memset(zero1k[:], 0.0), [], [CB])
            for u in range(NU):
                S.dma("sp", lambda e: e.dma_start(out=MOEd[u * 128:(u + 1) * 128, :], in_=zero1k[:]), [CB], [MOEb])
            wr = [Ring(S, f"ew{k_}", [128, 8, D], BF16, 2) for k_ in range(3)]
            xer = Ring(S, "xe", [128, HW_], F32, 3)
            xeT = S.sb("xeT", [128, 8, 512], BF16)
            XTB = Buf()
            hT = S.sb("hTf", [128, 8, 512], BF16)
            HTB_ = Buf()
            tmpr = Ring(S, "ftmp", [128, 512], F32, 2)
            yer = Ring(S, "ye", [128, D], F32, 2)
            gc = S.sb("gcol", [128, 4], F32)
            ix = [S.sb(f"ixc{i}", [128, 1], I32) for i in range(4)]
            GCB = Buf()
            for ex in range(16):
                W = []
                for k_, src in enumerate((exp_w_gate, exp_w_up, exp_w_down)):
                    wt, wb = wr[k_].next()
                    for kc in range(8):
                        S.dma("pool", lambda e: e.dma_start(out=wt[:, kc, :], in_=src[l, ex, kc * 128:(kc + 1) * 128, :]), [], [wb])
                    W.append((wt, wb))
                (wg, wgb), (wu, wub), (wd, wdb) = W
                for (s0, ns) in chunks:
                    subs = [(i * 128, min(128, ns - i * 128)) for i in range((ns + 127) // 128)]
                    for si, (so, sn) in enumerate(subs):
                        xe, xeb = xer.next()
                        S.dma("sp", lambda e: e.dma_start(out=xe[0:sn, :], in_=XEd[ex][s0 + so:s0 + so + sn, :]), [XEw[(ex, u_)] for u_ in range(NU)], [xeb])
                        S.op("pool", lambda e: e.tensor_copy(out=gc[0:sn, si:si + 1], in_=xe[0:sn, 1025 + ex:1026 + ex]), [xeb], [GCB])
                        S.op("pool", lambda e: e.tensor_copy(out=ix[si][0:sn, :], in_=xe[0:sn, 1024:1025].bitcast(I32)), [xeb], [GCB])
                        for half in range(2):
                            pt, pb = psum()
                            for q in range(4):
                                kc = half * 4 + q
                                S.op("pe", lambda e: e.transpose(pt[:, q * 128:q * 128 + sn], xe[0:sn, kc * 128:(kc + 1) * 128], ident[0:sn, 0:sn]), [xeb, CB], [pb])
                            copy_ev(evE(), xeT[:, half * 4:(half + 1) * 4, so:so + sn], pt[:, :].rearrange("p (q t) -> p q t", q=4)[:, :, 0:sn], [pb], [XTB])
                    for fc in range(8):
                        pg, pgb = psum()
                        pu, pub = psum()
                        for kc in range(8):
                            S.op("pe", lambda e: e.matmul(pg[:, 0:ns], lhsT=wg[:, kc, fc * 128:(fc + 1) * 128], rhs=xeT[:, kc, 0:ns], start=(kc == 0), stop=(kc == 7)), [wgb, XTB], [pgb])
                        for kc in range(8):
                            S.op("pe", lambda e: e.matmul(pu[:, 0:ns], lhsT=wu[:, kc, fc * 128:(fc + 1) * 128], rhs=xeT[:, kc, 0:ns], start=(kc == 0), stop=(kc == 7)), [wub, XTB], [pub])
                        t_, tb_ = tmpr.next()
                        S.op("act", lambda e: e.activation(out=t_[:, 0:ns], in_=pg[:, 0:ns], func=AF.Silu), [pgb], [tb_])
                        S.op("dve", lambda e: e.tensor_tensor(out=hT[:, fc, 0:ns], in0=t_[:, 0:ns], in1=pu[:, 0:ns], op=ALU.mult), [tb_, pub], [HTB_])
                    for si, (so, sn) in enumerate(subs):
                        ye, yeb = yer.next()
                        for half in range(2):
                            pt, pb = psum()
                            for fc in range(8):
                                S.op("pe", lambda e: e.matmul(pt[0:sn, :], lhsT=hT[:, fc, so:so + sn], rhs=wd[:, fc, half * 512:(half + 1) * 512], start=(fc == 0), stop=(fc == 7)), [HTB_, wdb], [pb])
                            S.op("dve", lambda e: e.tensor_scalar(out=ye[0:sn, half * 512:(half + 1) * 512], in0=pt[0:sn, :], scalar1=gc[0:sn, si:si + 1], scalar2=None, op0=ALU.mult), [pb, GCB], [yeb])
                        S.dma("pool", lambda e: e.indirect_dma_start(
                            out=MOEd, out_offset=bass.IndirectOffsetOnAxis(ap=ix[si][0:sn, :], axis=0),
                            in_=ye[0:sn, :], in_offset=None, bounds_check=RegRef(TT - 1), oob_is_err=True, compute_op=ALU.add), [yeb, GCB], [MOEb])

        def stage_combine(l, units):
            G2g = [S.sb(f"G2g{j}", [128, D], F32) for j in range(2)]
            GB = Buf()
            for j, rowi in ((0, 1), (1, 0)):
                bload(G2g[j][:], MODd[rowi:rowi + 1, 5 * D:6 * D], GB, [MODb])
            hr_ = Ring(S, "hC", [128, D], F32, 3)
            mr_ = Ring(S, "mC", [128, D], F32, 3)
            for u in units:
                j = 0 if u < 2 else 1
                rows = slice(u * 128, (u + 1) * 128)
                h_, hb_ = hr_.next()
                m_, mb_ = mr_.next()
                S.dma("sp", lambda e: e.dma_start(out=h_[:], in_=HT[rows, :]), [HTb[u]], [hb_])
                S.dma("sp", lambda e: e.dma_start(out=m_[:], in_=MOEd[rows, :]), [MOEb], [mb_])
                S.op("dve", lambda e: e.tensor_tensor(out=m_[:], in0=m_[:], in1=G2g[j][:], op=ALU.mult), [mb_, GB], [mb_])
                S.op("pool", lambda e: e.tensor_tensor(out=h_[:], in0=h_[:], in1=m_[:], op=ALU.add), [hb_, mb_], [hb_])
                S.dma("sp", lambda e: e.dma_start(out=HT[rows, :], in_=h_[:]), [hb_], [HTb[u]])

        def stage_final():
            gt = S.sb("fng", [128, D], F32)
            FGB = Buf()
            bload(gt[:], final_norm_g[0:1, :], FGB)
            xr = Ring(S, "fin_x", [128, D], F32, 3)
            sq = S.sb("fin_sq", [128, D], F32)
            SQB = Buf()
            st_ = Ring(S, "fin_st", [128, 2], F32, 4)
            OB = MBuf()
            for u in range(2, NU):
                xi, xib = xr.next()
                S.dma("sp", lambda e: e.dma_start(out=xi[:], in_=HT[u * 128:(u + 1) * 128, :]), [HTb[u]], [xib])
                sc, scb = st_.next()
                S.op("act", lambda e: e.activation(out=sq[:], in_=xi[:], func=AF.Square, accum_out=sc[:, 0:1]), [xib], [SQB, scb])
                S.op("act", lambda e: e.activation(out=sc[:, 1:2], in_=sc[:, 0:1], func=AF.Sqrt, scale=1.0 / D, bias=1e-6), [scb], [scb])
                S.op("dve", lambda e: e.reciprocal(out=sc[:, 1:2], in_=sc[:, 1:2]), [scb], [scb])
                S.op("dve", lambda e: e.scalar_tensor_tensor(out=xi[:], in0=xi[:], scalar=sc[:, 1:2], in1=gt[:], op0=ALU.mult, op1=ALU.mult), [xib, scb, FGB], [xib])
                S.dma("sp", lambda e: e.dma_start(out=out[(u - 2) * 128:(u - 1) * 128, :], in_=xi[:]), [xib], [OB[u]])

        nlayers = 1 if "l0only" in dbg else DEPTH
        for l in range(nlayers):
            last = (l == DEPTH - 1)
            units = range(2, NU) if last else range(NU)
            if "noadaln" not in dbg:
                with S.scope():
                    stage_adaln(l)
            if "noA" not in dbg:
                with S.scope():
                    stage_A(l)
            if "noscan" not in dbg:
                with S.scope():
                    stage_rwkv(l, need_y_ctx=not last)
                if "noreadout" not in dbg:
                    with S.scope():
                        stage_readout(l, range(2) if "blocks1" in dbg else (units if "short" not in dbg else range(10)))
            if "nopf" not in dbg:
                if "nopool" not in dbg:
                    with S.scope():
                        stage_pool(l, not last)
                if "nofourier" not in dbg:
                    with S.scope():
                        stage_fourier(l, not last)
            if "noO" not in dbg:
                with S.scope():
                    stage_O(l, units)
            if "nomoe" not in dbg:
                if not last:
                    with S.scope():
                        stage_route(0, 2, 32, 2048)
                with S.scope():
                    stage_route(2, 128, 2048, 0)
                with S.scope():
                    stage_ffn(l, [(0, 512), (512, 512), (1024, 512), (1536, 512)] + ([] if last else [(2048, 32)]))
                with S.scope():
                    stage_combine(l, units)
        with S.scope():
            stage_final()
        S.finish()
        S.run_block()
        print("instructions:", S.n_instr)
    return nc


def prep_inputs(inputs, b):
    hc = host_consts()
    m = dict(hc)
    f = lambda a: np.ascontiguousarray(np.asarray(a, dtype=np.float32))
    m["x"] = f(inputs["x"][b])
    m["ctx"] = f(inputs["ctx"][b])
    cc = np.zeros((128, 16), np.float32)
    cc[:, 0:16:2] = np.asarray(inputs["c"][b]).reshape(8, 128).T
    cc[:, 1:16:2] = np.asarray(inputs["c_ctx"]).reshape(8, 128).T
    m["ccond"] = cc
    for k in ("ada_w", "ada_b", "norm_mix_g", "norm_ffn_g", "w_in", "decay_w0", "decay_up", "iclr_up", "gate_up", "lnx_g", "lnx_b"):
        m[k] = f(inputs[k])
    pp = np.zeros((DEPTH, 128, 64), np.float32)
    for l in range(DEPTH):
        sw = np.asarray(inputs["shift_w"][l])
        pp[l, :, 0:42] = sw.reshape(3, 14, 128).transpose(2, 1, 0).reshape(128, 42)
        for d in range(2):
            pp[l, :, 42 + 4 * d:46 + 4 * d] = np.asarray(inputs["iclr_a0"][l, d]).reshape(4, 128).T
        pp[l, :, 50:54] = np.asarray(inputs["k_k"][l]).reshape(4, 128).T
        pp[l, :, 54:58] = np.asarray(inputs["k_a"][l]).reshape(4, 128).T
        pp[l, :, 58:62] = np.asarray(inputs["r_k"][l]).reshape(4, 128).T
        pp[l, :, 62:64] = np.asarray(inputs["pool_scale"][l]).reshape(2, 128).T
    m["pp"] = pp
    for k in ("pool_w", "fourier_w", "w_out", "router_w", "exp_w_gate", "exp_w_up", "exp_w_down"):
        m[k] = f(inputs[k])
    m["final_norm_g"] = f(inputs["final_norm_g"]).reshape(1, D)
    m["psc"] = np.ascontiguousarray(f(inputs["pool_scale"]).reshape(DEPTH, 4, 64).transpose(0, 2, 1))
    return m


_NC_CACHE = {}


def kernel(**inputs):
    if "nc" not in _NC_CACHE:
        _NC_CACHE["nc"] = build()
    nc = _NC_CACHE["nc"]
    in_maps = [prep_inputs(inputs, b) for b in range(2)]
    res = run_bass_kernel_spmd(nc, in_maps, core_ids=[0, 1])
    return np.stack([res.results[b]["out"] for b in range(2)], 0)
```
